# Optimizing a Trainium2 kernel written in Bass

```python
import jax, jax.numpy as jnp
from jax import lax
import numpy as np

D_MODEL = 1024
BATCH = 4
SEQ = 4096
DEPTH = 1

GRID_W = 64
CTX_LEN = 256
EPS = 1e-6

D_RNN = D_MODEL
RG_BLOCKS = 16
RG_BW = D_RNN // RG_BLOCKS
RG_CONV = 4
RG_C = 8.0

DN_HEADS = 8
DN_DK = 128
DN_DV = 128
DN_QK = DN_HEADS * DN_DK
DN_VW = DN_HEADS * DN_DV
DN_QKV = 2 * DN_QK + DN_VW
DN_CONV = 4
DN_CHUNK = 64

N_EXPERTS = 32
TOP_K = 4
D_EXPERT = D_MODEL
SWIGLU_LIMIT = 7.0
SWIGLU_ALPHA = 1.702

IN_SIZES = (D_RNN, D_RNN, DN_QK, DN_QK, DN_VW, DN_VW, 2 * DN_HEADS, 2 * DN_HEADS, D_MODEL, D_MODEL)
N_IN = sum(IN_SIZES)

kernel_name = 'hybrid_rglru_gdn_moe_dit_block'


def rmsnorm(x, g):
    xf = x.astype(jnp.float32)
    y = xf * lax.rsqrt(jnp.mean(xf * xf, axis=-1, keepdims=True) + EPS)
    return (y * g.astype(jnp.float32)).astype(x.dtype)


def modulate(h, shift, scale):
    return h * (1 + scale) + shift


def flip(t):
    return jnp.flip(t, axis=1)


def split_cols(p):
    offs, o = [], 0
    for s in IN_SIZES[:-1]:
        o += s
        offs.append(o)
    return jnp.split(p, offs, axis=-1)


def causal_dwconv(x, w, b=None):
    K, C = w.shape
    y = lax.conv_general_dilated(x, w[:, None, :].astype(x.dtype), window_strides=(1,),
                                 padding=[(K - 1, 0)], dimension_numbers=('NWC', 'WIO', 'NWC'),
                                 feature_group_count=C)
    return y if b is None else y + b


def linear_scan(a, b, h0):
    if h0 is not None:
        b = b.at[:, 0].add(a[:, 0] * h0)

    def combine(l, r):
        return (l[0] * r[0], r[0] * l[1] + r[1])

    _, h = lax.associative_scan(combine, (a, b), axis=1)
    return h


def rglru_coeffs(x, wa, ba, wi, bi, lam, reset_first):
    B, T, _ = x.shape
    xf = x.astype(jnp.float32)
    xb = xf.reshape(B, T, RG_BLOCKS, RG_BW)
    r = jax.nn.sigmoid(jnp.einsum('btnd,nde->btne', xb, wa.astype(jnp.float32)).reshape(B, T, D_RNN) + ba)
    i = jax.nn.sigmoid(jnp.einsum('btnd,nde->btne', xb, wi.astype(jnp.float32)).reshape(B, T, D_RNN) + bi)
    log_a = -RG_C * r * jax.nn.softplus(-lam.astype(jnp.float32))
    a = jnp.exp(log_a)
    mult = jnp.sqrt(-jnp.expm1(2.0 * log_a))
    if reset_first:
        mult = mult.at[:, 0].set(1.0)
    return a, mult * (i * xf)


def rglru_direction(xc, xl, conv_w, conv_b, wa, ba, wi, bi, lam):
    xc = causal_dwconv(xc, conv_w, conv_b)
    xl = causal_dwconv(xl, conv_w, conv_b)
    a_c, b_c = rglru_coeffs(xc, wa, ba, wi, bi, lam, True)
    h_c = linear_scan(a_c, b_c, None)
    a_l, b_l = rglru_coeffs(xl, wa, ba, wi, bi, lam, False)
    h_l = linear_scan(a_l, b_l, h_c[:, -1])
    return h_c, h_l


def rglru_bidir(xc, xl, conv_w, conv_b, wa, ba, wi, bi, lam):
    fc, fl = rglru_direction(xc, xl, conv_w[0], conv_b[0], wa[0], ba[0], wi[0], bi[0], lam[0])
    bc, bl = rglru_direction(flip(xc), flip(xl), conv_w[1], conv_b[1], wa[1], ba[1], wi[1], bi[1], lam[1])
    return fc + flip(bc), fl + flip(bl)


def l2norm(t):
    return t * lax.rsqrt(jnp.sum(t * t, axis=-1, keepdims=True) + EPS)


def dn_prepare(qkv, alpha, beta_logit, conv_w, a_log, dt_bias):
    B, T, _ = qkv.shape
    qkv = jax.nn.silu(causal_dwconv(qkv, conv_w)).astype(jnp.float32)
    q, k, v = jnp.split(qkv, [DN_QK, 2 * DN_QK], axis=-1)
    q = l2norm(q.reshape(B, T, DN_HEADS, DN_DK)) * (DN_DK ** -0.5)
    k = l2norm(k.reshape(B, T, DN_HEADS, DN_DK))
    v = v.reshape(B, T, DN_HEADS, DN_DV)
    g = -jnp.exp(a_log.astype(jnp.float32)) * jax.nn.softplus(alpha.astype(jnp.float32) + dt_bias.astype(jnp.float32))
    beta = jax.nn.sigmoid(beta_logit.astype(jnp.float32))
    return q, k, v, g, beta


def chunk_gated_delta(q, k, v, g, beta, h0):
    B, T, H, DK = q.shape
    DV = v.shape[-1]
    N = T // DN_CHUNK

    def chunks(t):
        return t.reshape(B, N, DN_CHUNK, H, -1).transpose(1, 0, 3, 2, 4)

    qc, kc, vc = chunks(q), chunks(k), chunks(v)
    gc = chunks(g[..., None])[..., 0]
    bc = chunks(beta[..., None])[..., 0]
    gcum = jnp.cumsum(gc, axis=-1)
    idx = jnp.arange(DN_CHUNK)
    causal = idx[:, None] >= idx[None, :]
    strict = idx[:, None] > idx[None, :]
    decay = jnp.where(causal, jnp.exp(jnp.where(causal, gcum[..., :, None] - gcum[..., None, :], 0.0)), 0.0)
    kb = kc * bc[..., None]
    lower = jnp.where(strict, jnp.einsum('nbhid,nbhjd->nbhij', kb, kc) * decay, 0.0)
    tri = lower + jnp.eye(DN_CHUNK, dtype=jnp.float32)
    rhs = jnp.concatenate([vc * bc[..., None], kb * jnp.exp(gcum)[..., None]], axis=-1)
    sol = lax.linalg.triangular_solve(tri, rhs, left_side=True, lower=True)
    u, w = sol[..., :DV], sol[..., DV:]
    qk = jnp.einsum('nbhid,nbhjd->nbhij', qc, kc) * decay

    def step(h, inp):
        qi, ki, ui, wi, gi, qki = inp
        v_new = ui - jnp.einsum('bhck,bhkv->bhcv', wi, h)
        o = jnp.einsum('bhck,bhkv->bhcv', qi * jnp.exp(gi)[..., None], h) + jnp.einsum('bhij,bhjv->bhiv', qki, v_new)
        g_last = gi[..., -1:]
        h = h * jnp.exp(g_last)[..., None] + jnp.einsum('bhck,bhcv->bhkv', ki * jnp.exp(g_last - gi)[..., None], v_new)
        return h, o

    h, o = lax.scan(step, h0, (qc, kc, u, w, gcum, qk))
    return o.transpose(1, 0, 3, 2, 4).reshape(B, T, H, DV), h


def delta_direction(qkv_c, qkv_l, al_c, al_l, be_c, be_l, conv_w, a_log, dt_bias):
    qc, kc, vc, gc, bc = dn_prepare(qkv_c, al_c, be_c, conv_w, a_log, dt_bias)
    h0 = jnp.zeros((qkv_c.shape[0], DN_HEADS, DN_DK, DN_DV), jnp.float32)
    oc, hc = chunk_gated_delta(qc, kc, vc, gc, bc, h0)
    ql, kl, vl, gl, bl = dn_prepare(qkv_l, al_l, be_l, conv_w, a_log, dt_bias)
    ol, _ = chunk_gated_delta(ql, kl, vl, gl, bl, hc)
    return oc, ol


def delta_bidir(qkv_c, qkv_l, al_c, al_l, be_c, be_l, conv_w, a_log, dt_bias):
    H = DN_HEADS
    fc, fl = delta_direction(qkv_c, qkv_l, al_c[..., :H], al_l[..., :H], be_c[..., :H], be_l[..., :H],
                             conv_w[0], a_log[0], dt_bias[0])
    bc, bl = delta_direction(flip(qkv_c), flip(qkv_l), flip(al_c[..., H:]), flip(al_l[..., H:]),
                             flip(be_c[..., H:]), flip(be_l[..., H:]), conv_w[1], a_log[1], dt_bias[1])
    oc = fc + flip(bc)
    ol = fl + flip(bl)
    return oc.reshape(oc.shape[0], oc.shape[1], -1), ol.reshape(ol.shape[0], ol.shape[1], -1)


def merge(parts, rg_h, dn_o, dn_norm_g, rg_w_o, dn_w_o, w_out):
    _, rg_gate, _, _, _, z, _, _, gate_rg, gate_dn = parts
    dt = rg_gate.dtype
    B, T, _ = rg_gate.shape
    y_rg = (rg_h * jax.nn.gelu(rg_gate.astype(jnp.float32))).astype(dt) @ rg_w_o
    o = rmsnorm(dn_o.reshape(B, T, DN_HEADS, DN_DV), dn_norm_g) * jax.nn.silu(z.astype(jnp.float32).reshape(B, T, DN_HEADS, DN_DV))
    y_dn = o.reshape(B, T, DN_VW).astype(dt) @ dn_w_o
    merged = jax.nn.sigmoid(gate_rg) * y_rg + jax.nn.sigmoid(gate_dn) * y_dn
    return merged @ w_out


def token_mixer(h_lat, h_ctx, rows, need_ctx, w_in, rg_conv_w, rg_conv_b, rg_wa, rg_ba, rg_wi, rg_bi, rg_lam,
                rg_w_o, dn_conv_w, dn_a_log, dn_dt_bias, dn_norm_g, dn_w_o, w_out):
    B, S, _ = h_lat.shape
    pl = split_cols(h_lat @ w_in)
    pc = split_cols(h_ctx @ w_in)
    rg_c, rg_l = rglru_bidir(pc[0], pl[0], rg_conv_w, rg_conv_b, rg_wa, rg_ba, rg_wi, rg_bi, rg_lam)

    def to_col(t):
        return t.reshape(B, rows, GRID_W, -1).transpose(0, 2, 1, 3).reshape(B, S, -1)

    def from_col(t):
        return t.reshape(B, GRID_W, rows, -1).transpose(0, 2, 1, 3).reshape(B, S, -1)

    qkv_c = jnp.concatenate(pc[2:5], axis=-1)
    qkv_l = to_col(jnp.concatenate(pl[2:5], axis=-1))
    dn_c, dn_l = delta_bidir(qkv_c, qkv_l, pc[6], to_col(pl[6]), pc[7], to_col(pl[7]),
                             dn_conv_w, dn_a_log, dn_dt_bias)
    dn_l = from_col(dn_l)
    y_lat = merge(pl, rg_l, dn_l, dn_norm_g, rg_w_o, dn_w_o, w_out)
    y_ctx = merge(pc, rg_c, dn_c, dn_norm_g, rg_w_o, dn_w_o, w_out) if need_ctx else None
    return y_lat, y_ctx


def moe(tok, router_w, router_b, e_w1, e_b1, e_w2, e_b2):
    logits = (tok @ router_w).astype(jnp.float32) + router_b
    top_v, top_i = lax.top_k(logits, TOP_K)
    top_w = jax.nn.softmax(top_v, axis=-1)
    combine = jnp.sum(jax.nn.one_hot(top_i, N_EXPERTS, dtype=jnp.float32) * top_w[..., None], axis=1)

    def expert(acc, p):
        w1, b1, w2, b2, cw = p
        hh = (tok @ w1 + b1).astype(jnp.float32)
        glu, lin = jnp.split(hh, 2, axis=-1)
        glu = jnp.minimum(glu, SWIGLU_LIMIT)
        lin = jnp.clip(lin, -SWIGLU_LIMIT, SWIGLU_LIMIT)
        act = glu * jax.nn.sigmoid(SWIGLU_ALPHA * glu) * (lin + 1.0)
        out = act.astype(tok.dtype) @ w2 + b2
        return acc + cw[:, None] * out.astype(jnp.float32), None

    acc, _ = lax.scan(expert, jnp.zeros(tok.shape, jnp.float32), (e_w1, e_b1, e_w2, e_b2, combine.T))
    return acc.astype(tok.dtype)


def setup_inputs(seed: int = 0) -> dict:
    key = jax.random.key(seed)
    ks = jax.random.split(key, 31)
    f32 = jnp.float32
    D, L = D_MODEL, DEPTH

    def nrm(k, shape, scale):
        return jax.random.normal(k, shape, f32) * scale

    def gain(k, shape):
        return 1.0 + 0.1 * jax.random.normal(k, shape, f32)

    u = jax.random.uniform(ks[15], (L, 2, D_RNN), f32, 0.9, 0.999)
    s = u ** (1.0 / RG_C)
    rg_lam = jnp.log(s) - jnp.log1p(-s)
    dn_a_log = jnp.log(jax.random.uniform(ks[18], (L, 2, DN_HEADS), f32, 1.0, 16.0))
    return {
        'x': nrm(ks[0], (BATCH, SEQ, D), 1.0),
        'c': nrm(ks[1], (BATCH, D), 1.0),
        'ctx': nrm(ks[2], (BATCH, CTX_LEN, D), 1.0),
        'c_ctx': nrm(ks[3], (D,), 1.0),
        'ada_w': nrm(ks[4], (L, D, 6 * D), 0.5 * D ** -0.5),
        'ada_b': nrm(ks[5], (L, 6 * D), 0.02),
        'mix_pre_g': gain(ks[6], (L, D)),
        'mix_post_g': gain(ks[7], (L, D)),
        'w_in': nrm(ks[8], (L, D, N_IN), D ** -0.5),
        'rg_conv_w': nrm(ks[9], (L, 2, RG_CONV, D_RNN), RG_CONV ** -0.5),
        'rg_conv_b': nrm(ks[10], (L, 2, D_RNN), 0.02),
        'rg_wa': nrm(ks[11], (L, 2, RG_BLOCKS, RG_BW, RG_BW), RG_BW ** -0.5),
        'rg_ba': nrm(ks[12], (L, 2, D_RNN), 0.02),
        'rg_wi': nrm(ks[13], (L, 2, RG_BLOCKS, RG_BW, RG_BW), RG_BW ** -0.5),
        'rg_bi': nrm(ks[14], (L, 2, D_RNN), 0.02),
        'rg_lam': rg_lam,
        'rg_w_o': nrm(ks[16], (L, D_RNN, D), D_RNN ** -0.5),
        'dn_conv_w': nrm(ks[17], (L, 2, DN_CONV, DN_QKV), DN_CONV ** -0.5),
        'dn_a_log': dn_a_log,
        'dn_dt_bias': gain(ks[19], (L, 2, DN_HEADS)),
        'dn_norm_g': gain(ks[20], (L, DN_DV)),
        'dn_w_o': nrm(ks[21], (L, DN_VW, D), DN_VW ** -0.5),
        'w_out': nrm(ks[22], (L, D, D), D ** -0.5),
        'ffn_pre_g': gain(ks[23], (L, D)),
        'ffn_post_g': gain(ks[24], (L, D)),
        'router_w': nrm(ks[25], (L, D, N_EXPERTS), D ** -0.5),
        'router_b': nrm(ks[26], (L, N_EXPERTS), 0.01),
        'e_w1': nrm(ks[27], (L, N_EXPERTS, D, 2 * D_EXPERT), D ** -0.5),
        'e_b1': nrm(ks[28], (L, N_EXPERTS, 2 * D_EXPERT), 0.01),
        'e_w2': nrm(ks[29], (L, N_EXPERTS, D_EXPERT, D), D_EXPERT ** -0.5),
        'e_b2': nrm(ks[30], (L, N_EXPERTS, D), 0.01),
    }


def reference(x, c, ctx, c_ctx, ada_w, ada_b, mix_pre_g, mix_post_g, w_in, rg_conv_w, rg_conv_b, rg_wa, rg_ba,
              rg_wi, rg_bi, rg_lam, rg_w_o, dn_conv_w, dn_a_log, dn_dt_bias, dn_norm_g, dn_w_o, w_out,
              ffn_pre_g, ffn_post_g, router_w, router_b, e_w1, e_b1, e_w2, e_b2):
    B, S, D = x.shape
    Lc = ctx.shape[1]
    rows = S // GRID_W
    for l in range(DEPTH):
        last = l == DEPTH - 1
        mod_lat = jax.nn.silu(c) @ ada_w[l] + ada_b[l]
        mod_ctx = jax.nn.silu(c_ctx) @ ada_w[l] + ada_b[l]
        sh1, sc1, g1, sh2, sc2, g2 = jnp.split(mod_lat[:, None, :], 6, axis=-1)
        sh1c, sc1c, g1c, sh2c, sc2c, g2c = jnp.split(mod_ctx, 6)

        h_lat = modulate(rmsnorm(x, mix_pre_g[l]), sh1, sc1)
        h_ctx = modulate(rmsnorm(ctx, mix_pre_g[l]), sh1c, sc1c)
        y_lat, y_ctx = token_mixer(h_lat, h_ctx, rows, not last, w_in[l], rg_conv_w[l], rg_conv_b[l], rg_wa[l],
                                   rg_ba[l], rg_wi[l], rg_bi[l], rg_lam[l], rg_w_o[l], dn_conv_w[l], dn_a_log[l],
                                   dn_dt_bias[l], dn_norm_g[l], dn_w_o[l], w_out[l])
        x = x + g1 * rmsnorm(y_lat, mix_post_g[l])

        h_f = modulate(rmsnorm(x, ffn_pre_g[l]), sh2, sc2).reshape(B * S, D)
        if not last:
            ctx = ctx + g1c * rmsnorm(y_ctx, mix_post_g[l])
            h_fc = modulate(rmsnorm(ctx, ffn_pre_g[l]), sh2c, sc2c).reshape(B * Lc, D)
            f = moe(jnp.concatenate([h_f, h_fc], axis=0), router_w[l], router_b[l], e_w1[l], e_b1[l], e_w2[l], e_b2[l])
            f_lat = f[:B * S]
            ctx = ctx + g2c * rmsnorm(f[B * S:].reshape(B, Lc, D), ffn_post_g[l])
        else:
            f_lat = moe(h_f, router_w[l], router_b[l], e_w1[l], e_b1[l], e_w2[l], e_b2[l])
        x = x + g2 * rmsnorm(f_lat.reshape(B, S, D), ffn_post_g[l])
    return x
```

```python
from contextlib import ExitStack
import numpy as np
import concourse.bass as bass
import concourse.mybir as mybir
from concourse.bass_utils import run_bass_kernel_spmd

F32 = mybir.dt.float32
BF16 = mybir.dt.bfloat16
AF = mybir.ActivationFunctionType
ALU = mybir.AluOpType

D = 1024
S_LAT = 4096
S_CTX = 256
S_ALL = S_LAT + S_CTX
OWN = 2048
EPS = 1e-6
NE = 32
N_DMA_SEM = 32
SEM_GEN = 30000


class Sched:
    ENGS = ("pe", "act", "dve", "pool", "sp")

    def __init__(self, nc):
        self.nc = nc
        self.ops = {e: [] for e in self.ENGS}
        self.cnt = {e: 0 for e in self.ENGS}
        self.known = {e: {} for e in self.ENGS}
        self.last_w = {}
        self.readers = {}
        self.dma_n = 0
        self.dma_slot_target = [0] * N_DMA_SEM
        self.semids = set()

    def _add_wait(self, eng, waits, tok, raw):
        semid, val, teng = tok
        if teng == eng:
            if eng == "pe" or not raw:
                return
        if self.known[eng].get(semid, 0) >= val:
            return
        waits[semid] = max(waits.get(semid, 0), val)

    def op(self, eng, fn, reads=(), writes=(), dma=False):
        waits = {}
        for k in reads:
            t = self.last_w.get(k)
            if t is not None:
                self._add_wait(eng, waits, t, True)
            if (isinstance(k, str) and k.startswith("bank")) or (isinstance(k, tuple) and k[0] == "obank"):
                for t in self.readers.get(k, ()):
                    self._add_wait(eng, waits, t, False)
        for k in writes:
            t = self.last_w.get(k)
            if t is not None:
                self._add_wait(eng, waits, t, False)
            for t in self.readers.get(k, ()):
                self._add_wait(eng, waits, t, False)
        if dma:
            slot = self.dma_n % N_DMA_SEM
            self.dma_n += 1
            prev = self.dma_slot_target[slot]
            if prev > 0:
                self._add_wait(eng, waits, ("q%d" % slot, prev, None), True)
            val = prev + 16
            self.dma_slot_target[slot] = val
            tok = ("q%d" % slot, val, None)
        else:
            self.cnt[eng] += 1
            gen, val = divmod(self.cnt[eng] - 1, SEM_GEN)
            tok = ("%s.%d" % (eng, gen), val + 1, eng)
            if gen > 0 and val == 0:
                pass
        self.semids.add(tok[0])
        for semid, val in waits.items():
            self.known[eng][semid] = val
        self.ops[eng].append((waits, fn, tok))
        for k in reads:
            self.readers.setdefault(k, []).append(tok)
        for k in writes:
            self.last_w[k] = tok
            self.readers[k] = []
        return tok

    def wait_tokens(self, eng, toks):
        waits = {}
        for t in toks:
            self._add_wait(eng, waits, t, True)
        for semid, val in waits.items():
            self.known[eng][semid] = val
        self.ops[eng].append((waits, None, None))

    def barrier(self):
        toks = []
        for e in self.ENGS:
            if self.cnt[e] > 0:
                gen, val = divmod(self.cnt[e] - 1, SEM_GEN)
                toks.append(("%s.%d" % (e, gen), val + 1, None))
        for slot in range(N_DMA_SEM):
            if self.dma_slot_target[slot] > 0:
                toks.append(("q%d" % slot, self.dma_slot_target[slot], None))
        for e in self.ENGS:
            self.wait_tokens(e, toks)
        self.last_w = {}
        self.readers = {}

    def emit(self, stack):
        nc = self.nc
        sems = {}
        for sid in sorted(self.semids):
            sems[sid] = stack.enter_context(nc.semaphore("s_" + sid.replace(".", "_")))
        block = stack.enter_context(nc.Block())

        def run(ename):
            def body(eng):
                for waits, fn, tok in self.ops[ename]:
                    for semid, val in waits.items():
                        eng.wait_ge(sems[semid], val)
                    if fn is None:
                        continue
                    inst = fn(eng)
                    semid, val, _ = tok
                    inst.then_inc(sems[semid], 16 if semid.startswith("q") else 1)
            return body

        block.tensor(run("pe"))
        block.scalar(run("act"))
        block.vector(run("dve"))
        block.gpsimd(run("pool"))
        block.sync(run("sp"))


class Rot:
    def __init__(self, items):
        self.items = items
        self.i = 0

    def next(self):
        it = self.items[self.i % len(self.items)]
        self.i += 1
        return it


def build_program(debug=False, phases=("pa", "pb", "rg", "dn", "tail", "moe"), dn_heads=tuple(range(8)), moe_experts=NE, tail_stop=99):
    nc = bass.Bass("TRN2", target_bir_lowering=False)
    dbg_kind = "ExternalOutput" if debug else "Internal"

    def din(name, shape, dt=F32):
        return nc.dram_tensor(name, list(shape), dt, kind="ExternalInput").ap()

    def dscr(name, shape, dt):
        return nc.dram_tensor(name, list(shape), dt, kind=dbg_kind).ap()

    x = din("x", [S_LAT, D])
    ctx = din("ctx", [S_CTX, D])
    cc = din("cc", [128, 8, 2])
    ada_w = din("ada_w", [D, 6 * D])
    ada_bT = din("ada_bT", [128, 48])
    ada_b = din("ada_b", [1, 6 * D])
    gpreT = din("gpreT", [128, 8])
    w_in = din("w_in", [D, 8224])
    rgp = din("rgp", [128, 8, 2, 8])
    rg_wa = din("rg_wa", [2, 16, 64, 64])
    rg_wi = din("rg_wi", [2, 16, 64, 64])
    dncw = din("dncw", [128, 24, 2, 4])
    dnc = din("dnc", [1, 32])
    dn_norm_g = din("dn_norm_g", [128, 1])
    gpost = din("gpost", [1, D])
    gfpre = din("gfpre", [1, D])
    gfpost = din("gfpost", [1, D])
    rg_w_o = din("rg_w_o", [D, D])
    dn_w_o = din("dn_w_o", [D, D])
    w_out = din("w_out", [D, D])
    router_w = din("router_w", [D, NE])
    router_b = din("router_b", [1, NE])
    e_w1 = din("e_w1", [NE, D, 2 * D])
    e_b1T = din("e_b1T", [128, NE, 16])
    e_w2 = din("e_w2", [NE, D, D])
    e_b2 = din("e_b2", [NE, D])
    out = nc.dram_tensor("out", [OWN, D], F32, kind="ExternalOutput").ap()

    PRG = dscr("PRG", [D, S_ALL], BF16)
    PQKV = dscr("PQKV", [3 * D, S_ALL], BF16)
    PAB = dscr("PAB", [S_ALL, 32], F32)
    GATES = dscr("GATES", [4 * D, OWN], BF16)
    XRG = dscr("XRG", [D, OWN], BF16)
    XDN = dscr("XDN", [D, OWN], BF16)
    XNEW = dscr("XNEW", [OWN, D], F32)

    with ExitStack() as st:
        E = st.enter_context
        S = Sched(nc)

        ARENA_WORDS = 52000
        arena_t = E(nc.sbuf_tensor("arena", [128, ARENA_WORDS], F32))
        arena = {"off": 0}

        def sb(name, shape, dt=F32):
            shape = list(shape)
            elems = int(np.prod(shape[1:]))
            words = elems if dt == F32 else (elems + 1) // 2
            words += words % 2
            off = arena["off"]
            assert off + words <= ARENA_WORDS, ("SBUF arena overflow", name, off, words)
            arena["off"] = off + words
            ap = arena_t[0:shape[0], off:off + words]
            if dt != F32:
                ap = ap.bitcast(dt)[:, 0:elems]
            else:
                ap = ap[:, 0:elems]
            if len(shape) == 3:
                ap = ap.rearrange("p (a b) -> p a b", a=shape[1])
            elif len(shape) == 4:
                ap = ap.rearrange("p (a b c) -> p a b c", a=shape[1], b=shape[2])
            return ap

        def ps(name):
            return E(nc.psum_tensor(name, [128, 512], F32))

        banks = [ps("bank%d" % i) for i in range(8)]

        def dbg(name, ap, reads):
            if not debug:
                return
            t = nc.dram_tensor("dbg_" + name, list(ap.shape), ap.dtype, kind="ExternalOutput").ap()
            S.op("sp", lambda e: e.dma_start(out=t, in_=ap), reads=reads, dma=True)

        ident = sb("ident", [128, 128])
        S.op("pool", lambda e: e.memset(ident[:], 0.0), writes=["ident"])
        S.op("pool", lambda e: e.affine_select(out=ident[:], in_=ident[:], pattern=[[-1, 128]],
                                               compare_op=ALU.not_equal, fill=1.0, base=0,
                                               channel_multiplier=1),
             reads=["ident"], writes=["ident"])

        cs = sb("cs", [128, 8, 2])
        modF = sb("modF", [128, 16, 2])
        Gm = sb("Gm", [128, 8, 2])
        Sh = sb("Sh", [128, 8, 2])
        adabT = sb("adabT", [128, 48])
        gpre = sb("gpre", [128, 8])
        S.op("sp", lambda e: e.dma_start(out=cs[:], in_=cc), writes=["cs"], dma=True)
        S.op("sp", lambda e: e.dma_start(out=adabT[:], in_=ada_bT), writes=["adabT"], dma=True)
        S.op("sp", lambda e: e.dma_start(out=gpre[:], in_=gpreT), writes=["gpre"], dma=True)
        S.op("act", lambda e: e.activation(out=cs[:], in_=cs[:], func=AF.Silu), reads=["cs"], writes=["cs"])
        mark0 = arena["off"]
        if True:
            adaw0 = sb("adaw0", [128, 8, 2048])
            for kc in range(8):
                S.op("sp", lambda e, kc=kc: e.dma_start(out=adaw0[:, kc, :], in_=ada_w[kc * 128:(kc + 1) * 128, 0:2048]),
                     writes=[("adaw0", kc)], dma=True)
            pm = banks[0]
            for j in range(16):
                for kc in range(8):
                    S.op("pe", lambda e, j=j, kc=kc: e.matmul(pm[:, 2 * j:2 * j + 2], lhsT=adaw0[:, kc, j * 128:(j + 1) * 128],
                                                              rhs=cs[:, kc, :], start=(kc == 0), stop=(kc == 7)),
                         reads=[("adaw0", kc), "cs"], writes=["bank0"])
            S.op("dve", lambda e: e.tensor_tensor(out=modF[:], in0=pm[:, 0:32].rearrange("p (j t) -> p j t", t=2),
                                                  in1=adabT[:, 0:16].unsqueeze(2).to_broadcast([128, 16, 2]), op=ALU.add),
                 reads=["bank0", "adabT"], writes=["modF"])
            S.op("dve", lambda e: e.tensor_scalar(out=Gm[:], in0=modF[:, 8:16, :], scalar1=1.0, scalar2=None, op0=ALU.add),
                 reads=["modF"], writes=["Gm"])
            S.op("dve", lambda e: e.tensor_tensor(out=Gm[:], in0=Gm[:], in1=gpre[:].unsqueeze(2).to_broadcast([128, 8, 2]), op=ALU.mult),
                 reads=["Gm", "gpre"], writes=["Gm"])
            S.op("dve", lambda e: e.tensor_copy(out=Sh[:], in_=modF[:, 0:8, :]), reads=["modF"], writes=["Sh"])
            S.barrier()
            arena["off"] = mark0

        def proj_pass(pname, Wb, wkey, blocks, chunk_fn, ab_cols=None):
            xts = Rot([(sb("%s_xt%d" % (pname, i), [128, D]), "%s_xt%d" % (pname, i)) for i in range(4)])
            xrs = [[(sb("%s_xr%d_%d" % (pname, b, i), [128, D]), "%s_xr%d_%d" % (pname, b, i)) for i in range(4)] for b in range(2)]
            hTs = [(sb("%s_hT%d" % (pname, i), [128, 8, 512], BF16), "%s_hT%d" % (pname, i)) for i in range(2)]
            junk = sb(pname + "_junk", [128, D], BF16)
            ssq = [(sb("%s_ss%d" % (pname, i), [128, 4]), "%s_ss%d" % (pname, i)) for i in range(2)]
            stg = Rot([(sb("%s_stg%d" % (pname, i), [128, 512], BF16), "%s_stg%d" % (pname, i)) for i in range(6)])
            tmp = Rot([(sb("%s_tmp%d" % (pname, i), [128, 512]), "%s_tmp%d" % (pname, i)) for i in range(4)])
            tb = Rot([(banks[0], "bank0"), (banks[1], "bank1")])
            pb = Rot([(banks[2 + i], "bank%d" % (2 + i)) for i in range(4)])
            abb = Rot([(banks[6], "bank6"), (banks[7], "bank7")])
            abst = Rot([(sb("%s_abst%d" % (pname, i), [128, 4, 32]), "%s_abst%d" % (pname, i)) for i in range(2)])
            evq = Rot(["act", "dve"])

            def stage1(bi):
                blk = blocks[bi]
                nt = blk["ntok"] // 128
                ss, ssk = ssq[bi % 2]
                xtl = []
                for j in range(nt):
                    xt, xk = xts.next()
                    for (ap, p0, npart) in blk["loads"][j]:
                        S.op("sp", lambda e, xt=xt, ap=ap, p0=p0, npart=npart: e.dma_start(out=xt[p0:p0 + npart, :], in_=ap),
                             writes=[xk], dma=True)
                    S.op("act", lambda e, xt=xt, ss=ss, j=j: e.activation(out=junk[:], in_=xt[:], func=AF.Square, accum_out=ss[:, j:j + 1]),
                         reads=[xk], writes=[pname + "_junk", ssk])
                    xtl.append((xt, xk))
                S.op("dve", lambda e, ss=ss, nt=nt: e.tensor_scalar(out=ss[:, 0:nt], in0=ss[:, 0:nt], scalar1=1.0 / D, scalar2=EPS,
                                                                   op0=ALU.mult, op1=ALU.add), reads=[ssk], writes=[ssk])
                S.op("act", lambda e, ss=ss, nt=nt: e.activation(out=ss[:, 0:nt], in_=ss[:, 0:nt], func=AF.Sqrt), reads=[ssk], writes=[ssk])
                S.op("dve", lambda e, ss=ss, nt=nt: e.reciprocal(out=ss[:, 0:nt], in_=ss[:, 0:nt]), reads=[ssk], writes=[ssk])
                xrl = xrs[bi % 2]
                for j in range(nt):
                    xt, xk = xtl[j]
                    xr, xrk = xrl[j]
                    S.op("pool", lambda e, xr=xr, xt=xt, ss=ss, j=j: e.tensor_scalar(out=xr[:], in0=xt[:], scalar1=ss[:, j:j + 1], scalar2=0.0,
                                                                                    op0=ALU.mult, op1=ALU.add), reads=[xk, ssk], writes=[xrk])
                hT, hk = hTs[bi % 2]
                m = blk["mod"]
                for c in range(8):
                    bk, bkk = tb.next()
                    for j in range(nt):
                        xr, xrk = xrl[j]
                        S.op("pe", lambda e, bk=bk, xr=xr, c=c, j=j: e.transpose(out=bk[:, j * 128:(j + 1) * 128], in_=xr[:, c * 128:(c + 1) * 128],
                                                                                 identity=ident[:]), reads=[xrk, "ident"], writes=[bkk])
                    S.op("act", lambda e, bk=bk, hT=hT, c=c, nt=nt, m=m: e.activation(out=hT[:, c, 0:nt * 128], in_=bk[:, 0:nt * 128], func=AF.Identity,
                                                                                   scale=Gm[:, c, m:m + 1], bias=Sh[:, c, m:m + 1]),
                         reads=[bkk, "Gm", "Sh"], writes=[(hk, c)])

            def stage2(bi):
                blk = blocks[bi]
                ntok = blk["ntok"]
                hT, hk = hTs[bi % 2]
                for spec in chunk_fn(blk):
                    col0, kind, dst = spec
                    bk, bkk = pb.next()
                    for kc in range(8):
                        S.op("pe", lambda e, bk=bk, kc=kc, col0=col0: e.matmul(bk[:, 0:ntok], lhsT=Wb[:, kc, col0:col0 + 128], rhs=hT[:, kc, 0:ntok],
                                                                               start=(kc == 0), stop=(kc == 7)),
                             reads=[(wkey, kc), (hk, kc)], writes=[bkk])
                    sg, sgk = stg.next()
                    if kind == "copy":
                        q = evq.next()
                        if q == "act":
                            S.op("act", lambda e, sg=sg, bk=bk: e.activation(out=sg[:, 0:ntok], in_=bk[:, 0:ntok], func=AF.Identity),
                                 reads=[bkk], writes=[sgk])
                        else:
                            S.op("dve", lambda e, sg=sg, bk=bk: e.tensor_copy(out=sg[:, 0:ntok], in_=bk[:, 0:ntok]), reads=[bkk], writes=[sgk])
                    elif kind == "silu":
                        S.op("act", lambda e, sg=sg, bk=bk: e.activation(out=sg[:, 0:ntok], in_=bk[:, 0:ntok], func=AF.Silu), reads=[bkk], writes=[sgk])
                    elif kind == "sigmoid":
                        S.op("act", lambda e, sg=sg, bk=bk: e.activation(out=sg[:, 0:ntok], in_=bk[:, 0:ntok], func=AF.Sigmoid), reads=[bkk], writes=[sgk])
                    elif kind == "gelu":
                        t1, t1k = tmp.next()
                        S.op("act", lambda e, t1=t1, bk=bk: e.activation(out=t1[:, 0:ntok], in_=bk[:, 0:ntok], func=AF.Square), reads=[bkk], writes=[t1k])
                        S.op("dve", lambda e, t1=t1: e.tensor_scalar(out=t1[:, 0:ntok], in0=t1[:, 0:ntok], scalar1=0.044715, scalar2=1.0,
                                                                    op0=ALU.mult, op1=ALU.add), reads=[t1k], writes=[t1k])
                        S.op("dve", lambda e, t1=t1, bk=bk: e.tensor_tensor(out=t1[:, 0:ntok], in0=t1[:, 0:ntok], in1=bk[:, 0:ntok], op=ALU.mult),
                             reads=[t1k, bkk], writes=[t1k])
                        S.op("act", lambda e, t1=t1: e.activation(out=t1[:, 0:ntok], in_=t1[:, 0:ntok], func=AF.Sigmoid, scale=1.5957691216057308),
                             reads=[t1k], writes=[t1k])
                        S.op("dve", lambda e, t1=t1, bk=bk, sg=sg: e.tensor_tensor(out=sg[:, 0:ntok], in0=t1[:, 0:ntok], in1=bk[:, 0:ntok], op=ALU.mult),
                             reads=[t1k, bkk], writes=[sgk])
                    S.op("sp", lambda e, sg=sg, dst=dst: e.dma_start(out=dst, in_=sg[:, 0:ntok]), reads=[sgk], dma=True)
                if ab_cols is not None:
                    nt = ntok // 128
                    bk, bkk = abb.next()
                    for j in range(nt):
                        for kc in range(8):
                            S.op("pe", lambda e, bk=bk, kc=kc, j=j: e.matmul(bk[:, j * 32:(j + 1) * 32], lhsT=hT[:, kc, j * 128:(j + 1) * 128],
                                                                             rhs=Wb[:, kc, ab_cols:ab_cols + 32], start=(kc == 0), stop=(kc == 7)),
                                 reads=[(wkey, kc), (hk, kc)], writes=[bkk])
                    ab, abk = abst.next()
                    S.op("dve", lambda e, ab=ab, bk=bk, nt=nt: e.tensor_copy(out=ab[:, 0:nt, :], in_=bk[:, 0:nt * 32].rearrange("p (j c) -> p j c", c=32)),
                         reads=[bkk], writes=[abk])
                    t0 = blk["tokoff"]
                    S.op("sp", lambda e, ab=ab, nt=nt, t0=t0: e.dma_start(out=PAB[t0:t0 + nt * 128, :].rearrange("(j p) c -> p j c", p=128),
                                                                         in_=ab[:, 0:nt, :]), reads=[abk], dma=True)

            stage1(0)
            for bi in range(len(blocks)):
                if bi + 1 < len(blocks):
                    stage1(bi + 1)
                stage2(bi)

        WA = sb("WA", [128, 8, 5120], BF16)
        srcA = [(0, 0, 1024), (1024, 1024, 1024), (2048, 5120, 1024), (3072, 6176, 1024), (4096, 7200, 1024)]
        for (d0, s0, n) in srcA:
            for kc in range(8):
                S.op("pool", lambda e, kc=kc, d0=d0, s0=s0, n=n: e.dma_start(out=WA[:, kc, d0:d0 + n], in_=w_in[kc * 128:(kc + 1) * 128, s0:s0 + n]),
                     writes=[("WA", kc)], dma=True)
        blocksA = [dict(ntok=256, mod=1, tokoff=0, own=False,
                        loads=[[(ctx[j * 128:(j + 1) * 128, :], 0, 128)] for j in range(2)])]
        for bi in range(8):
            blocksA.append(dict(ntok=512, mod=0, tokoff=S_CTX + bi * 512, own=(bi < 4), lat0=bi * 512,
                                loads=[[(x[bi * 512 + j * 128: bi * 512 + (j + 1) * 128, :], 0, 128)] for j in range(4)]))

        def chunksA(blk):
            ntok, t0 = blk["ntok"], blk["tokoff"]
            specs = [(c * 128, "copy", PRG[c * 128:(c + 1) * 128, t0:t0 + ntok]) for c in range(8)]
            if blk["own"]:
                l0 = blk["lat0"]
                for gi, kind in enumerate(["gelu", "silu", "sigmoid", "sigmoid"]):
                    for c in range(8):
                        specs.append((1024 + gi * 1024 + c * 128, kind, GATES[gi * 1024 + c * 128: gi * 1024 + (c + 1) * 128, l0:l0 + ntok]))
            return specs

        if "pa" in phases:
            proj_pass("pa", WA, "WA", blocksA, chunksA)
        S.barrier()
        arena["off"] = mark0

        WB = sb("WB", [128, 8, 3104], BF16)
        srcB = [(0, 2048, 1024), (1024, 3072, 1024), (2048, 4096, 1024), (3072, 6144, 32)]
        for (d0, s0, n) in srcB:
            for kc in range(8):
                S.op("pool", lambda e, kc=kc, d0=d0, s0=s0, n=n: e.dma_start(out=WB[:, kc, d0:d0 + n], in_=w_in[kc * 128:(kc + 1) * 128, s0:s0 + n]),
                     writes=[("WB", kc)], dma=True)
        xcm = x.rearrange("(r c) d -> c r d", c=64)
        blocksB = [dict(ntok=256, mod=1, tokoff=0,
                        loads=[[(ctx[j * 128:(j + 1) * 128, :], 0, 128)] for j in range(2)])]
        for bi in range(8):
            blocksB.append(dict(ntok=512, mod=0, tokoff=S_CTX + bi * 512,
                                loads=[[(xcm[bi * 8 + j * 2 + h], h * 64, 64) for h in range(2)] for j in range(4)]))

        def chunksB(blk):
            ntok, t0 = blk["ntok"], blk["tokoff"]
            return [(c * 128, "copy", PQKV[c * 128:(c + 1) * 128, t0:t0 + ntok]) for c in range(24)]

        if "pb" in phases:
            proj_pass("pb", WB, "WB", blocksB, chunksB, ab_cols=3072)

        S.barrier()
        arena["off"] = mark0

        if "rg" in phases:
            XW = 4 + S_CTX + 4 + S_LAT + 4
            rgp_t = sb("rgp_t", [128, 8, 2, 8])
            S.op("sp", lambda e: e.dma_start(out=rgp_t[:], in_=rgp), writes=["rgp"], dma=True)
            c8 = sb("c8", [128, 8, 2])
            S.op("act", lambda e: e.activation(out=c8[:], in_=rgp_t[:, :, :, 7], func=AF.Exp, scale=-1.0), reads=["rgp"], writes=["c8"])
            S.op("act", lambda e: e.activation(out=c8[:], in_=c8[:], func=AF.Ln, bias=1.0), reads=["c8"], writes=["c8"])
            S.op("dve", lambda e: e.tensor_scalar(out=c8[:], in0=c8[:], scalar1=-8.0, scalar2=None, op0=ALU.mult), reads=["c8"], writes=["c8"])
            NSET = 2
            sets = []
            for si in range(NSET):
                sets.append(dict(
                    xc=sb("rg_xc%d" % si, [128, S_ALL]), ra=sb("rg_ra%d" % si, [128, S_ALL]),
                    ib=sb("rg_ib%d" % si, [128, S_ALL]), mh=sb("rg_mh%d" % si, [128, S_ALL]),
                    wa=sb("rg_wa%d" % si, [128, 128]), wi=sb("rg_wi%d" % si, [128, 128]), k="rgs%d" % si))
            xps = [(sb("rg_xp%d" % i, [128, XW], BF16), "rg_xp%d" % i) for i in range(2)]
            gts = [(sb("rg_gt%d" % i, [128, OWN], BF16), "rg_gt%d" % i) for i in range(2)]
            accs = [(sb("rg_acc%d" % i, [128, OWN]), "rg_acc%d" % i) for i in range(2)]
            xos = [(sb("rg_xo%d" % i, [128, OWN], BF16), "rg_xo%d" % i) for i in range(2)]
            pbk = Rot([(banks[i], "bank%d" % i) for i in range(8)])
            def rg_one(g, d, st_, xp, xpk, gt, gtk, acc, acck, xo, xok):
                sk = st_["k"]
                xc, ra, ib, mh, wa_t, wi_t = st_["xc"], st_["ra"], st_["ib"], st_["mh"], st_["wa"], st_["wi"]
                prm = rgp_t[:, g, d, :]
                for wt, src, wk in ((wa_t, rg_wa, sk + "wa"), (wi_t, rg_wi, sk + "wi")):
                    S.op("pool", lambda e, wt=wt: e.memset(wt[:], 0.0), writes=[wk])
                    for h in range(2):
                        S.op("sp", lambda e, wt=wt, src=src, h=h, g=g, d=d: e.dma_start(out=wt[h * 64:(h + 1) * 64, h * 64:(h + 1) * 64],
                                                                                   in_=src[d, 2 * g + h]), writes=[wk], dma=True)
                V = xp if d == 0 else xp[:, ::-1]
                ctx_off, lat_off = (4, 264) if d == 0 else (4104, 4)
                n_lat = OWN if d == 0 else S_LAT
                segs = [(ctx_off, 0, S_CTX)] + [(lat_off + i * 512, S_CTX + i * 512, 512) for i in range(n_lat // 512)]
                ntot = S_CTX + n_lat
                for (vo, o, n) in segs:
                    kx = (sk + "xc", o)
                    S.op("dve", lambda e, vo=vo, o=o, n=n: e.tensor_scalar(out=xc[:, o:o + n], in0=V[:, vo:vo + n], scalar1=prm[:, 3:4], scalar2=prm[:, 4:5],
                                                                          op0=ALU.mult, op1=ALU.add), reads=[xpk, "rgp"], writes=[kx])
                    for j in (2, 1, 0):
                        sh = 3 - j
                        S.op("dve", lambda e, vo=vo, o=o, n=n, j=j, sh=sh: e.scalar_tensor_tensor(out=xc[:, o:o + n], in0=V[:, vo - sh:vo - sh + n], scalar=prm[:, j:j + 1],
                                                                                                  in1=xc[:, o:o + n], op0=ALU.mult, op1=ALU.add),
                             reads=[xpk, "rgp", kx], writes=[kx])
                    b1, b1k = pbk.next()
                    b2, b2k = pbk.next()
                    S.op("pe", lambda e, b1=b1, o=o, n=n: e.matmul(b1[:, 0:n], lhsT=wa_t[:], rhs=xc[:, o:o + n], start=True, stop=True),
                         reads=[sk + "wa", kx], writes=[b1k])
                    S.op("pe", lambda e, b2=b2, o=o, n=n: e.matmul(b2[:, 0:n], lhsT=wi_t[:], rhs=xc[:, o:o + n], start=True, stop=True),
                         reads=[sk + "wi", kx], writes=[b2k])
                    S.op("act", lambda e, b1=b1, o=o, n=n: e.activation(out=ra[:, o:o + n], in_=b1[:, 0:n], func=AF.Sigmoid, bias=prm[:, 5:6]),
                         reads=[b1k, "rgp"], writes=[(sk + "ra", o)])
                    S.op("act", lambda e, b2=b2, o=o, n=n: e.activation(out=ib[:, o:o + n], in_=b2[:, 0:n], func=AF.Sigmoid, bias=prm[:, 6:7]),
                         reads=[b2k, "rgp"], writes=[(sk + "ib", o)])
                allr = [(sk + "ra", o) for (_, o, _) in segs]
                alli = [(sk + "ib", o) for (_, o, _) in segs]
                allx = [(sk + "xc", o) for (_, o, _) in segs]
                allm = [sk + "mh"]
                S.op("act", lambda e, g=g, d=d: e.activation(out=ra[:, 0:ntot], in_=ra[:, 0:ntot], func=AF.Exp, scale=c8[:, g, d:d + 1]),
                     reads=allr + ["c8"], writes=allr)
                S.op("pool", lambda e: e.tensor_tensor(out=mh[:, 0:ntot], in0=ra[:, 0:ntot], in1=ra[:, 0:ntot], op=ALU.mult),
                     reads=allr, writes=allm)
                S.op("act", lambda e: e.activation(out=mh[:, 0:ntot], in_=mh[:, 0:ntot], func=AF.Sqrt, scale=-1.0, bias=1.0),
                     reads=allm, writes=allm)
                S.op("pool", lambda e: e.memset(mh[:, 0:1], 1.0), reads=allm, writes=allm)
                S.op("pool", lambda e: e.tensor_tensor(out=ib[:, 0:ntot], in0=ib[:, 0:ntot], in1=mh[:, 0:ntot], op=ALU.mult),
                     reads=alli + allm, writes=alli)
                S.op("pool", lambda e: e.tensor_tensor(out=ib[:, 0:ntot], in0=ib[:, 0:ntot], in1=xc[:, 0:ntot], op=ALU.mult),
                     reads=alli + allx, writes=alli)
                S.op("dve", lambda e: e.tensor_tensor_scan(out=mh[:, 0:S_CTX], data0=ra[:, 0:S_CTX], data1=ib[:, 0:S_CTX], initial=0.0,
                                                           op0=ALU.mult, op1=ALU.add), reads=allr + alli + allm, writes=allm)
                for o in range(S_CTX, ntot, 1024):
                    S.op("dve", lambda e, o=o: e.tensor_tensor_scan(out=mh[:, o:o + 1024], data0=ra[:, o:o + 1024], data1=ib[:, o:o + 1024],
                                                                    initial=mh[:, o - 1:o], op0=ALU.mult, op1=ALU.add),
                         reads=allr + alli + allm, writes=allm)
                if g == 0:
                    if d == 0:
                        dbg("xp", xp[:, :], [xpk])
                    dbg("xc%d" % d, xc[:, 0:ntot], allx)
                    dbg("a%d" % d, ra[:, 0:ntot], allr)
                    dbg("b%d" % d, ib[:, 0:ntot], alli)
                    dbg("h%d" % d, mh[:, 0:ntot], allm)
                    dbg("c8_%d" % d, c8[:, 0, :], ["c8"])
                if d == 0:
                    S.op("pool", lambda e, acc=acc: e.tensor_copy(out=acc[:], in_=mh[:, S_CTX:S_CTX + OWN]), reads=allm, writes=[acck])
                else:
                    S.op("pool", lambda e, acc=acc: e.tensor_tensor(out=acc[:], in0=acc[:], in1=mh[:, S_CTX + OWN:S_ALL][:, ::-1], op=ALU.add),
                         reads=allm + [acck], writes=[acck])
                    S.op("pool", lambda e, acc=acc, gt=gt, xo=xo: e.tensor_tensor(out=xo[:], in0=acc[:], in1=gt[:], op=ALU.mult),
                         reads=[acck, gtk], writes=[xok])
                    S.op("sp", lambda e, xo=xo, g=g: e.dma_start(out=XRG[g * 128:(g + 1) * 128, :], in_=xo[:]), reads=[xok], dma=True)

            seti = 0
            for g in range(8):
                xp, xpk = xps[g % 2]
                gt, gtk = gts[g % 2]
                acc, acck = accs[g % 2]
                xo, xok = xos[g % 2]
                S.op("pool", lambda e, xp=xp: e.memset(xp[:, 0:4], 0.0), writes=[xpk])
                S.op("pool", lambda e, xp=xp: e.memset(xp[:, 260:264], 0.0), writes=[xpk])
                S.op("pool", lambda e, xp=xp: e.memset(xp[:, XW - 4:XW], 0.0), writes=[xpk])
                S.op("sp", lambda e, xp=xp, g=g: e.dma_start(out=xp[:, 4:260], in_=PRG[g * 128:(g + 1) * 128, 0:S_CTX]), writes=[xpk], dma=True)
                S.op("sp", lambda e, xp=xp, g=g: e.dma_start(out=xp[:, 264:264 + S_LAT], in_=PRG[g * 128:(g + 1) * 128, S_CTX:S_ALL]), writes=[xpk], dma=True)
                S.op("sp", lambda e, gt=gt, g=g: e.dma_start(out=gt[:], in_=GATES[g * 128:(g + 1) * 128, :]), writes=[gtk], dma=True)
                for d in range(2):
                    rg_one(g, d, sets[seti % NSET], xp, xpk, gt, gtk, acc, acck, xo, xok)
                    seti += 1
            S.barrier()
            arena["off"] = mark0

        def OP(eng, method, reads, writes, **kw):
            S.op(eng, lambda e: getattr(e, method)(**kw), reads=reads, writes=writes)

        def DMA(outap, inap, reads=(), writes=()):
            S.op("sp", lambda e: e.dma_start(out=outap, in_=inap), reads=reads, writes=writes, dma=True)

        class TP:
            def __init__(self, name, n, shape, dt):
                self.t = [(sb("%s%d" % (name, i), shape, dt), "%s%d" % (name, i)) for i in range(n)]
                self.i = 0

            def get(self):
                r = self.t[self.i % len(self.t)]
                self.i += 1
                return r

        if "dn" in phases:
            NEG = -30000.0
            XW = 4 + S_CTX + 4 + S_LAT + 4
            onesb = sb("onesb", [128, 128], BF16)
            onesf = sb("onesf", [128, 128])
            identb = sb("identb", [128, 128], BF16)
            LOW = sb("LOW", [128, 128])
            UPI = sb("UPI", [128, 128])
            NEGL = sb("NEGL", [128, 128])
            NEGU = sb("NEGU", [128, 128])
            H0 = sb("H0", [128, 128])
            H1 = sb("H1", [128, 128])
            OP("pool", "memset", [], ["onesb"], ap=onesb[:], constant=1.0)
            OP("pool", "memset", [], ["onesf"], ap=onesf[:], constant=1.0)
            OP("pool", "tensor_copy", ["ident"], ["identb"], out=identb[:], in_=ident[:])
            OP("pool", "affine_select", ["onesf"], ["LOW"], out=LOW[:], in_=onesf[:], pattern=[[-1, 128]], compare_op=ALU.is_gt, fill=0.0,
               base=0, channel_multiplier=1)
            OP("pool", "memset", ["LOW"], ["LOW"], ap=LOW[64:128, 0:64], constant=0.0)
            OP("pool", "affine_select", ["onesf"], ["UPI"], out=UPI[:], in_=onesf[:], pattern=[[1, 128]], compare_op=ALU.is_ge, fill=0.0,
               base=0, channel_multiplier=-1)
            OP("pool", "memset", ["UPI"], ["UPI"], ap=UPI[0:64, 64:128], constant=0.0)
            OP("dve", "tensor_scalar", ["LOW"], ["NEGL"], out=NEGL[:], in0=LOW[:], scalar1=-1.0, scalar2=-NEG, op0=ALU.add, op1=ALU.mult)
            OP("dve", "tensor_scalar", ["UPI"], ["NEGU"], out=NEGU[:], in0=UPI[:], scalar1=-1.0, scalar2=-NEG, op0=ALU.add, op1=ALU.mult)
            OP("pool", "memset", [], ["H0"], ap=H0[:], constant=0.0)
            OP("pool", "memset", ["H0"], ["H0"], ap=H0[0:64, :], constant=1.0)
            OP("pool", "memset", [], ["H1"], ap=H1[:], constant=0.0)
            OP("pool", "memset", ["H1"], ["H1"], ap=H1[64:128, :], constant=1.0)
            dncw_t = sb("dncw_t", [128, 24, 2, 4])
            dnc_t = sb("dnc_t", [128, 32])
            gnorm = sb("gnorm", [128, 1])
            negA = sb("negA", [128, 16])
            DMA(dncw_t[:], dncw, writes=["dncw"])
            DMA(dnc_t[:], dnc.partition_broadcast(128), writes=["dnc"])
            DMA(gnorm[:], dn_norm_g, writes=["gnorm"])
            OP("act", "activation", ["dnc"], ["negA"], out=negA[:], in_=dnc_t[:, 0:16], func=AF.Exp)
            OP("dve", "tensor_scalar", ["negA"], ["negA"], out=negA[:], in0=negA[:], scalar1=-1.0, scalar2=None, op0=ALU.mult)

            NP = S_ALL // 128
            GCt, Btt, EGLt, EGRt, BEGt = [], [], [], [], []
            with_tmp = arena["off"]
            ABt = sb("ABt", [128, NP, 32])
            ABr = sb("ABr", [128, NP, 32])
            Jm = sb("Jm", [128, 128])
            OP("pool", "memset", [], ["Jm"], ap=Jm[:], constant=0.0)
            OP("pool", "affine_select", ["Jm"], ["Jm"], out=Jm[:], in_=Jm[:], pattern=[[1, 128]], compare_op=ALU.not_equal, fill=1.0,
               base=-127, channel_multiplier=1)
            Zt = sb("Zt", [128, NP, 8])
            Lt = sb("Lt", [128, NP, 8])
            Gt = sb("Gt", [128, NP, 8])
            GLtok = sb("GLtok", [128, NP, 8])
            for d in range(2):
                GC = sb("GC%d" % d, [128, NP, 8]); Bt = sb("Bt%d" % d, [128, NP, 8]); EGL = sb("EGL%d" % d, [128, NP, 2, 8])
                EGR = sb("EGR%d" % d, [128, NP, 8]); BEG = sb("BEG%d" % d, [128, NP, 8])
                GCt.append(GC); Btt.append(Bt); EGLt.append(EGL); EGRt.append(EGR); BEGt.append(BEG)
            mark_dn = arena["off"]
            for d in range(2):
                GC, Bt, EGL, EGR, BEG = GCt[d], Btt[d], EGLt[d], EGRt[d], BEGt[d]
                k = "sc%d" % d
                if d == 0:
                    DMA(ABt[:, 0:2, :], PAB[0:S_CTX, :].rearrange("(n p) c -> p n c", p=128), writes=["ABt"])
                    DMA(ABt[:, 2:NP, :], PAB[S_CTX:S_ALL, :].rearrange("(n p) c -> p n c", p=128), writes=["ABt"])
                    ABs, ABk = ABt, "ABt"
                else:
                    ABf = ABt[:].rearrange("p n c -> p (n c)")
                    for bi_, (s0, s1) in enumerate(((0, 16), (16, 32), (32, 34))):
                        OP("pe", "matmul", ["Jm", "ABt"], ["bank%d" % (3 + bi_)], out=banks[3 + bi_][:, 0:(s1 - s0) * 32], lhsT=Jm[:], rhs=ABf[:, s0 * 32:s1 * 32],
                           start=True, stop=True)
                    b3v = banks[3][:, 0:512].rearrange("p (n c) -> p n c", c=32)
                    b4v = banks[4][:, 0:512].rearrange("p (n c) -> p n c", c=32)
                    b5v = banks[5][:, 0:64].rearrange("p (n c) -> p n c", c=32)
                    OP("dve", "tensor_copy", ["bank3"], ["ABr"], out=ABr[:, 0:2, :], in_=b3v[:, 0:2, :][:, ::-1, :])
                    OP("dve", "tensor_copy", ["bank3"], ["ABr"], out=ABr[:, 20:34, :], in_=b3v[:, 2:16, :][:, ::-1, :])
                    OP("dve", "tensor_copy", ["bank4"], ["ABr"], out=ABr[:, 4:20, :], in_=b4v[:, 0:16, :][:, ::-1, :])
                    OP("dve", "tensor_copy", ["bank5"], ["ABr"], out=ABr[:, 2:4, :], in_=b5v[:, 0:2, :][:, ::-1, :])
                    ABs, ABk = ABr, "ABr"
                dtb = dnc_t[:, 16 + d * 8:24 + d * 8].unsqueeze(1).to_broadcast([128, NP, 8])
                nab = negA[:, d * 8:(d + 1) * 8].unsqueeze(1).to_broadcast([128, NP, 8])
                OP("dve", "tensor_tensor", [ABk, "dnc"], ["Zt"], out=Zt[:], in0=ABs[:, :, d * 8:(d + 1) * 8], in1=dtb, op=ALU.add)
                OP("dve", "scalar_tensor_tensor", ["Zt"], ["Lt"], out=Lt[:].rearrange("p n c -> p (n c)"), in0=Zt[:].rearrange("p n c -> p (n c)"), scalar=-1.0,
                   in1=Zt[:].rearrange("p n c -> p (n c)"), op0=ALU.mult, op1=ALU.max)
                OP("act", "activation", ["Lt"], ["Lt"], out=Lt[:], in_=Lt[:], func=AF.Exp, scale=-1.0)
                OP("act", "activation", ["Lt"], ["Lt"], out=Lt[:], in_=Lt[:], func=AF.Ln, bias=1.0)
                OP("dve", "scalar_tensor_tensor", ["Zt", "Lt"], ["Gt"], out=Gt[:].rearrange("p n c -> p (n c)"), in0=Zt[:].rearrange("p n c -> p (n c)"),
                   scalar=0.0, in1=Lt[:].rearrange("p n c -> p (n c)"), op0=ALU.max, op1=ALU.add)
                OP("dve", "tensor_tensor", ["Gt", "negA"], ["Gt"], out=Gt[:], in0=Gt[:], in1=nab, op=ALU.mult)
                OP("act", "activation", [ABk], [k + "B"], out=Bt[:], in_=ABs[:, :, 16 + d * 8:24 + d * 8], func=AF.Sigmoid)
                Gf = Gt[:].rearrange("p n c -> p (n c)")
                W = NP * 8
                OP("pe", "matmul", ["UPI", "Gt"], ["bank0"], out=banks[0][:, 0:W], lhsT=UPI[:], rhs=Gf, start=True, stop=True)
                OP("pe", "matmul", ["H0", "Gt"], ["bank1"], out=banks[1][:, 0:W], lhsT=H0[:], rhs=Gf, start=True, stop=True)
                OP("pe", "matmul", ["H1", "Gt"], ["bank2"], out=banks[2][:, 0:W], lhsT=H1[:], rhs=Gf, start=True, stop=True)
                OP("dve", "tensor_copy", ["bank0"], [k + "GC"], out=GC[:].rearrange("p n c -> p (n c)"), in_=banks[0][:, 0:W])
                OP("act", "activation", ["bank1"], [k + "EGL"], out=EGL[:, :, 0, :], in_=banks[1][:, 0:W].rearrange("p (n c) -> p n c", c=8), func=AF.Exp)
                OP("act", "activation", ["bank2"], [k + "EGL"], out=EGL[:, :, 1, :], in_=banks[2][:, 0:W].rearrange("p (n c) -> p n c", c=8), func=AF.Exp)
                OP("dve", "tensor_copy", ["bank1"], ["GLtok"], out=GLtok[0:64].rearrange("p n c -> p (n c)"), in_=banks[1][0:64, 0:W])
                OP("dve", "tensor_copy", ["bank2", "GLtok"], ["GLtok"], out=GLtok[64:128].rearrange("p n c -> p (n c)"), in_=banks[2][64:128, 0:W])
                OP("dve", "tensor_tensor", ["GLtok", k + "GC"], ["GLtok"], out=GLtok[:], in0=GLtok[:], in1=GC[:], op=ALU.subtract)
                OP("act", "activation", ["GLtok"], [k + "EGR"], out=EGR[:], in_=GLtok[:], func=AF.Exp)
                OP("act", "activation", [k + "GC"], [k + "BEG"], out=BEG[:], in_=GC[:], func=AF.Exp)
                OP("dve", "tensor_tensor", [k + "BEG", k + "B"], [k + "BEG"], out=BEG[:], in0=BEG[:], in1=Bt[:], op=ALU.mult)
            S.barrier()
            arena["off"] = mark_dn

            xin = [[(sb("dn_x%d_%d" % (par, part), [128, XW], BF16), "dn_x%d_%d" % (par, part)) for part in range(3)] for par in range(1)]
            szs = [(sb("dn_sz%d" % i, [128, OWN], BF16), "dn_sz%d" % i) for i in range(2)]
            Obuf = [[(sb("dn_O%d_%d" % (par, d), [128, 64, 32]), "dn_O%d_%d" % (par, d)) for d in range(2)] for par in range(1)]
            xos = [(sb("dn_xo%d" % i, [128, OWN], BF16), "dn_xo%d" % i) for i in range(2)]
            F32P = TP("dn_f", 10, [128, 512], F32)
            BFP = TP("dn_b", 36, [128, 512], BF16)
            VNP = [TP("dn_vn%d" % d, 4, [128, 128], BF16) for d in range(2)]
            h32 = [sb("dn_h32_%d" % d, [128, 128]) for d in range(2)]
            hbf = [[sb("dn_hbf%d_%d" % (d, i), [128, 128], BF16) for i in range(2)] for d in range(2)]
            pers = {}
            for d in range(2):
                for par in range(2):
                    pers[(d, par)] = dict(
                        qe=sb("dn_qe%d%d" % (d, par), [128, 512], BF16), qkt=sb("dn_qkt%d%d" % (d, par), [128, 4, 128], BF16),
                        u=sb("dn_u%d%d" % (d, par), [128, 4, 128]), wT=sb("dn_wT%d%d" % (d, par), [128, 512], BF16),
                        kd=sb("dn_kd%d%d" % (d, par), [128, 4, 128], BF16), k="dnp%d%d" % (d, par))
            prot = TP.__new__(TP)
            prot.t = [(banks[i], "bank%d" % i) for i in range(6)]
            prot.i = 0
            banks_bf = [bk.bitcast(BF16) for bk in banks]
            LNQ = -0.5 * float(np.log(128.0))

            def dn_head(hd):
                par = hd % 2
                xb = xin[0]
                sz, szk = szs[par]
                xo, xok = xos[par]
                for part in range(3):
                    xp, xpk = xb[part]
                    r0 = part * 1024 + hd * 128
                    OP("pool", "memset", [], [xpk], ap=xp[:, 0:4], constant=0.0)
                    OP("pool", "memset", [], [xpk], ap=xp[:, 260:264], constant=0.0)
                    OP("pool", "memset", [], [xpk], ap=xp[:, XW - 4:XW], constant=0.0)
                    DMA(xp[:, 4:260], PQKV[r0:r0 + 128, 0:S_CTX], writes=[xpk])
                    DMA(xp[:, 264:264 + S_LAT], PQKV[r0:r0 + 128, S_CTX:S_ALL], writes=[xpk])
                DMA(sz[:], GATES[1024 + hd * 128:1024 + (hd + 1) * 128, :], writes=[szk])
                hstate = {}
                for d in range(2):
                    OP("pool", "memset", [], [("h32", d)], ap=h32[d][:], constant=0.0)
                    OP("pool", "memset", [], [("hbf", d, 0)], ap=hbf[d][0][:], constant=0.0)
                    hstate[d] = 0

                groups = [(0, 256)] + [(S_CTX + i * 512, 512) for i in range(8)]

                def prep(d, gi):
                    o, n = groups[gi]
                    npair = n // 128
                    p0 = o // 128
                    P = pers[(d, gi % 2)]
                    pk = P["k"]
                    GC, Bt, EGR, BEG = GCt[d], Btt[d], EGRt[d], BEGt[d]
                    ctx_off, lat_off = (4, 264) if d == 0 else (4104, 4)
                    base = (ctx_off + o) if o < S_CTX else (lat_off + o - S_CTX)
                    res = {}
                    for part in range(3):
                        xp, xpk = xb[part]
                        V = xp if d == 0 else xp[:, ::-1]
                        wv = dncw_t[:, part * 8 + hd, d, :]
                        T1, T1k = F32P.get()
                        OP("dve", "tensor_scalar", [xpk, "dncw"], [T1k], out=T1[:, 0:n], in0=V[:, base:base + n], scalar1=wv[:, 3:4], scalar2=None, op0=ALU.mult)
                        for j in (2, 1, 0):
                            sh = 3 - j
                            OP("dve", "scalar_tensor_tensor", [xpk, "dncw", T1k], [T1k], out=T1[:, 0:n], in0=V[:, base - sh:base - sh + n],
                               scalar=wv[:, j:j + 1], in1=T1[:, 0:n], op0=ALU.mult, op1=ALU.add)
                        if part == 2:
                            vT, vTk = BFP.get()
                            OP("act", "activation", [T1k], [vTk], out=vT[:, 0:n], in_=T1[:, 0:n], func=AF.Silu)
                            res["v"] = (vT, vTk)
                            continue
                        T2, T2k = F32P.get()
                        OP("act", "activation", [T1k], [T2k], out=T2[:, 0:n], in_=T1[:, 0:n], func=AF.Silu)
                        SQ, SQk = BFP.get()
                        OP("pool", "tensor_tensor", [T2k], [SQk], out=SQ[:, 0:n], in0=T2[:, 0:n], in1=T2[:, 0:n], op=ALU.mult)
                        bk, bkk = prot.get()
                        OP("pe", "matmul", ["onesb", SQk], [bkk], out=bk[:, 0:n], lhsT=onesb[:], rhs=SQ[:, 0:n], start=True, stop=True)
                        RS, RSk = F32P.get()
                        OP("act", "activation", [bkk], [RSk], out=RS[:, 0:n], in_=bk[:, 0:n], func=AF.Ln, bias=EPS)
                        OP("act", "activation", [RSk], [RSk], out=RS[:, 0:n], in_=RS[:, 0:n], func=AF.Exp, scale=-0.5, bias=(LNQ if part == 0 else 0.0))
                        NN, NNk = BFP.get()
                        OP("pool", "tensor_tensor", [T2k, RSk], [NNk], out=NN[:, 0:n], in0=T2[:, 0:n], in1=RS[:, 0:n], op=ALU.mult)
                        res["qk"[part]] = (NN, NNk)
                    qn, qnk = res["q"]
                    kn, knk = res["k"]
                    vT, vTk = res["v"]
                    gcb = GC[:, p0:p0 + npair, hd].unsqueeze(2).to_broadcast([128, npair, 128])
                    v3 = lambda t: t[:, 0:n].rearrange("p (a b) -> p a b", b=128)
                    Rt, Rtk = F32P.get()
                    OP("dve", "tensor_tensor", ["ident", "sc%dGC" % d], [Rtk], out=v3(Rt), in0=ident[:].unsqueeze(1).to_broadcast([128, npair, 128]), in1=gcb, op=ALU.mult)
                    bg, bgk = prot.get()
                    OP("pe", "matmul", ["onesf", Rtk], [bgk], out=bg[:, 0:n], lhsT=onesf[:], rhs=Rt[:, 0:n], start=True, stop=True)
                    EB, EBk = F32P.get()
                    OP("act", "activation", [bgk], [EBk], out=EB[:, 0:n], in_=bg[:, 0:n], func=AF.Exp)
                    OP("pool", "tensor_tensor", [qnk, EBk], [(pk, "qe")], out=P["qe"][:, 0:n], in0=qn[:, 0:n], in1=EB[:, 0:n], op=ALU.mult)
                    Dl, Dlk = F32P.get()
                    OP("dve", "tensor_tensor", ["sc%dGC" % d, bgk], [Dlk], out=v3(Dl), in0=gcb, in1=v3(bg), op=ALU.subtract)
                    OP("pool", "tensor_tensor", [Dlk, "NEGL"], [Dlk], out=v3(Dl), in0=v3(Dl), in1=NEGL[:].unsqueeze(1).to_broadcast([128, npair, 128]), op=ALU.add)
                    OP("act", "activation", [Dlk], [Dlk], out=Dl[:, 0:n], in_=Dl[:, 0:n], func=AF.Exp)
                    W1, W1k = BFP.get()
                    OP("pool", "tensor_tensor", [Dlk, "sc%dB" % d], [W1k], out=v3(W1), in0=v3(Dl),
                       in1=Bt[:, p0:p0 + npair, hd].unsqueeze(2).to_broadcast([128, npair, 128]), op=ALU.mult)
                    Du, Duk = F32P.get()
                    OP("dve", "tensor_tensor", ["sc%dGC" % d, bgk], [Duk], out=v3(Du), in0=v3(bg), in1=gcb, op=ALU.subtract)
                    OP("pool", "tensor_tensor", [Duk, "NEGU"], [Duk], out=v3(Du), in0=v3(Du), in1=NEGU[:].unsqueeze(1).to_broadcast([128, npair, 128]), op=ALU.add)
                    EDT, EDTk = BFP.get()
                    OP("act", "activation", [Duk], [EDTk], out=EDT[:, 0:n], in_=Du[:, 0:n], func=AF.Exp)
                    bkk_, bkkk = prot.get()
                    bqk, bqkk = prot.get()
                    for pi in range(npair):
                        cs_ = slice(pi * 128, (pi + 1) * 128)
                        OP("pe", "matmul", [knk], [bkkk], out=bkk_[:, cs_], lhsT=kn[:, cs_], rhs=kn[:, cs_], start=True, stop=True)
                        OP("pe", "matmul", [knk, qnk], [bqkk], out=bqk[:, cs_], lhsT=kn[:, cs_], rhs=qn[:, cs_], start=True, stop=True)
                    Lm, Lmk = BFP.get()
                    OP("dve", "tensor_tensor", [bkkk, W1k], [Lmk], out=Lm[:, 0:n], in0=bkk_[:, 0:n], in1=W1[:, 0:n], op=ALU.mult)
                    OP("dve", "tensor_tensor", [bqkk, EDTk], [(pk, "qkt")], out=P["qkt"][:, 0:npair, :], in0=v3(bqk), in1=v3(EDT), op=ALU.mult)

                    def mm_pairs(lhs, lhsk, rhs, rhsk, trans=False):
                        bi_ = prot.i % 6
                        bk2, bk2k = prot.get()
                        for pi in range(npair):
                            cs_ = slice(pi * 128, (pi + 1) * 128)
                            if trans:
                                OP("pe", "transpose", [lhsk, "identb"], [bk2k], out=banks_bf[bi_][:, cs_], in_=lhs[:, cs_], identity=identb[:])
                            else:
                                OP("pe", "matmul", [lhsk, rhsk], [bk2k], out=bk2[:, cs_], lhsT=lhs[:, cs_], rhs=rhs[:, cs_], start=True, stop=True)
                        return (banks_bf[bi_] if trans else bk2), bk2k

                    def evac_copy(src, srck, eng="act"):
                        t, tk = BFP.get()
                        if eng == "act":
                            OP("act", "activation", [srck], [tk], out=t[:, 0:n], in_=src[:, 0:n], func=AF.Identity)
                        else:
                            OP("dve", "tensor_copy", [srck], [tk], out=t[:, 0:n], in_=src[:, 0:n])
                        return t, tk

                    bu, buk = mm_pairs(Lm, Lmk, None, None, trans=True)
                    Um, Umk = evac_copy(bu, buk, "act")
                    Pt, Ptk = BFP.get()
                    OP("pool", "tensor_tensor", ["identb", Umk], [Ptk], out=v3(Pt), in0=identb[:].unsqueeze(1).to_broadcast([128, npair, 128]), in1=v3(Um), op=ALU.subtract)
                    Lp, Lpk, Up, Upk = Lm, Lmk, Um, Umk
                    for lvl in range(1, 6):
                        b1, b1k = mm_pairs(Up, Upk, Lp, Lpk)
                        Ln_, Lnk = evac_copy(b1, b1k, "act")
                        if lvl < 5:
                            b2, b2k = mm_pairs(Lp, Lpk, Up, Upk)
                            Un_, Unk = evac_copy(b2, b2k, "dve")
                        b3, b3k = mm_pairs(Ln_, Lnk, Pt, Ptk)
                        Pn, Pnk = BFP.get()
                        OP("dve", "tensor_tensor", [b3k, Ptk], [Pnk], out=Pn[:, 0:n], in0=b3[:, 0:n], in1=Pt[:, 0:n], op=ALU.add)
                        Pt, Ptk = Pn, Pnk
                        Lp, Lpk = Ln_, Lnk
                        if lvl < 5:
                            Up, Upk = Un_, Unk
                    Tt, Ttk = Pt, Ptk
                    bkt, bktk = mm_pairs(kn, knk, None, None, trans=True)
                    kbe, kbek = BFP.get()
                    for pi in range(npair):
                        cs_ = slice(pi * 128, (pi + 1) * 128)
                        OP("act", "activation", [bktk, "sc%dBEG" % d], [kbek], out=kbe[:, cs_], in_=bkt[:, cs_], func=AF.Identity, scale=BEG[:, p0 + pi, hd:hd + 1])
                        OP("act", "activation", [bktk, "sc%dEGR" % d], [(pk, "kd")], out=P["kd"][:, pi, :], in_=bkt[:, cs_], func=AF.Identity, scale=EGR[:, p0 + pi, hd:hd + 1])
                    bvt, bvtk = mm_pairs(vT, vTk, None, None, trans=True)
                    vb, vbk = BFP.get()
                    for pi in range(npair):
                        cs_ = slice(pi * 128, (pi + 1) * 128)
                        OP("act", "activation", [bvtk, "sc%dB" % d], [vbk], out=vb[:, cs_], in_=bvt[:, cs_], func=AF.Identity, scale=Bt[:, p0 + pi, hd:hd + 1])
                    bu2, bu2k = mm_pairs(Tt, Ttk, vb, vbk)
                    OP("dve", "tensor_copy", [bu2k], [(pk, "u")], out=P["u"][:, 0:npair, :], in_=v3(bu2))
                    bw, bwk = mm_pairs(kbe, kbek, Tt, Ttk)
                    OP("act", "activation", [bwk], [(pk, "wT")], out=P["wT"][:, 0:n], in_=bw[:, 0:n], func=AF.Identity)

                def rec(gi):
                    o, n = groups[gi]
                    nch = n // 64
                    p0 = o // 128
                    is_lat = o >= S_CTX
                    for ci in range(nch):
                        pi, hf = ci // 2, ci % 2
                        Ps = slice(hf * 64, hf * 64 + 64)
                        for d in range(2):
                            P = pers[(d, gi % 2)]
                            pk = P["k"]
                            cur = hstate[d]
                            nxt = 1 - cur
                            hb_c, hb_n = hbf[d][cur], hbf[d][nxt]
                            bwh, bwhk = prot.get()
                            OP("pe", "matmul", [(pk, "wT"), ("hbf", d, cur)], [bwhk], out=bwh[Ps, 0:128], lhsT=P["wT"][:, ci * 64:(ci + 1) * 64], rhs=hb_c[:],
                               start=True, stop=True)
                            vn, vnk = VNP[d].get()
                            OP("dve", "tensor_tensor", [(pk, "u"), bwhk], [vnk], out=vn[Ps, :], in0=P["u"][Ps, pi, :], in1=bwh[Ps, 0:128], op=ALU.subtract)
                            if is_lat:
                                ob = banks[6 + d]
                                OP("pe", "matmul", [("hbf", d, cur), (pk, "qe")], [("obank", d)], out=ob[:, ci * 64:(ci + 1) * 64], lhsT=hb_c[:],
                                   rhs=P["qe"][:, ci * 64:(ci + 1) * 64], start=True, stop=False)
                                OP("pe", "matmul", [vnk, (pk, "qkt")], [("obank", d)], out=ob[:, ci * 64:(ci + 1) * 64], lhsT=vn[Ps, :],
                                   rhs=P["qkt"][Ps, pi, hf * 64:hf * 64 + 64], start=False, stop=True)
                            bh, bhk = prot.get()
                            OP("pe", "matmul", [(pk, "kd"), vnk], [bhk], out=bh[:, 0:128], lhsT=P["kd"][Ps, pi, :], rhs=vn[Ps, :], start=True, stop=True)
                            egl = EGLt[d][:, p0 + pi, hf, hd:hd + 1]
                            OP("dve", "scalar_tensor_tensor", [("h32", d), bhk, "sc%dEGL" % d], [("hbf", d, nxt)], out=hb_n[:], in0=h32[d][:], scalar=egl,
                               in1=bh[:, 0:128], op0=ALU.mult, op1=ALU.add)
                            OP("dve", "scalar_tensor_tensor", [("h32", d), bhk, "sc%dEGL" % d], [("h32", d)], out=h32[d][:], in0=h32[d][:], scalar=egl,
                               in1=bh[:, 0:128], op0=ALU.mult, op1=ALU.add)
                            hstate[d] = nxt
                    if is_lat:
                        cl0 = (o - S_CTX) // 64
                        for d in range(2):
                            Ob, Obk = Obuf[0][d]
                            src = banks[6 + d][:, 0:512].rearrange("p (c i) -> p c i", i=64)
                            if d == 0:
                                OP("act", "activation", [("obank", d)], [Obk], out=Ob[:, cl0:cl0 + 8, :], in_=src[:, :, 0:32], func=AF.Identity)
                            else:
                                hi = 63 - cl0
                                OP("act", "activation", [("obank", d)], [Obk], out=Ob[:, hi - 7:hi + 1, :][:, ::-1, :], in_=src[:, :, 32:64][:, :, ::-1], func=AF.Identity)

                for gi in range(len(groups)):
                    prep(0, gi)
                    prep(1, gi)
                    if gi >= 1:
                        rec(gi - 1)
                rec(len(groups) - 1)

                O0, O0k = Obuf[0][0]
                O1, O1k = Obuf[0][1]
                Of = O0[:].rearrange("p c r -> p (c r)")
                OP("pool", "tensor_tensor", [O0k, O1k], [O0k], out=Of, in0=Of, in1=O1[:].rearrange("p c r -> p (c r)"), op=ALU.add)
                for q4 in range(4):
                    cs_ = slice(q4 * 512, (q4 + 1) * 512)
                    SQ, SQk = BFP.get()
                    OP("pool", "tensor_tensor", [O0k], [SQk], out=SQ[:], in0=Of[:, cs_], in1=Of[:, cs_], op=ALU.mult)
                    bk, bkk = prot.get()
                    OP("pe", "matmul", ["onesb", SQk], [bkk], out=bk[:], lhsT=onesb[:], rhs=SQ[:], start=True, stop=True)
                    RS, RSk = F32P.get()
                    OP("act", "activation", [bkk], [RSk], out=RS[:], in_=bk[:], func=AF.Ln, scale=1.0 / 128, bias=EPS)
                    OP("act", "activation", [RSk], [RSk], out=RS[:], in_=RS[:], func=AF.Exp, scale=-0.5)
                    OP("dve", "scalar_tensor_tensor", [O0k, RSk, "gnorm"], [O1k], out=O1[:].rearrange("p c r -> p (c r)")[:, cs_], in0=Of[:, cs_],
                       scalar=gnorm[:, 0:1], in1=RS[:], op0=ALU.mult, op1=ALU.mult)
                OP("pool", "tensor_tensor", [O1k, szk], [xok], out=xo[:].rearrange("p (r c) -> p r c", c=64), in0=O1[:].rearrange("p c r -> p r c"),
                   in1=sz[:].rearrange("p (r c) -> p r c", c=64), op=ALU.mult)
                DMA(XDN[hd * 128:(hd + 1) * 128, :], xo[:], reads=[xok])

            for hd in dn_heads:
                dn_head(hd)
            S.barrier()
            arena["off"] = mark0

        top = {"off": ARENA_WORDS}

        def sb_top(name, shape, dt=F32):
            shape = list(shape)
            elems = int(np.prod(shape[1:]))
            words = elems if dt == F32 else (elems + 1) // 2
            words += words % 2
            top["off"] -= words
            off = top["off"]
            ap = arena_t[0:shape[0], off:off + words]
            if dt != F32:
                ap = ap.bitcast(dt)[:, 0:elems]
            else:
                ap = ap[:, 0:elems]
            if len(shape) == 3:
                ap = ap.rearrange("p (a b) -> p a b", a=shape[1])
            return ap

        NT = OWN // 128
        if "tail" in phases:
            hfT = sb_top("hfT", [128, 8, OWN], BF16)
            CW = sb_top("CW", [128, NT, NE])
            G2P = sb_top("G2P", [128, D])
            prot = TP.__new__(TP)
            prot.t = [(banks[i], "bank%d" % i) for i in range(8)]
            prot.i = 0
            G1P = sb("G1P", [128, D])
            GP2 = sb("GP2", [128, D])
            SH2t = sb("SH2t", [128, D])
            rb = sb("rb", [128, NE])
            rw = sb("rw", [128, 8, NE])
            mark_t = arena["off"]
            MOD = sb("MOD", [128, 4, D])
            rows = sb("rows", [128, 3, D])
            csrep = sb("csrep", [128, 8, 128])
            adab_r = sb("adab_r", [128, 4 * D])
            DMA(adab_r[:], ada_b[:, 2048:6144].partition_broadcast(128), writes=["adab_r"])
            DMA(rows[:, 0, :], gpost.partition_broadcast(128), writes=["rows"])
            DMA(rows[:, 1, :], gfpre.partition_broadcast(128), writes=["rows"])
            DMA(rows[:, 2, :], gfpost.partition_broadcast(128), writes=["rows"])
            DMA(rb[:], router_b.partition_broadcast(128), writes=["rb"])
            DMA(rw[:], router_w.rearrange("(kc p) e -> p kc e", p=128), writes=["rw"])
            for kc in range(8):
                OP("dve", "tensor_copy", [], [("csrep", kc)], out=csrep[:, kc, :], in_=cs[:, kc, 0:1].to_broadcast([128, 128]))
            awp = TP("awp", 2, [128, 8, 512], F32)
            for pc_ in range(8):
                aw, awk = awp.get()
                for kc in range(8):
                    DMA(aw[:, kc, :], ada_w[kc * 128:(kc + 1) * 128, 2048 + pc_ * 512:2048 + (pc_ + 1) * 512], writes=[(awk, kc)])
                bk, bkk = prot.get()
                for kc in range(8):
                    OP("pe", "matmul", [(awk, kc), ("csrep", kc)], [bkk], out=bk[:], lhsT=csrep[:, kc, :], rhs=aw[:, kc, :], start=(kc == 0), stop=(kc == 7))
                OP("dve", "tensor_tensor", [bkk, "adab_r"], ["MOD"], out=MOD[:].rearrange("p j d -> p (j d)")[:, pc_ * 512:(pc_ + 1) * 512], in0=bk[:],
                   in1=adab_r[:, pc_ * 512:(pc_ + 1) * 512], op=ALU.add)
            OP("dve", "tensor_tensor", ["MOD", "rows"], ["G1P"], out=G1P[:], in0=MOD[:, 0, :], in1=rows[:, 0, :], op=ALU.mult)
            OP("dve", "scalar_tensor_tensor", ["MOD", "rows"], ["GP2"], out=GP2[:], in0=MOD[:, 2, :], scalar=1.0, in1=rows[:, 1, :], op0=ALU.add, op1=ALU.mult)
            OP("dve", "tensor_tensor", ["MOD", "rows"], ["G2P"], out=G2P[:], in0=MOD[:, 3, :], in1=rows[:, 2, :], op=ALU.mult)
            OP("pool", "tensor_copy", ["MOD"], ["SH2t"], out=SH2t[:], in_=MOD[:, 1, :])
            SH2 = SH2t[:]
            S.barrier()
            arena["off"] = mark_t
            n_blk = 4 if tail_stop >= 2 else 0
            Wrg = sb("Wrg", [128, 8, D], BF16)
            Wdn = sb("Wdn", [128, 8, D], BF16)
            Wou = sb("Wou", [128, 8, D], BF16)
            for (wt, src, wk) in ((Wrg, rg_w_o, "Wrg"), (Wdn, dn_w_o, "Wdn"), (Wou, w_out, "Wou")):
                for kc in range(8):
                    S.op("pool", lambda e, wt=wt, src=src, kc=kc: e.dma_start(out=wt[:, kc, :], in_=src[kc * 128:(kc + 1) * 128, :]), writes=[(wk, kc)], dma=True)
            xrb = sb("t_xrg", [128, 8, 512], BF16)
            xdb = sb("t_xdn", [128, 8, 512], BF16)
            s1b = sb("t_sg1", [128, 8, 512], BF16)
            s2b = sb("t_sg2", [128, 8, 512], BF16)
            mrg = sb("t_mrg", [128, 8, 512], BF16)
            F4 = TP("t_f4", 6, [128, D], F32)
            F2 = TP("t_f2", 4, [128, 512], F32)
            hf32 = TP("t_hf32", 2, [128, 8, 128], F32)
            sst = TP("t_ss", 4, [128, 8], F32)
            junk = sb("t_junk", [128, D], BF16)
            print("TAIL arena bottom", arena["off"], "top", top["off"])
            assert arena["off"] <= top["off"], "tail arena overlap"
            for blk in range(n_blk):
                t0 = blk * 512
                for kc in range(8):
                    DMA(xrb[:, kc, :], XRG[kc * 128:(kc + 1) * 128, t0:t0 + 512], writes=[("xrb", kc)])
                    DMA(xdb[:, kc, :], XDN[kc * 128:(kc + 1) * 128, t0:t0 + 512], writes=[("xdb", kc)])
                    DMA(s1b[:, kc, :], GATES[2048 + kc * 128:2048 + (kc + 1) * 128, t0:t0 + 512], writes=[("s1b", kc)])
                    DMA(s2b[:, kc, :], GATES[3072 + kc * 128:3072 + (kc + 1) * 128, t0:t0 + 512], writes=[("s2b", kc)])
                for m in range(8):
                    b1, b1k = prot.get()
                    b2, b2k = prot.get()
                    for kc in range(8):
                        OP("pe", "matmul", [("Wrg", kc), ("xrb", kc)], [b1k], out=b1[:], lhsT=Wrg[:, kc, m * 128:(m + 1) * 128], rhs=xrb[:, kc, :], start=(kc == 0), stop=(kc == 7))
                    for kc in range(8):
                        OP("pe", "matmul", [("Wdn", kc), ("xdb", kc)], [b2k], out=b2[:], lhsT=Wdn[:, kc, m * 128:(m + 1) * 128], rhs=xdb[:, kc, :], start=(kc == 0), stop=(kc == 7))
                    ta, tak = F2.get()
                    tb_, tbk = F2.get()
                    OP("dve", "tensor_tensor", [b1k, ("s1b", m)], [tak], out=ta[:], in0=b1[:], in1=s1b[:, m, :], op=ALU.mult)
                    OP("dve", "tensor_tensor", [b2k, ("s2b", m)], [tbk], out=tb_[:], in0=b2[:], in1=s2b[:, m, :], op=ALU.mult)
                    OP("pool", "tensor_tensor", [tak, tbk], [("mrg", m)], out=mrg[:, m, :], in0=ta[:], in1=tb_[:], op=ALU.add)
                for tt in range(4 if tail_stop >= 3 else 0):
                    gt_ = blk * 4 + tt
                    r0 = gt_ * 128
                    xt, xtk = F4.get()
                    DMA(xt[:], x[r0:r0 + 128, :], writes=[xtk])
                    ss, ssk = sst.get()
                    yb = []
                    for n2 in range(2):
                        bk, bkk = prot.get()
                        for kc in range(8):
                            OP("pe", "matmul", [("Wou", kc), ("mrg", kc)], [bkk], out=bk[:], lhsT=mrg[:, kc, tt * 128:(tt + 1) * 128], rhs=Wou[:, kc, n2 * 512:(n2 + 1) * 512],
                               start=(kc == 0), stop=(kc == 7))
                        OP("act", "activation", [bkk], ["t_junk", (ssk, n2)], out=junk[:, 0:512], in_=bk[:], func=AF.Square, accum_out=ss[:, n2:n2 + 1])
                        yb.append((bk, bkk))
                    if tail_stop < 3.2:
                        continue
                    OP("dve", "tensor_tensor", [(ssk, 0), (ssk, 1)], [(ssk, 2)], out=ss[:, 2:3], in0=ss[:, 0:1], in1=ss[:, 1:2], op=ALU.add)
                    OP("dve", "tensor_scalar", [(ssk, 2)], [(ssk, 2)], out=ss[:, 2:3], in0=ss[:, 2:3], scalar1=1.0 / D, scalar2=EPS, op0=ALU.mult, op1=ALU.add)
                    OP("act", "activation", [(ssk, 2)], [(ssk, 2)], out=ss[:, 2:3], in_=ss[:, 2:3], func=AF.Sqrt)
                    OP("dve", "reciprocal", [(ssk, 2)], [(ssk, 2)], out=ss[:, 2:3], in_=ss[:, 2:3])
                    xn, xnk = F4.get()
                    for n2 in range(2):
                        bk, bkk = yb[n2]
                        cs_ = slice(n2 * 512, (n2 + 1) * 512)
                        OP("dve", "scalar_tensor_tensor", [bkk, (ssk, 2), "G1P"], [(xnk, n2)], out=xn[:, cs_], in0=bk[:], scalar=ss[:, 2:3], in1=G1P[:, cs_], op0=ALU.mult, op1=ALU.mult)
                    OP("pool", "tensor_tensor", [(xnk, 0), (xnk, 1), xtk], [xnk, (xnk, 0), (xnk, 1)], out=xn[:], in0=xn[:], in1=xt[:], op=ALU.add)
                    if tail_stop < 3.4:
                        continue
                    DMA(XNEW[r0:r0 + 128, :], xn[:], reads=[xnk])
                    OP("act", "activation", [xnk], ["t_junk", (ssk, 3)], out=junk[:], in_=xn[:], func=AF.Square, accum_out=ss[:, 3:4])
                    OP("dve", "tensor_scalar", [(ssk, 3)], [(ssk, 3)], out=ss[:, 3:4], in0=ss[:, 3:4], scalar1=1.0 / D, scalar2=EPS, op0=ALU.mult, op1=ALU.add)
                    OP("act", "activation", [(ssk, 3)], [(ssk, 3)], out=ss[:, 3:4], in_=ss[:, 3:4], func=AF.Sqrt)
                    OP("dve", "reciprocal", [(ssk, 3)], [(ssk, 3)], out=ss[:, 3:4], in_=ss[:, 3:4])
                    if tail_stop < 3.6:
                        continue
                    hf, hfk = F4.get()
                    OP("dve", "scalar_tensor_tensor", [xnk, (ssk, 3), "GP2"], [hfk], out=hf[:], in0=xn[:], scalar=ss[:, 3:4], in1=GP2[:], op0=ALU.mult, op1=ALU.mult)
                    OP("pool", "tensor_tensor", [hfk, "SH2t"], [hfk], out=hf[:], in0=hf[:], in1=SH2, op=ALU.add)
                    if tail_stop < 3.8:
                        continue
                    h32t, h32k = hf32.get()
                    for half in range(2):
                        bk, bkk = prot.get()
                        for c4 in range(4):
                            c = half * 4 + c4
                            OP("pe", "transpose", [hfk, "ident"], [bkk], out=bk[:, c4 * 128:(c4 + 1) * 128], in_=hf[:, c * 128:(c + 1) * 128], identity=ident[:])
                        OP("dve", "tensor_copy", [bkk], [(h32k, half)], out=h32t[:, half * 4:half * 4 + 4, :], in_=bk[:].rearrange("p (c t) -> p c t", c=4))
                        OP("pool", "tensor_copy", [(h32k, half)], [("hfT", gt_, half)], out=hfT[:, half * 4:half * 4 + 4, r0:r0 + 128], in_=h32t[:, half * 4:half * 4 + 4, :])
                    if tail_stop < 4:
                        continue
                    bl, blk_ = prot.get()
                    for kc in range(8):
                        OP("pe", "matmul", [(h32k, kc // 4), "rw"], [blk_], out=bl[:, 0:NE], lhsT=h32t[:, kc, :], rhs=rw[:, kc, :], start=(kc == 0), stop=(kc == 7))
                    lg, lgk = F2.get()
                    OP("dve", "tensor_tensor", [blk_, "rb"], [lgk], out=lg[:, 0:NE], in0=bl[:, 0:NE], in1=rb[:], op=ALU.add)
                    OP("dve", "max", [lgk], [(lgk, "m8")], out=lg[:, 64:72], in_=lg[:, 0:NE])
                    OP("dve", "tensor_scalar", [lgk, (lgk, "m8")], [(lgk, "mask")], out=lg[:, 128:128 + NE], in0=lg[:, 0:NE], scalar1=lg[:, 67:68], scalar2=None, op0=ALU.is_ge)
                    OP("dve", "tensor_scalar", [(lgk, "m8")], [(lgk, "nm")], out=lg[:, 72:73], in0=lg[:, 64:65], scalar1=-1.0, scalar2=None, op0=ALU.mult)
                    OP("act", "activation", [lgk, (lgk, "nm")], [(lgk, "e")], out=lg[:, 192:192 + NE], in_=lg[:, 0:NE], func=AF.Exp, bias=lg[:, 72:73])
                    OP("dve", "tensor_tensor", [(lgk, "e"), (lgk, "mask")], [(lgk, "em")], out=lg[:, 256:256 + NE], in0=lg[:, 192:192 + NE], in1=lg[:, 128:128 + NE], op=ALU.mult)
                    OP("dve", "tensor_reduce", [(lgk, "em")], [(lgk, "sum")], out=lg[:, 73:74], in_=lg[:, 256:256 + NE], axis=mybir.AxisListType.X, op=ALU.add)
                    OP("dve", "reciprocal", [(lgk, "sum")], [(lgk, "sum")], out=lg[:, 73:74], in_=lg[:, 73:74])
                    OP("dve", "tensor_scalar", [(lgk, "em"), (lgk, "sum")], [("CW", gt_)], out=CW[:, gt_, :], in0=lg[:, 256:256 + NE], scalar1=lg[:, 73:74], scalar2=None, op0=ALU.mult)
            if debug:
                dbg("CW", CW[:], [("CW", i) for i in range(NT)])
                dbg("hfT", hfT[:, :, :], [("hfT", i, h) for i in range(NT) for h in range(2)])
            S.barrier()
            arena["off"] = mark0

        if "moe" in phases:
            prot = TP.__new__(TP)
            prot.t = [(banks[i], "bank%d" % i) for i in range(8)]
            prot.i = 0
            acc = sb("acc", [128, NT, D])
            b1T = sb("b1T", [128, NE, 16])
            mark_m = arena["off"]
            eb2 = sb("eb2", [NE, D])
            cwT = sb("cwT", [NE, NT, 128])
            DMA(b1T[:], e_b1T, writes=["b1T"])
            DMA(eb2[:], e_b2, writes=["eb2"])
            for tt in range(NT):
                bk, bkk = prot.get()
                OP("pe", "transpose", [], [bkk], out=bk[0:NE, 0:128], in_=CW[:, tt, :], identity=ident[:])
                OP("dve", "tensor_copy", [bkk], [("cwT", tt)], out=cwT[:, tt, :], in_=bk[0:NE, 0:128])
                for n2 in range(2):
                    bk2, bk2k = prot.get()
                    OP("pe", "matmul", [("cwT", tt), "eb2"], [bk2k], out=bk2[:], lhsT=cwT[:, tt, :], rhs=eb2[:, n2 * 512:(n2 + 1) * 512], start=True, stop=True)
                    OP("act", "activation", [bk2k], [("acc", tt, n2)], out=acc[:, tt, n2 * 512:(n2 + 1) * 512], in_=bk2[:], func=AF.Identity)
            S.barrier()
            arena["off"] = mark_m
            w1b = sb("w1b", [128, 8, 2 * D], BF16)
            w2b = sb("w2b", [128, 8, D], BF16)
            stg = TP("m_stg", 2, [128, 2 * D], F32)
            actT = sb("actT", [128, 8, OWN // 2], BF16)
            EF = TP("m_ef", 6, [128, 512], F32)
            n_exp = moe_experts
            castq = Rot(["pool", "pool", "act"])
            for e_ in range(n_exp):
                for kc in range(8):
                    sg, sgk = stg.get()
                    DMA(sg[:], e_w1[e_, kc * 128:(kc + 1) * 128, :], writes=[sgk])
                    OP("pool", "tensor_copy", [sgk], [("w1b", kc)], out=w1b[:, kc, :], in_=sg[:])
                for kc in range(0, 8, 2):
                    sg, sgk = stg.get()
                    DMA(sg[:].rearrange("p (a b) -> p a b", a=2), e_w2[e_, kc * 128:(kc + 2) * 128, :].rearrange("(a p) d -> p a d", p=128), writes=[sgk])
                    OP("pool", "tensor_copy", [sgk], [("w2b", kc), ("w2b", kc + 1)], out=w2b[:, kc:kc + 2, :], in_=sg[:].rearrange("p (a b) -> p a b", a=2))
                for half in range(2):
                    h0 = half * (OWN // 2)
                    for m in range(8):
                        for nt_ in range(2):
                            tk0 = h0 + nt_ * 512
                            bg, bgk = prot.get()
                            bl, blk_ = prot.get()
                            for kc in range(8):
                                OP("pe", "matmul", [("w1b", kc)], [bgk], out=bg[:], lhsT=w1b[:, kc, m * 128:(m + 1) * 128], rhs=hfT[:, kc, tk0:tk0 + 512], start=(kc == 0), stop=(kc == 7))
                            for kc in range(8):
                                OP("pe", "matmul", [("w1b", kc)], [blk_], out=bl[:], lhsT=w1b[:, kc, D + m * 128:D + (m + 1) * 128], rhs=hfT[:, kc, tk0:tk0 + 512], start=(kc == 0), stop=(kc == 7))
                            tg, tgk = EF.get()
                            ts, tsk = EF.get()
                            tl, tlk = EF.get()
                            OP("dve", "tensor_scalar", [bgk, "b1T"], [tgk], out=tg[:], in0=bg[:], scalar1=b1T[:, e_, m:m + 1], scalar2=7.0, op0=ALU.add, op1=ALU.min)
                            OP("act", "activation", [tgk], [tsk], out=ts[:], in_=tg[:], func=AF.Sigmoid, scale=1.702)
                            OP("dve", "tensor_scalar", [blk_, "b1T"], [tlk], out=tl[:], in0=bl[:], scalar1=b1T[:, e_, 8 + m:9 + m], scalar2=7.0, op0=ALU.add, op1=ALU.min)
                            OP("dve", "tensor_scalar", [tlk], [tlk], out=tl[:], in0=tl[:], scalar1=-7.0, scalar2=1.0, op0=ALU.max, op1=ALU.add)
                            OP("pool", "tensor_tensor", [tgk, tsk], [tgk], out=tg[:], in0=tg[:], in1=ts[:], op=ALU.mult)
                            OP("pool", "tensor_tensor", [tgk, tlk], [("actT", m, nt_)], out=actT[:, m, nt_ * 512:(nt_ + 1) * 512], in0=tg[:], in1=tl[:], op=ALU.mult)
                    for tt in range(8):
                        gt_ = half * 8 + tt
                        for n2 in range(2):
                            bk, bkk = prot.get()
                            for m in range(8):
                                OP("pe", "matmul", [("actT", m, tt // 4), ("w2b", m)], [bkk], out=bk[:], lhsT=actT[:, m, tt * 128:(tt + 1) * 128], rhs=w2b[:, m, n2 * 512:(n2 + 1) * 512],
                                   start=(m == 0), stop=(m == 7))
                            OP("dve", "scalar_tensor_tensor", [bkk, ("acc", gt_, n2)], [("acc", gt_, n2)], out=acc[:, gt_, n2 * 512:(n2 + 1) * 512], in0=bk[:],
                               scalar=CW[:, gt_, e_:e_ + 1], in1=acc[:, gt_, n2 * 512:(n2 + 1) * 512], op0=ALU.mult, op1=ALU.add)
            S.barrier()
            arena["off"] = mark_m
            FX = TP("m_fx", 3, [128, D], F32)
            FO = TP("m_fo", 3, [128, D], F32)
            sst2 = TP("m_ss", 3, [128, 2], F32)
            junk2 = sb("m_junk", [128, D], BF16)
            outs = []
            for tt in range(NT):
                r0 = tt * 128
                xn, xnk = FX.get()
                DMA(xn[:], XNEW[r0:r0 + 128, :], writes=[xnk])
                ss, ssk = sst2.get()
                OP("act", "activation", [("acc", tt, 0), ("acc", tt, 1)], ["m_junk", ssk], out=junk2[:], in_=acc[:, tt, :], func=AF.Square, accum_out=ss[:, 0:1])
                OP("dve", "tensor_scalar", [ssk], [ssk], out=ss[:, 0:1], in0=ss[:, 0:1], scalar1=1.0 / D, scalar2=EPS, op0=ALU.mult, op1=ALU.add)
                OP("act", "activation", [ssk], [ssk], out=ss[:, 0:1], in_=ss[:, 0:1], func=AF.Sqrt)
                OP("dve", "reciprocal", [ssk], [ssk], out=ss[:, 0:1], in_=ss[:, 0:1])
                fo, fok = FO.get()
                OP("dve", "scalar_tensor_tensor", [("acc", tt, 0), ("acc", tt, 1), ssk], [fok], out=fo[:], in0=acc[:, tt, :], scalar=ss[:, 0:1], in1=G2P[:], op0=ALU.mult, op1=ALU.mult)
                OP("pool", "tensor_tensor", [fok, xnk], [fok], out=fo[:], in0=fo[:], in1=xn[:], op=ALU.add)
                outs.append(S.op("sp", lambda e, fo=fo, r0=r0: e.dma_start(out=out[r0:r0 + 128, :], in_=fo[:]), reads=[fok], dma=True))
            S.barrier()

        S.emit(st)
    return nc


def prepare_core_inputs(inputs, b, half):
    f = np.ascontiguousarray
    flip = (half == 1)
    xs = inputs["x"][b]
    cs = inputs["ctx"][b]
    if flip:
        xs = xs[::-1]
        cs = cs[::-1]
    w_in = inputs["w_in"][0]
    if flip:
        w_in = w_in.copy()
        a0 = 6144
        w_in[:, a0:a0 + 8], w_in[:, a0 + 8:a0 + 16] = inputs["w_in"][0][:, a0 + 8:a0 + 16], inputs["w_in"][0][:, a0:a0 + 8]
        b0 = 6160
        w_in[:, b0:b0 + 8], w_in[:, b0 + 8:b0 + 16] = inputs["w_in"][0][:, b0 + 8:b0 + 16], inputs["w_in"][0][:, b0:b0 + 8]
    cvec = np.stack([inputs["c"][b], inputs["c_ctx"]], axis=-1)
    m = {
        "x": f(xs), "ctx": f(cs),
        "cc": f(cvec.reshape(8, 128, 2).transpose(1, 0, 2)),
        "ada_w": f(inputs["ada_w"][0]),
        "ada_bT": f(inputs["ada_b"][0].reshape(48, 128).T),
        "ada_b": f(inputs["ada_b"][0].reshape(1, -1)),
        "gpreT": f(inputs["mix_pre_g"][0].reshape(8, 128).T),
        "w_in": f(w_in),
    }
    dsel = (lambda a: a[::-1]) if flip else (lambda a: a)
    P = lambda k: dsel(inputs[k][0])
    rgp = np.concatenate([P("rg_conv_w").transpose(0, 2, 1),
                          P("rg_conv_b")[:, :, None], P("rg_ba")[:, :, None], P("rg_bi")[:, :, None], P("rg_lam")[:, :, None]], axis=2)
    m["rgp"] = f(rgp.reshape(2, 8, 128, 8).transpose(2, 1, 0, 3))
    m["rg_wa"] = f(P("rg_wa"))
    m["rg_wi"] = f(P("rg_wi"))
    cw = P("dn_conv_w")
    m["dncw"] = f(cw.reshape(2, 4, 24, 128).transpose(3, 2, 0, 1))
    m["dnc"] = f(np.concatenate([P("dn_a_log").reshape(-1), P("dn_dt_bias").reshape(-1)]).reshape(1, 32))
    m["dn_norm_g"] = f(inputs["dn_norm_g"][0].reshape(128, 1))
    m["gpost"] = f(inputs["mix_post_g"][0].reshape(1, -1))
    m["gfpre"] = f(inputs["ffn_pre_g"][0].reshape(1, -1))
    m["gfpost"] = f(inputs["ffn_post_g"][0].reshape(1, -1))
    for k in ("rg_w_o", "dn_w_o", "w_out", "router_w", "e_w1", "e_w2", "e_b2"):
        m[k] = f(inputs[k][0])
    m["router_b"] = f(inputs["router_b"][0].reshape(1, -1))
    m["e_b1T"] = f(inputs["e_b1"][0].reshape(NE, 16, 128).transpose(2, 0, 1))
    return {k: np.asarray(v, dtype=np.float32) for k, v in m.items()}


_PROG = {}


def kernel(**inputs):
    inputs = {k: np.asarray(v) for k, v in inputs.items()}
    if "nc" not in _PROG:
        _PROG["nc"] = build_program()
    nc = _PROG["nc"]
    in_maps = []
    for core in range(8):
        b, half = core // 2, core % 2
        in_maps.append(prepare_core_inputs(inputs, b, half))
    res = run_bass_kernel_spmd(nc, in_maps, core_ids=list(range(8)))
    B = inputs["x"].shape[0]
    outp = np.zeros((B, S_LAT, D), np.float32)
    for core in range(8):
        b, half = core // 2, core % 2
        o = np.asarray(res.results[core]["out"], dtype=np.float32)
        if half == 0:
            outp[b, 0:OWN] = o
        else:
            outp[b, S_LAT - OWN:] = o[::-1]
    return outp
```

```python
from contextlib import ExitStack
import numpy as np
import concourse.bass as bass
import concourse.mybir as mybir
from concourse.bass_utils import run_bass_kernel_spmd

F32 = mybir.dt.float32
BF16 = mybir.dt.bfloat16
AF = mybir.ActivationFunctionType
ALU = mybir.AluOpType

D = 1024
S_LAT = 4096
S_CTX = 256
S_ALL = S_LAT + S_CTX
OWN = 2048
EPS = 1e-6
NE = 32
N_DMA_SEM = 32
SEM_GEN = 30000


class Sched:
    ENGS = ("pe", "act", "dve", "pool", "sp")

    def __init__(self, nc):
        self.nc = nc
        self.ops = {e: [] for e in self.ENGS}
        self.cnt = {e: 0 for e in self.ENGS}
        self.known = {e: {} for e in self.ENGS}
        self.last_w = {}
        self.readers = {}
        self.dma_n = 0
        self.dma_slot_target = [0] * N_DMA_SEM
        self.semids = set()

    def _add_wait(self, eng, waits, tok, raw):
        semid, val, teng = tok
        if teng == eng:
            if eng == "pe" or not raw:
                return
        if self.known[eng].get(semid, 0) >= val:
            return
        waits[semid] = max(waits.get(semid, 0), val)

    def op(self, eng, fn, reads=(), writes=(), dma=False):
        waits = {}
        for k in reads:
            t = self.last_w.get(k)
            if t is not None:
                self._add_wait(eng, waits, t, True)
            if (isinstance(k, str) and k.startswith("bank")) or (isinstance(k, tuple) and k[0] == "obank"):
                for t in self.readers.get(k, ()):
                    self._add_wait(eng, waits, t, False)
        for k in writes:
            t = self.last_w.get(k)
            if t is not None:
                self._add_wait(eng, waits, t, False)
            for t in self.readers.get(k, ()):
                self._add_wait(eng, waits, t, False)
        if dma:
            slot = self.dma_n % N_DMA_SEM
            self.dma_n += 1
            prev = self.dma_slot_target[slot]
            if prev > 0:
                self._add_wait(eng, waits, ("q%d" % slot, prev, None), True)
            val = prev + 16
            self.dma_slot_target[slot] = val
            tok = ("q%d" % slot, val, None)
        else:
            self.cnt[eng] += 1
            gen, val = divmod(self.cnt[eng] - 1, SEM_GEN)
            tok = ("%s.%d" % (eng, gen), val + 1, eng)
            if gen > 0 and val == 0:
                pass
        self.semids.add(tok[0])
        for semid, val in waits.items():
            self.known[eng][semid] = val
        self.ops[eng].append((waits, fn, tok))
        for k in reads:
            self.readers.setdefault(k, []).append(tok)
        for k in writes:
            self.last_w[k] = tok
            self.readers[k] = []
        return tok

    def wait_tokens(self, eng, toks):
        waits = {}
        for t in toks:
            self._add_wait(eng, waits, t, True)
        for semid, val in waits.items():
            self.known[eng][semid] = val
        self.ops[eng].append((waits, None, None))

    def barrier(self):
        toks = []
        for e in self.ENGS:
            if self.cnt[e] > 0:
                gen, val = divmod(self.cnt[e] - 1, SEM_GEN)
                toks.append(("%s.%d" % (e, gen), val + 1, None))
        for slot in range(N_DMA_SEM):
            if self.dma_slot_target[slot] > 0:
                toks.append(("q%d" % slot, self.dma_slot_target[slot], None))
        for e in self.ENGS:
            self.wait_tokens(e, toks)
        self.last_w = {}
        self.readers = {}

    def emit(self, stack):
        nc = self.nc
        sems = {}
        for sid in sorted(self.semids):
            sems[sid] = stack.enter_context(nc.semaphore("s_" + sid.replace(".", "_")))
        block = stack.enter_context(nc.Block())

        def run(ename):
            def body(eng):
                for waits, fn, tok in self.ops[ename]:
                    for semid, val in waits.items():
                        eng.wait_ge(sems[semid], val)
                    if fn is None:
                        continue
                    inst = fn(eng)
                    semid, val, _ = tok
                    inst.then_inc(sems[semid], 16 if semid.startswith("q") else 1)
            return body

        block.tensor(run("pe"))
        block.scalar(run("act"))
        block.vector(run("dve"))
        block.gpsimd(run("pool"))
        block.sync(run("sp"))


class Rot:
    def __init__(self, items):
        self.items = items
        self.i = 0

    def next(self):
        it = self.items[self.i % len(self.items)]
        self.i += 1
        return it


def build_program(debug=False, phases=("pa", "pb", "rg", "dn", "tail", "moe"), dn_heads=tuple(range(8)), moe_experts=NE, tail_stop=99):
    nc = bass.Bass("TRN2", target_bir_lowering=False)
    dbg_kind = "ExternalOutput" if debug else "Internal"

    def din(name, shape, dt=F32):
        return nc.dram_tensor(name, list(shape), dt, kind="ExternalInput").ap()

    def dscr(name, shape, dt):
        return nc.dram_tensor(name, list(shape), dt, kind=dbg_kind).ap()

    x = din("x", [S_LAT, D])
    ctx = din("ctx", [S_CTX, D])
    cc = din("cc", [128, 8, 2])
    ada_w = din("ada_w", [D, 6 * D])
    ada_bT = din("ada_bT", [128, 48])
    ada_b = din("ada_b", [1, 6 * D])
    gpreT = din("gpreT", [128, 8])
    w_in = din("w_in", [D, 8224])
    rgp = din("rgp", [128, 8, 2, 8])
    rg_wa = din("rg_wa", [2, 16, 64, 64])
    rg_wi = din("rg_wi", [2, 16, 64, 64])
    dncw = din("dncw", [128, 24, 2, 4])
    dnc = din("dnc", [1, 32])
    dn_norm_g = din("dn_norm_g", [128, 1])
    gpost = din("gpost", [1, D])
    gfpre = din("gfpre", [1, D])
    gfpost = din("gfpost", [1, D])
    rg_w_o = din("rg_w_o", [D, D])
    dn_w_o = din("dn_w_o", [D, D])
    w_out = din("w_out", [D, D])
    router_w = din("router_w", [D, NE])
    router_b = din("router_b", [1, NE])
    e_w1 = din("e_w1", [NE, D, 2 * D])
    e_b1T = din("e_b1T", [128, NE, 16])
    e_w2 = din("e_w2", [NE, D, D])
    e_b2 = din("e_b2", [NE, D])
    out = nc.dram_tensor("out", [OWN, D], F32, kind="ExternalOutput").ap()

    PRG = dscr("PRG", [D, S_ALL], BF16)
    PQKV = dscr("PQKV", [3 * D, S_ALL], BF16)
    PAB = dscr("PAB", [S_ALL, 32], F32)
    GATES = dscr("GATES", [4 * D, OWN], BF16)
    XRG = dscr("XRG", [D, OWN], BF16)
    XDN = dscr("XDN", [D, OWN], BF16)
    XNEW = dscr("XNEW", [OWN, D], F32)

    with ExitStack() as st:
        E = st.enter_context
        S = Sched(nc)

        ARENA_WORDS = 52000
        arena_t = E(nc.sbuf_tensor("arena", [128, ARENA_WORDS], F32))
        arena = {"off": 0}

        def sb(name, shape, dt=F32):
            shape = list(shape)
            elems = int(np.prod(shape[1:]))
            words = elems if dt == F32 else (elems + 1) // 2
            words += words % 2
            off = arena["off"]
            assert off + words <= ARENA_WORDS, ("SBUF arena overflow", name, off, words)
            arena["off"] = off + words
            ap = arena_t[0:shape[0], off:off + words]
            if dt != F32:
                ap = ap.bitcast(dt)[:, 0:elems]
            else:
                ap = ap[:, 0:elems]
            if len(shape) == 3:
                ap = ap.rearrange("p (a b) -> p a b", a=shape[1])
            elif len(shape) == 4:
                ap = ap.rearrange("p (a b c) -> p a b c", a=shape[1], b=shape[2])
            return ap

        def ps(name):
            return E(nc.psum_tensor(name, [128, 512], F32))

        banks = [ps("bank%d" % i) for i in range(8)]

        def dbg(name, ap, reads):
            if not debug:
                return
            t = nc.dram_tensor("dbg_" + name, list(ap.shape), ap.dtype, kind="ExternalOutput").ap()
            S.op("sp", lambda e: e.dma_start(out=t, in_=ap), reads=reads, dma=True)

        ident = sb("ident", [128, 128])
        S.op("pool", lambda e: e.memset(ident[:], 0.0), writes=["ident"])
        S.op("pool", lambda e: e.affine_select(out=ident[:], in_=ident[:], pattern=[[-1, 128]],
                                               compare_op=ALU.not_equal, fill=1.0, base=0,
                                               channel_multiplier=1),
             reads=["ident"], writes=["ident"])

        cs = sb("cs", [128, 8, 2])
        modF = sb("modF", [128, 16, 2])
        Gm = sb("Gm", [128, 8, 2])
        Sh = sb("Sh", [128, 8, 2])
        adabT = sb("adabT", [128, 48])
        gpre = sb("gpre", [128, 8])
        S.op("sp", lambda e: e.dma_start(out=cs[:], in_=cc), writes=["cs"], dma=True)
        S.op("sp", lambda e: e.dma_start(out=adabT[:], in_=ada_bT), writes=["adabT"], dma=True)
        S.op("sp", lambda e: e.dma_start(out=gpre[:], in_=gpreT), writes=["gpre"], dma=True)
        S.op("act", lambda e: e.activation(out=cs[:], in_=cs[:], func=AF.Silu), reads=["cs"], writes=["cs"])
        mark0 = arena["off"]
        if True:
            adaw0 = sb("adaw0", [128, 8, 2048])
            for kc in range(8):
                S.op("sp", lambda e, kc=kc: e.dma_start(out=adaw0[:, kc, :], in_=ada_w[kc * 128:(kc + 1) * 128, 0:2048]),
                     writes=[("adaw0", kc)], dma=True)
            pm = banks[0]
            for j in range(16):
                for kc in range(8):
                    S.op("pe", lambda e, j=j, kc=kc: e.matmul(pm[:, 2 * j:2 * j + 2], lhsT=adaw0[:, kc, j * 128:(j + 1) * 128],
                                                              rhs=cs[:, kc, :], start=(kc == 0), stop=(kc == 7)),
                         reads=[("adaw0", kc), "cs"], writes=["bank0"])
            S.op("dve", lambda e: e.tensor_tensor(out=modF[:], in0=pm[:, 0:32].rearrange("p (j t) -> p j t", t=2),
                                                  in1=adabT[:, 0:16].unsqueeze(2).to_broadcast([128, 16, 2]), op=ALU.add),
                 reads=["bank0", "adabT"], writes=["modF"])
            S.op("dve", lambda e: e.tensor_scalar(out=Gm[:], in0=modF[:, 8:16, :], scalar1=1.0, scalar2=None, op0=ALU.add),
                 reads=["modF"], writes=["Gm"])
            S.op("dve", lambda e: e.tensor_tensor(out=Gm[:], in0=Gm[:], in1=gpre[:].unsqueeze(2).to_broadcast([128, 8, 2]), op=ALU.mult),
                 reads=["Gm", "gpre"], writes=["Gm"])
            S.op("dve", lambda e: e.tensor_copy(out=Sh[:], in_=modF[:, 0:8, :]), reads=["modF"], writes=["Sh"])
            S.barrier()
            arena["off"] = mark0

        def proj_pass(pname, Wb, wkey, blocks, chunk_fn, ab_cols=None):
            xts = Rot([(sb("%s_xt%d" % (pname, i), [128, D]), "%s_xt%d" % (pname, i)) for i in range(4)])
            xrs = [[(sb("%s_xr%d_%d" % (pname, b, i), [128, D]), "%s_xr%d_%d" % (pname, b, i)) for i in range(4)] for b in range(2)]
            hTs = [(sb("%s_hT%d" % (pname, i), [128, 8, 512], BF16), "%s_hT%d" % (pname, i)) for i in range(2)]
            junk = sb(pname + "_junk", [128, D], BF16)
            ssq = [(sb("%s_ss%d" % (pname, i), [128, 4]), "%s_ss%d" % (pname, i)) for i in range(2)]
            stg = Rot([(sb("%s_stg%d" % (pname, i), [128, 512], BF16), "%s_stg%d" % (pname, i)) for i in range(6)])
            tmp = Rot([(sb("%s_tmp%d" % (pname, i), [128, 512]), "%s_tmp%d" % (pname, i)) for i in range(4)])
            tb = Rot([(banks[0], "bank0"), (banks[1], "bank1")])
            pb = Rot([(banks[2 + i], "bank%d" % (2 + i)) for i in range(4)])
            abb = Rot([(banks[6], "bank6"), (banks[7], "bank7")])
            abst = Rot([(sb("%s_abst%d" % (pname, i), [128, 4, 32]), "%s_abst%d" % (pname, i)) for i in range(2)])
            evq = Rot(["act", "dve"])

            def stage1(bi):
                blk = blocks[bi]
                nt = blk["ntok"] // 128
                ss, ssk = ssq[bi % 2]
                xtl = []
                for j in range(nt):
                    xt, xk = xts.next()
                    for (ap, p0, npart) in blk["loads"][j]:
                        S.op("sp", lambda e, xt=xt, ap=ap, p0=p0, npart=npart: e.dma_start(out=xt[p0:p0 + npart, :], in_=ap),
                             writes=[xk], dma=True)
                    S.op("act", lambda e, xt=xt, ss=ss, j=j: e.activation(out=junk[:], in_=xt[:], func=AF.Square, accum_out=ss[:, j:j + 1]),
                         reads=[xk], writes=[pname + "_junk", ssk])
                    xtl.append((xt, xk))
                S.op("dve", lambda e, ss=ss, nt=nt: e.tensor_scalar(out=ss[:, 0:nt], in0=ss[:, 0:nt], scalar1=1.0 / D, scalar2=EPS,
                                                                   op0=ALU.mult, op1=ALU.add), reads=[ssk], writes=[ssk])
                S.op("act", lambda e, ss=ss, nt=nt: e.activation(out=ss[:, 0:nt], in_=ss[:, 0:nt], func=AF.Sqrt), reads=[ssk], writes=[ssk])
                S.op("dve", lambda e, ss=ss, nt=nt: e.reciprocal(out=ss[:, 0:nt], in_=ss[:, 0:nt]), reads=[ssk], writes=[ssk])
                xrl = xrs[bi % 2]
                for j in range(nt):
                    xt, xk = xtl[j]
                    xr, xrk = xrl[j]
                    S.op("pool", lambda e, xr=xr, xt=xt, ss=ss, j=j: e.tensor_scalar(out=xr[:], in0=xt[:], scalar1=ss[:, j:j + 1], scalar2=0.0,
                                                                                    op0=ALU.mult, op1=ALU.add), reads=[xk, ssk], writes=[xrk])
                hT, hk = hTs[bi % 2]
                m = blk["mod"]
                for c in range(8):
                    bk, bkk = tb.next()
                    for j in range(nt):
                        xr, xrk = xrl[j]
                        S.op("pe", lambda e, bk=bk, xr=xr, c=c, j=j: e.transpose(out=bk[:, j * 128:(j + 1) * 128], in_=xr[:, c * 128:(c + 1) * 128],
                                                                                 identity=ident[:]), reads=[xrk, "ident"], writes=[bkk])
                    S.op("act", lambda e, bk=bk, hT=hT, c=c, nt=nt, m=m: e.activation(out=hT[:, c, 0:nt * 128], in_=bk[:, 0:nt * 128], func=AF.Identity,
                                                                                   scale=Gm[:, c, m:m + 1], bias=Sh[:, c, m:m + 1]),
                         reads=[bkk, "Gm", "Sh"], writes=[(hk, c)])

            def stage2(bi):
                blk = blocks[bi]
                ntok = blk["ntok"]
                hT, hk = hTs[bi % 2]
                for spec in chunk_fn(blk):
                    col0, kind, dst = spec
                    bk, bkk = pb.next()
                    for kc in range(8):
                        S.op("pe", lambda e, bk=bk, kc=kc, col0=col0: e.matmul(bk[:, 0:ntok], lhsT=Wb[:, kc, col0:col0 + 128], rhs=hT[:, kc, 0:ntok],
                                                                               start=(kc == 0), stop=(kc == 7)),
                             reads=[(wkey, kc), (hk, kc)], writes=[bkk])
                    sg, sgk = stg.next()
                    if kind == "copy":
                        q = evq.next()
                        if q == "act":
                            S.op("act", lambda e, sg=sg, bk=bk: e.activation(out=sg[:, 0:ntok], in_=bk[:, 0:ntok], func=AF.Identity),
                                 reads=[bkk], writes=[sgk])
                        else:
                            S.op("dve", lambda e, sg=sg, bk=bk: e.tensor_copy(out=sg[:, 0:ntok], in_=bk[:, 0:ntok]), reads=[bkk], writes=[sgk])
                    elif kind == "silu":
                        S.op("act", lambda e, sg=sg, bk=bk: e.activation(out=sg[:, 0:ntok], in_=bk[:, 0:ntok], func=AF.Silu), reads=[bkk], writes=[sgk])
                    elif kind == "sigmoid":
                        S.op("act", lambda e, sg=sg, bk=bk: e.activation(out=sg[:, 0:ntok], in_=bk[:, 0:ntok], func=AF.Sigmoid), reads=[bkk], writes=[sgk])
                    elif kind == "gelu":
                        t1, t1k = tmp.next()
                        S.op("act", lambda e, t1=t1, bk=bk: e.activation(out=t1[:, 0:ntok], in_=bk[:, 0:ntok], func=AF.Square), reads=[bkk], writes=[t1k])
                        S.op("dve", lambda e, t1=t1: e.tensor_scalar(out=t1[:, 0:ntok], in0=t1[:, 0:ntok], scalar1=0.044715, scalar2=1.0,
                                                                    op0=ALU.mult, op1=ALU.add), reads=[t1k], writes=[t1k])
                        S.op("dve", lambda e, t1=t1, bk=bk: e.tensor_tensor(out=t1[:, 0:ntok], in0=t1[:, 0:ntok], in1=bk[:, 0:ntok], op=ALU.mult),
                             reads=[t1k, bkk], writes=[t1k])
                        S.op("act", lambda e, t1=t1: e.activation(out=t1[:, 0:ntok], in_=t1[:, 0:ntok], func=AF.Sigmoid, scale=1.5957691216057308),
                             reads=[t1k], writes=[t1k])
                        S.op("dve", lambda e, t1=t1, bk=bk, sg=sg: e.tensor_tensor(out=sg[:, 0:ntok], in0=t1[:, 0:ntok], in1=bk[:, 0:ntok], op=ALU.mult),
                             reads=[t1k, bkk], writes=[sgk])
                    S.op("sp", lambda e, sg=sg, dst=dst: e.dma_start(out=dst, in_=sg[:, 0:ntok]), reads=[sgk], dma=True)
                if ab_cols is not None:
                    nt = ntok // 128
                    bk, bkk = abb.next()
                    for j in range(nt):
                        for kc in range(8):
                            S.op("pe", lambda e, bk=bk, kc=kc, j=j: e.matmul(bk[:, j * 32:(j + 1) * 32], lhsT=hT[:, kc, j * 128:(j + 1) * 128],
                                                                             rhs=Wb[:, kc, ab_cols:ab_cols + 32], start=(kc == 0), stop=(kc == 7)),
                                 reads=[(wkey, kc), (hk, kc)], writes=[bkk])
                    ab, abk = abst.next()
                    S.op("dve", lambda e, ab=ab, bk=bk, nt=nt: e.tensor_copy(out=ab[:, 0:nt, :], in_=bk[:, 0:nt * 32].rearrange("p (j c) -> p j c", c=32)),
                         reads=[bkk], writes=[abk])
                    t0 = blk["tokoff"]
                    S.op("sp", lambda e, ab=ab, nt=nt, t0=t0: e.dma_start(out=PAB[t0:t0 + nt * 128, :].rearrange("(j p) c -> p j c", p=128),
                                                                         in_=ab[:, 0:nt, :]), reads=[abk], dma=True)

            stage1(0)
            for bi in range(len(blocks)):
                if bi + 1 < len(blocks):
                    stage1(bi + 1)
                stage2(bi)

        WA = sb("WA", [128, 8, 5120], BF16)
        srcA = [(0, 0, 1024), (1024, 1024, 1024), (2048, 5120, 1024), (3072, 6176, 1024), (4096, 7200, 1024)]
        for (d0, s0, n) in srcA:
            for kc in range(8):
                S.op("pool", lambda e, kc=kc, d0=d0, s0=s0, n=n: e.dma_start(out=WA[:, kc, d0:d0 + n], in_=w_in[kc * 128:(kc + 1) * 128, s0:s0 + n]),
                     writes=[("WA", kc)], dma=True)
        blocksA = [dict(ntok=256, mod=1, tokoff=0, own=False,
                        loads=[[(ctx[j * 128:(j + 1) * 128, :], 0, 128)] for j in range(2)])]
        for bi in range(8):
            blocksA.append(dict(ntok=512, mod=0, tokoff=S_CTX + bi * 512, own=(bi < 4), lat0=bi * 512,
                                loads=[[(x[bi * 512 + j * 128: bi * 512 + (j + 1) * 128, :], 0, 128)] for j in range(4)]))

        def chunksA(blk):
            ntok, t0 = blk["ntok"], blk["tokoff"]
            specs = [(c * 128, "copy", PRG[c * 128:(c + 1) * 128, t0:t0 + ntok]) for c in range(8)]
            if blk["own"]:
                l0 = blk["lat0"]
                for gi, kind in enumerate(["gelu", "silu", "sigmoid", "sigmoid"]):
                    for c in range(8):
                        specs.append((1024 + gi * 1024 + c * 128, kind, GATES[gi * 1024 + c * 128: gi * 1024 + (c + 1) * 128, l0:l0 + ntok]))
            return specs

        if "pa" in phases:
            proj_pass("pa", WA, "WA", blocksA, chunksA)
        S.barrier()
        arena["off"] = mark0

        WB = sb("WB", [128, 8, 3104], BF16)
        srcB = [(0, 2048, 1024), (1024, 3072, 1024), (2048, 4096, 1024), (3072, 6144, 32)]
        for (d0, s0, n) in srcB:
            for kc in range(8):
                S.op("pool", lambda e, kc=kc, d0=d0, s0=s0, n=n: e.dma_start(out=WB[:, kc, d0:d0 + n], in_=w_in[kc * 128:(kc + 1) * 128, s0:s0 + n]),
                     writes=[("WB", kc)], dma=True)
        xcm = x.rearrange("(r c) d -> c r d", c=64)
        blocksB = [dict(ntok=256, mod=1, tokoff=0,
                        loads=[[(ctx[j * 128:(j + 1) * 128, :], 0, 128)] for j in range(2)])]
        for bi in range(8):
            blocksB.append(dict(ntok=512, mod=0, tokoff=S_CTX + bi * 512,
                                loads=[[(xcm[bi * 8 + j * 2 + h], h * 64, 64) for h in range(2)] for j in range(4)]))

        def chunksB(blk):
            ntok, t0 = blk["ntok"], blk["tokoff"]
            return [(c * 128, "copy", PQKV[c * 128:(c + 1) * 128, t0:t0 + ntok]) for c in range(24)]

        if "pb" in phases:
            proj_pass("pb", WB, "WB", blocksB, chunksB, ab_cols=3072)

        S.barrier()
        arena["off"] = mark0

        if "rg" in phases:
            XW = 4 + S_CTX + 4 + S_LAT + 4
            rgp_t = sb("rgp_t", [128, 8, 2, 8])
            S.op("sp", lambda e: e.dma_start(out=rgp_t[:], in_=rgp), writes=["rgp"], dma=True)
            c8 = sb("c8", [128, 8, 2])
            S.op("act", lambda e: e.activation(out=c8[:], in_=rgp_t[:, :, :, 7], func=AF.Exp, scale=-1.0), reads=["rgp"], writes=["c8"])
            S.op("act", lambda e: e.activation(out=c8[:], in_=c8[:], func=AF.Ln, bias=1.0), reads=["c8"], writes=["c8"])
            S.op("dve", lambda e: e.tensor_scalar(out=c8[:], in0=c8[:], scalar1=-8.0, scalar2=None, op0=ALU.mult), reads=["c8"], writes=["c8"])
            NSET = 2
            sets = []
            for si in range(NSET):
                sets.append(dict(
                    xc=sb("rg_xc%d" % si, [128, S_ALL]), ra=sb("rg_ra%d" % si, [128, S_ALL]),
                    ib=sb("rg_ib%d" % si, [128, S_ALL]), mh=sb("rg_mh%d" % si, [128, S_ALL]),
                    wa=sb("rg_wa%d" % si, [128, 128]), wi=sb("rg_wi%d" % si, [128, 128]), k="rgs%d" % si))
            xps = [(sb("rg_xp%d" % i, [128, XW], BF16), "rg_xp%d" % i) for i in range(2)]
            gts = [(sb("rg_gt%d" % i, [128, OWN], BF16), "rg_gt%d" % i) for i in range(2)]
            accs = [(sb("rg_acc%d" % i, [128, OWN]), "rg_acc%d" % i) for i in range(2)]
            xos = [(sb("rg_xo%d" % i, [128, OWN], BF16), "rg_xo%d" % i) for i in range(2)]
            pbk = Rot([(banks[i], "bank%d" % i) for i in range(8)])
            def rg_one(g, d, st_, xp, xpk, gt, gtk, acc, acck, xo, xok):
                sk = st_["k"]
                xc, ra, ib, mh, wa_t, wi_t = st_["xc"], st_["ra"], st_["ib"], st_["mh"], st_["wa"], st_["wi"]
                prm = rgp_t[:, g, d, :]
                for wt, src, wk in ((wa_t, rg_wa, sk + "wa"), (wi_t, rg_wi, sk + "wi")):
                    S.op("pool", lambda e, wt=wt: e.memset(wt[:], 0.0), writes=[wk])
                    for h in range(2):
                        S.op("sp", lambda e, wt=wt, src=src, h=h, g=g, d=d: e.dma_start(out=wt[h * 64:(h + 1) * 64, h * 64:(h + 1) * 64],
                                                                                   in_=src[d, 2 * g + h]), writes=[wk], dma=True)
                V = xp if d == 0 else xp[:, ::-1]
                ctx_off, lat_off = (4, 264) if d == 0 else (4104, 4)
                n_lat = OWN if d == 0 else S_LAT
                segs = [(ctx_off, 0, S_CTX)] + [(lat_off + i * 512, S_CTX + i * 512, 512) for i in range(n_lat // 512)]
                ntot = S_CTX + n_lat
                for (vo, o, n) in segs:
                    kx = (sk + "xc", o)
                    S.op("dve", lambda e, vo=vo, o=o, n=n: e.tensor_scalar(out=xc[:, o:o + n], in0=V[:, vo:vo + n], scalar1=prm[:, 3:4], scalar2=prm[:, 4:5],
                                                                          op0=ALU.mult, op1=ALU.add), reads=[xpk, "rgp"], writes=[kx])
                    for j in (2, 1, 0):
                        sh = 3 - j
                        S.op("dve", lambda e, vo=vo, o=o, n=n, j=j, sh=sh: e.scalar_tensor_tensor(out=xc[:, o:o + n], in0=V[:, vo - sh:vo - sh + n], scalar=prm[:, j:j + 1],
                                                                                                  in1=xc[:, o:o + n], op0=ALU.mult, op1=ALU.add),
                             reads=[xpk, "rgp", kx], writes=[kx])
                    b1, b1k = pbk.next()
                    b2, b2k = pbk.next()
                    S.op("pe", lambda e, b1=b1, o=o, n=n: e.matmul(b1[:, 0:n], lhsT=wa_t[:], rhs=xc[:, o:o + n], start=True, stop=True),
                         reads=[sk + "wa", kx], writes=[b1k])
                    S.op("pe", lambda e, b2=b2, o=o, n=n: e.matmul(b2[:, 0:n], lhsT=wi_t[:], rhs=xc[:, o:o + n], start=True, stop=True),
                         reads=[sk + "wi", kx], writes=[b2k])
                    S.op("act", lambda e, b1=b1, o=o, n=n: e.activation(out=ra[:, o:o + n], in_=b1[:, 0:n], func=AF.Sigmoid, bias=prm[:, 5:6]),
                         reads=[b1k, "rgp"], writes=[(sk + "ra", o)])
                    S.op("act", lambda e, b2=b2, o=o, n=n: e.activation(out=ib[:, o:o + n], in_=b2[:, 0:n], func=AF.Sigmoid, bias=prm[:, 6:7]),
                         reads=[b2k, "rgp"], writes=[(sk + "ib", o)])
                allr = [(sk + "ra", o) for (_, o, _) in segs]
                alli = [(sk + "ib", o) for (_, o, _) in segs]
                allx = [(sk + "xc", o) for (_, o, _) in segs]
                allm = [sk + "mh"]
                S.op("act", lambda e, g=g, d=d: e.activation(out=ra[:, 0:ntot], in_=ra[:, 0:ntot], func=AF.Exp, scale=c8[:, g, d:d + 1]),
                     reads=allr + ["c8"], writes=allr)
                S.op("pool", lambda e: e.tensor_tensor(out=mh[:, 0:ntot], in0=ra[:, 0:ntot], in1=ra[:, 0:ntot], op=ALU.mult),
                     reads=allr, writes=allm)
                S.op("act", lambda e: e.activation(out=mh[:, 0:ntot], in_=mh[:, 0:ntot], func=AF.Sqrt, scale=-1.0, bias=1.0),
                     reads=allm, writes=allm)
                S.op("pool", lambda e: e.memset(mh[:, 0:1], 1.0), reads=allm, writes=allm)
                S.op("pool", lambda e: e.tensor_tensor(out=ib[:, 0:ntot], in0=ib[:, 0:ntot], in1=mh[:, 0:ntot], op=ALU.mult),
                     reads=alli + allm, writes=alli)
                S.op("pool", lambda e: e.tensor_tensor(out=ib[:, 0:ntot], in0=ib[:, 0:ntot], in1=xc[:, 0:ntot], op=ALU.mult),
                     reads=alli + allx, writes=alli)
                S.op("dve", lambda e: e.tensor_tensor_scan(out=mh[:, 0:S_CTX], data0=ra[:, 0:S_CTX], data1=ib[:, 0:S_CTX], initial=0.0,
                                                           op0=ALU.mult, op1=ALU.add), reads=allr + alli + allm, writes=allm)
                for o in range(S_CTX, ntot, 1024):
                    S.op("dve", lambda e, o=o: e.tensor_tensor_scan(out=mh[:, o:o + 1024], data0=ra[:, o:o + 1024], data1=ib[:, o:o + 1024],
                                                                    initial=mh[:, o - 1:o], op0=ALU.mult, op1=ALU.add),
                         reads=allr + alli + allm, writes=allm)
                if g == 0:
                    if d == 0:
                        dbg("xp", xp[:, :], [xpk])
                    dbg("xc%d" % d, xc[:, 0:ntot], allx)
                    dbg("a%d" % d, ra[:, 0:ntot], allr)
                    dbg("b%d" % d, ib[:, 0:ntot], alli)
                    dbg("h%d" % d, mh[:, 0:ntot], allm)
                    dbg("c8_%d" % d, c8[:, 0, :], ["c8"])
                if d == 0:
                    S.op("pool", lambda e, acc=acc: e.tensor_copy(out=acc[:], in_=mh[:, S_CTX:S_CTX + OWN]), reads=allm, writes=[acck])
                else:
                    S.op("pool", lambda e, acc=acc: e.tensor_tensor(out=acc[:], in0=acc[:], in1=mh[:, S_CTX + OWN:S_ALL][:, ::-1], op=ALU.add),
                         reads=allm + [acck], writes=[acck])
                    S.op("pool", lambda e, acc=acc, gt=gt, xo=xo: e.tensor_tensor(out=xo[:], in0=acc[:], in1=gt[:], op=ALU.mult),
                         reads=[acck, gtk], writes=[xok])
                    S.op("sp", lambda e, xo=xo, g=g: e.dma_start(out=XRG[g * 128:(g + 1) * 128, :], in_=xo[:]), reads=[xok], dma=True)

            seti = 0
            for g in range(8):
                xp, xpk = xps[g % 2]
                gt, gtk = gts[g % 2]
                acc, acck = accs[g % 2]
                xo, xok = xos[g % 2]
                S.op("pool", lambda e, xp=xp: e.memset(xp[:, 0:4], 0.0), writes=[xpk])
                S.op("pool", lambda e, xp=xp: e.memset(xp[:, 260:264], 0.0), writes=[xpk])
                S.op("pool", lambda e, xp=xp: e.memset(xp[:, XW - 4:XW], 0.0), writes=[xpk])
                S.op("sp", lambda e, xp=xp, g=g: e.dma_start(out=xp[:, 4:260], in_=PRG[g * 128:(g + 1) * 128, 0:S_CTX]), writes=[xpk], dma=True)
                S.op("sp", lambda e, xp=xp, g=g: e.dma_start(out=xp[:, 264:264 + S_LAT], in_=PRG[g * 128:(g + 1) * 128, S_CTX:S_ALL]), writes=[xpk], dma=True)
                S.op("sp", lambda e, gt=gt, g=g: e.dma_start(out=gt[:], in_=GATES[g * 128:(g + 1) * 128, :]), writes=[gtk], dma=True)
                for d in range(2):
                    rg_one(g, d, sets[seti % NSET], xp, xpk, gt, gtk, acc, acck, xo, xok)
                    seti += 1
            S.barrier()
            arena["off"] = mark0

        def OP(eng, method, reads, writes, **kw):
            S.op(eng, lambda e: getattr(e, method)(**kw), reads=reads, writes=writes)

        def DMA(outap, inap, reads=(), writes=()):
            S.op("sp", lambda e: e.dma_start(out=outap, in_=inap), reads=reads, writes=writes, dma=True)

        class TP:
            def __init__(self, name, n, shape, dt):
                self.t = [(sb("%s%d" % (name, i), shape, dt), "%s%d" % (name, i)) for i in range(n)]
                self.i = 0

            def get(self):
                r = self.t[self.i % len(self.t)]
                self.i += 1
                return r

        if "dn" in phases:
            NEG = -30000.0
            XW = 4 + S_CTX + 4 + S_LAT + 4
            onesb = sb("onesb", [128, 128], BF16)
            onesf = sb("onesf", [128, 128])
            identb = sb("identb", [128, 128], BF16)
            LOW = sb("LOW", [128, 128])
            UPI = sb("UPI", [128, 128])
            NEGL = sb("NEGL", [128, 128])
            NEGU = sb("NEGU", [128, 128])
            H0 = sb("H0", [128, 128])
            H1 = sb("H1", [128, 128])
            OP("pool", "memset", [], ["onesb"], ap=onesb[:], constant=1.0)
            OP("pool", "memset", [], ["onesf"], ap=onesf[:], constant=1.0)
            OP("pool", "tensor_copy", ["ident"], ["identb"], out=identb[:], in_=ident[:])
            OP("pool", "affine_select", ["onesf"], ["LOW"], out=LOW[:], in_=onesf[:], pattern=[[-1, 128]], compare_op=ALU.is_gt, fill=0.0,
               base=0, channel_multiplier=1)
            OP("pool", "memset", ["LOW"], ["LOW"], ap=LOW[64:128, 0:64], constant=0.0)
            OP("pool", "affine_select", ["onesf"], ["UPI"], out=UPI[:], in_=onesf[:], pattern=[[1, 128]], compare_op=ALU.is_ge, fill=0.0,
               base=0, channel_multiplier=-1)
            OP("pool", "memset", ["UPI"], ["UPI"], ap=UPI[0:64, 64:128], constant=0.0)
            OP("dve", "tensor_scalar", ["LOW"], ["NEGL"], out=NEGL[:], in0=LOW[:], scalar1=-1.0, scalar2=-NEG, op0=ALU.add, op1=ALU.mult)
            OP("dve", "tensor_scalar", ["UPI"], ["NEGU"], out=NEGU[:], in0=UPI[:], scalar1=-1.0, scalar2=-NEG, op0=ALU.add, op1=ALU.mult)
            OP("pool", "memset", [], ["H0"], ap=H0[:], constant=0.0)
            OP("pool", "memset", ["H0"], ["H0"], ap=H0[0:64, :], constant=1.0)
            OP("pool", "memset", [], ["H1"], ap=H1[:], constant=0.0)
            OP("pool", "memset", ["H1"], ["H1"], ap=H1[64:128, :], constant=1.0)
            dncw_t = sb("dncw_t", [128, 24, 2, 4])
            dnc_t = sb("dnc_t", [128, 32])
            gnorm = sb("gnorm", [128, 1])
            negA = sb("negA", [128, 16])
            DMA(dncw_t[:], dncw, writes=["dncw"])
            DMA(dnc_t[:], dnc.partition_broadcast(128), writes=["dnc"])
            DMA(gnorm[:], dn_norm_g, writes=["gnorm"])
            OP("act", "activation", ["dnc"], ["negA"], out=negA[:], in_=dnc_t[:, 0:16], func=AF.Exp)
            OP("dve", "tensor_scalar", ["negA"], ["negA"], out=negA[:], in0=negA[:], scalar1=-1.0, scalar2=None, op0=ALU.mult)

            NP = S_ALL // 128
            GCt, Btt, EGLt, EGRt, BEGt = [], [], [], [], []
            with_tmp = arena["off"]
            ABt = sb("ABt", [128, NP, 32])
            ABr = sb("ABr", [128, NP, 32])
            Jm = sb("Jm", [128, 128])
            OP("pool", "memset", [], ["Jm"], ap=Jm[:], constant=0.0)
            OP("pool", "affine_select", ["Jm"], ["Jm"], out=Jm[:], in_=Jm[:], pattern=[[1, 128]], compare_op=ALU.not_equal, fill=1.0,
               base=-127, channel_multiplier=1)
            Zt = sb("Zt", [128, NP, 8])
            Lt = sb("Lt", [128, NP, 8])
            Gt = sb("Gt", [128, NP, 8])
            GLtok = sb("GLtok", [128, NP, 8])
            for d in range(2):
                GC = sb("GC%d" % d, [128, NP, 8]); Bt = sb("Bt%d" % d, [128, NP, 8]); EGL = sb("EGL%d" % d, [128, NP, 2, 8])
                EGR = sb("EGR%d" % d, [128, NP, 8]); BEG = sb("BEG%d" % d, [128, NP, 8])
                GCt.append(GC); Btt.append(Bt); EGLt.append(EGL); EGRt.append(EGR); BEGt.append(BEG)
            mark_dn = arena["off"]
            for d in range(2):
                GC, Bt, EGL, EGR, BEG = GCt[d], Btt[d], EGLt[d], EGRt[d], BEGt[d]
                k = "sc%d" % d
                if d == 0:
                    DMA(ABt[:, 0:2, :], PAB[0:S_CTX, :].rearrange("(n p) c -> p n c", p=128), writes=["ABt"])
                    DMA(ABt[:, 2:NP, :], PAB[S_CTX:S_ALL, :].rearrange("(n p) c -> p n c", p=128), writes=["ABt"])
                    ABs, ABk = ABt, "ABt"
                else:
                    ABf = ABt[:].rearrange("p n c -> p (n c)")
                    for bi_, (s0, s1) in enumerate(((0, 16), (16, 32), (32, 34))):
                        OP("pe", "matmul", ["Jm", "ABt"], ["bank%d" % (3 + bi_)], out=banks[3 + bi_][:, 0:(s1 - s0) * 32], lhsT=Jm[:], rhs=ABf[:, s0 * 32:s1 * 32],
                           start=True, stop=True)
                    b3v = banks[3][:, 0:512].rearrange("p (n c) -> p n c", c=32)
                    b4v = banks[4][:, 0:512].rearrange("p (n c) -> p n c", c=32)
                    b5v = banks[5][:, 0:64].rearrange("p (n c) -> p n c", c=32)
                    OP("dve", "tensor_copy", ["bank3"], ["ABr"], out=ABr[:, 0:2, :], in_=b3v[:, 0:2, :][:, ::-1, :])
                    OP("dve", "tensor_copy", ["bank3"], ["ABr"], out=ABr[:, 20:34, :], in_=b3v[:, 2:16, :][:, ::-1, :])
                    OP("dve", "tensor_copy", ["bank4"], ["ABr"], out=ABr[:, 4:20, :], in_=b4v[:, 0:16, :][:, ::-1, :])
                    OP("dve", "tensor_copy", ["bank5"], ["ABr"], out=ABr[:, 2:4, :], in_=b5v[:, 0:2, :][:, ::-1, :])
                    ABs, ABk = ABr, "ABr"
                dtb = dnc_t[:, 16 + d * 8:24 + d * 8].unsqueeze(1).to_broadcast([128, NP, 8])
                nab = negA[:, d * 8:(d + 1) * 8].unsqueeze(1).to_broadcast([128, NP, 8])
                OP("dve", "tensor_tensor", [ABk, "dnc"], ["Zt"], out=Zt[:], in0=ABs[:, :, d * 8:(d + 1) * 8], in1=dtb, op=ALU.add)
                OP("dve", "scalar_tensor_tensor", ["Zt"], ["Lt"], out=Lt[:].rearrange("p n c -> p (n c)"), in0=Zt[:].rearrange("p n c -> p (n c)"), scalar=-1.0,
                   in1=Zt[:].rearrange("p n c -> p (n c)"), op0=ALU.mult, op1=ALU.max)
                OP("act", "activation", ["Lt"], ["Lt"], out=Lt[:], in_=Lt[:], func=AF.Exp, scale=-1.0)
                OP("act", "activation", ["Lt"], ["Lt"], out=Lt[:], in_=Lt[:], func=AF.Ln, bias=1.0)
                OP("dve", "scalar_tensor_tensor", ["Zt", "Lt"], ["Gt"], out=Gt[:].rearrange("p n c -> p (n c)"), in0=Zt[:].rearrange("p n c -> p (n c)"),
                   scalar=0.0, in1=Lt[:].rearrange("p n c -> p (n c)"), op0=ALU.max, op1=ALU.add)
                OP("dve", "tensor_tensor", ["Gt", "negA"], ["Gt"], out=Gt[:], in0=Gt[:], in1=nab, op=ALU.mult)
                OP("act", "activation", [ABk], [k + "B"], out=Bt[:], in_=ABs[:, :, 16 + d * 8:24 + d * 8], func=AF.Sigmoid)
                Gf = Gt[:].rearrange("p n c -> p (n c)")
                W = NP * 8
                OP("pe", "matmul", ["UPI", "Gt"], ["bank0"], out=banks[0][:, 0:W], lhsT=UPI[:], rhs=Gf, start=True, stop=True)
                OP("pe", "matmul", ["H0", "Gt"], ["bank1"], out=banks[1][:, 0:W], lhsT=H0[:], rhs=Gf, start=True, stop=True)
                OP("pe", "matmul", ["H1", "Gt"], ["bank2"], out=banks[2][:, 0:W], lhsT=H1[:], rhs=Gf, start=True, stop=True)
                OP("dve", "tensor_copy", ["bank0"], [k + "GC"], out=GC[:].rearrange("p n c -> p (n c)"), in_=banks[0][:, 0:W])
                OP("act", "activation", ["bank1"], [k + "EGL"], out=EGL[:, :, 0, :], in_=banks[1][:, 0:W].rearrange("p (n c) -> p n c", c=8), func=AF.Exp)
                OP("act", "activation", ["bank2"], [k + "EGL"], out=EGL[:, :, 1, :], in_=banks[2][:, 0:W].rearrange("p (n c) -> p n c", c=8), func=AF.Exp)
                OP("dve", "tensor_copy", ["bank1"], ["GLtok"], out=GLtok[0:64].rearrange("p n c -> p (n c)"), in_=banks[1][0:64, 0:W])
                OP("dve", "tensor_copy", ["bank2", "GLtok"], ["GLtok"], out=GLtok[64:128].rearrange("p n c -> p (n c)"), in_=banks[2][64:128, 0:W])
                OP("dve", "tensor_tensor", ["GLtok", k + "GC"], ["GLtok"], out=GLtok[:], in0=GLtok[:], in1=GC[:], op=ALU.subtract)
                OP("act", "activation", ["GLtok"], [k + "EGR"], out=EGR[:], in_=GLtok[:], func=AF.Exp)
                OP("act", "activation", [k + "GC"], [k + "BEG"], out=BEG[:], in_=GC[:], func=AF.Exp)
                OP("dve", "tensor_tensor", [k + "BEG", k + "B"], [k + "BEG"], out=BEG[:], in0=BEG[:], in1=Bt[:], op=ALU.mult)
            S.barrier()
            arena["off"] = mark_dn

            xin = [[(sb("dn_x%d_%d" % (par, part), [128, XW], BF16), "dn_x%d_%d" % (par, part)) for part in range(3)] for par in range(1)]
            szs = [(sb("dn_sz%d" % i, [128, OWN], BF16), "dn_sz%d" % i) for i in range(2)]
            Obuf = [[(sb("dn_O%d_%d" % (par, d), [128, 64, 32]), "dn_O%d_%d" % (par, d)) for d in range(2)] for par in range(1)]
            xos = [(sb("dn_xo%d" % i, [128, OWN], BF16), "dn_xo%d" % i) for i in range(2)]
            F32Pd = [TP("dn_f%d" % d, 7, [128, 512], F32) for d in range(2)]
            BFPd = [TP("dn_b%d" % d, 16, [128, 512], BF16) for d in range(2)]
            VNP = [TP("dn_vn%d" % d, 4, [128, 128], BF16) for d in range(2)]
            h32s = [[sb("dn_h32_%d%d" % (hp, d), [128, 128]) for d in range(2)] for hp in range(2)]
            hbfs = [[[sb("dn_hbf%d%d_%d" % (hp, d, i), [128, 128], BF16) for i in range(2)] for d in range(2)] for hp in range(2)]
            pers = {}
            for d in range(2):
                for par in range(2):
                    pers[(d, par)] = dict(
                        qe=sb("dn_qe%d%d" % (d, par), [128, 512], BF16), qkt=sb("dn_qkt%d%d" % (d, par), [128, 4, 128], BF16),
                        u=sb("dn_u%d%d" % (d, par), [128, 4, 128]), wT=sb("dn_wT%d%d" % (d, par), [128, 512], BF16),
                        kd=sb("dn_kd%d%d" % (d, par), [128, 4, 128], BF16), k="dnp%d%d" % (d, par),
                        qn=sb("dn_qn%d%d" % (d, par), [128, 512], BF16), kn=sb("dn_kn%d%d" % (d, par), [128, 512], BF16),
                        vT=sb("dn_vT%d%d" % (d, par), [128, 512], BF16))
            prot = TP.__new__(TP)
            prot.t = [(banks[i], "bank%d" % i) for i in range(6)]
            prot.i = 0
            banks_bf = [bk.bitcast(BF16) for bk in banks]
            LNQ = -0.5 * float(np.log(128.0))

            tcount = {"t": 0}

            def run_rr(gens):
                gens = list(gens)
                while gens:
                    for g_ in list(gens):
                        try:
                            next(g_)
                        except StopIteration:
                            gens.remove(g_)

            def dn_head(hd, prev):
                par = hd % 2
                h32 = h32s[par]
                hbf = hbfs[par]
                xb = xin[0]
                sz, szk = szs[par]
                xo, xok = xos[par]
                for part in range(3):
                    xp, xpk = xb[part]
                    r0 = part * 1024 + hd * 128
                    OP("pool", "memset", [], [xpk], ap=xp[:, 0:4], constant=0.0)
                    OP("pool", "memset", [], [xpk], ap=xp[:, 260:264], constant=0.0)
                    OP("pool", "memset", [], [xpk], ap=xp[:, XW - 4:XW], constant=0.0)
                    DMA(xp[:, 4:260], PQKV[r0:r0 + 128, 0:S_CTX], writes=[xpk])
                    DMA(xp[:, 264:264 + S_LAT], PQKV[r0:r0 + 128, S_CTX:S_ALL], writes=[xpk])
                DMA(sz[:], GATES[1024 + hd * 128:1024 + (hd + 1) * 128, :], writes=[szk])
                hstate = {}
                for d in range(2):
                    OP("pool", "memset", [], [("h32", par, d)], ap=h32[d][:], constant=0.0)
                    OP("pool", "memset", [], [("hbf", par, d, 0)], ap=hbf[d][0][:], constant=0.0)
                    hstate[d] = 0

                groups = [(0, 256)] + [(S_CTX + i * 512, 512) for i in range(8)]

                def prep(d, gi, tpar):
                    F32P, BFP = F32Pd[d], BFPd[d]
                    o, n = groups[gi]
                    npair = n // 128
                    p0 = o // 128
                    P = pers[(d, tpar)]
                    pk = P["k"]
                    GC, Bt, EGR, BEG = GCt[d], Btt[d], EGRt[d], BEGt[d]
                    ctx_off, lat_off = (4, 264) if d == 0 else (4104, 4)
                    base = (ctx_off + o) if o < S_CTX else (lat_off + o - S_CTX)
                    res = {}
                    for part in range(3):
                        xp, xpk = xb[part]
                        V = xp if d == 0 else xp[:, ::-1]
                        wv = dncw_t[:, part * 8 + hd, d, :]
                        T1, T1k = F32P.get()
                        OP("dve", "tensor_scalar", [xpk, "dncw"], [T1k], out=T1[:, 0:n], in0=V[:, base:base + n], scalar1=wv[:, 3:4], scalar2=None, op0=ALU.mult)
                        for j in (2, 1, 0):
                            sh = 3 - j
                            OP("dve", "scalar_tensor_tensor", [xpk, "dncw", T1k], [T1k], out=T1[:, 0:n], in0=V[:, base - sh:base - sh + n],
                               scalar=wv[:, j:j + 1], in1=T1[:, 0:n], op0=ALU.mult, op1=ALU.add)
                        yield
                        if part == 2:
                            vT, vTk = P["vT"], (pk, "vT")
                            OP("act", "activation", [T1k], [vTk], out=vT[:, 0:n], in_=T1[:, 0:n], func=AF.Silu)
                            res["v"] = (vT, vTk)
                            continue
                        T2, T2k = F32P.get()
                        OP("act", "activation", [T1k], [T2k], out=T2[:, 0:n], in_=T1[:, 0:n], func=AF.Silu)
                        SQ, SQk = BFP.get()
                        OP("pool", "tensor_tensor", [T2k], [SQk], out=SQ[:, 0:n], in0=T2[:, 0:n], in1=T2[:, 0:n], op=ALU.mult)
                        bk, bkk = prot.get()
                        OP("pe", "matmul", ["onesb", SQk], [bkk], out=bk[:, 0:n], lhsT=onesb[:], rhs=SQ[:, 0:n], start=True, stop=True)
                        yield
                        RS, RSk = F32P.get()
                        OP("act", "activation", [bkk], [RSk], out=RS[:, 0:n], in_=bk[:, 0:n], func=AF.Ln, bias=EPS)
                        OP("act", "activation", [RSk], [RSk], out=RS[:, 0:n], in_=RS[:, 0:n], func=AF.Exp, scale=-0.5, bias=(LNQ if part == 0 else 0.0))
                        NN, NNk = (P["qn"], (pk, "qn")) if part == 0 else (P["kn"], (pk, "kn"))
                        OP("pool", "tensor_tensor", [T2k, RSk], [NNk], out=NN[:, 0:n], in0=T2[:, 0:n], in1=RS[:, 0:n], op=ALU.mult)
                        res["qk"[part]] = (NN, NNk)
                        yield
                    qn, qnk = res["q"]
                    kn, knk = res["k"]
                    vT, vTk = res["v"]
                    gcb = GC[:, p0:p0 + npair, hd].unsqueeze(2).to_broadcast([128, npair, 128])
                    v3 = lambda t: t[:, 0:n].rearrange("p (a b) -> p a b", b=128)
                    Rt, Rtk = F32P.get()
                    OP("dve", "tensor_tensor", ["ident", "sc%dGC" % d], [Rtk], out=v3(Rt), in0=ident[:].unsqueeze(1).to_broadcast([128, npair, 128]), in1=gcb, op=ALU.mult)
                    bg, bgk = prot.get()
                    OP("pe", "matmul", ["onesf", Rtk], [bgk], out=bg[:, 0:n], lhsT=onesf[:], rhs=Rt[:, 0:n], start=True, stop=True)
                    yield
                    EB, EBk = F32P.get()
                    OP("act", "activation", [bgk], [EBk], out=EB[:, 0:n], in_=bg[:, 0:n], func=AF.Exp)
                    OP("pool", "tensor_tensor", [qnk, EBk], [(pk, "qe")], out=P["qe"][:, 0:n], in0=qn[:, 0:n], in1=EB[:, 0:n], op=ALU.mult)
                    Dl, Dlk = F32P.get()
                    OP("dve", "tensor_tensor", ["sc%dGC" % d, bgk], [Dlk], out=v3(Dl), in0=gcb, in1=v3(bg), op=ALU.subtract)
                    OP("pool", "tensor_tensor", [Dlk, "NEGL"], [Dlk], out=v3(Dl), in0=v3(Dl), in1=NEGL[:].unsqueeze(1).to_broadcast([128, npair, 128]), op=ALU.add)
                    OP("act", "activation", [Dlk], [Dlk], out=Dl[:, 0:n], in_=Dl[:, 0:n], func=AF.Exp)
                    W1, W1k = BFP.get()
                    OP("pool", "tensor_tensor", [Dlk, "sc%dB" % d], [W1k], out=v3(W1), in0=v3(Dl),
                       in1=Bt[:, p0:p0 + npair, hd].unsqueeze(2).to_broadcast([128, npair, 128]), op=ALU.mult)
                    yield
                    Du, Duk = F32P.get()
                    OP("dve", "tensor_tensor", ["sc%dGC" % d, bgk], [Duk], out=v3(Du), in0=v3(bg), in1=gcb, op=ALU.subtract)
                    OP("pool", "tensor_tensor", [Duk, "NEGU"], [Duk], out=v3(Du), in0=v3(Du), in1=NEGU[:].unsqueeze(1).to_broadcast([128, npair, 128]), op=ALU.add)
                    EDT, EDTk = BFP.get()
                    OP("act", "activation", [Duk], [EDTk], out=EDT[:, 0:n], in_=Du[:, 0:n], func=AF.Exp)
                    yield
                    bkk_, bkkk = prot.get()
                    bqk, bqkk = prot.get()
                    for pi in range(npair):
                        cs_ = slice(pi * 128, (pi + 1) * 128)
                        OP("pe", "matmul", [knk], [bkkk], out=bkk_[:, cs_], lhsT=kn[:, cs_], rhs=kn[:, cs_], start=True, stop=True)
                        OP("pe", "matmul", [knk, qnk], [bqkk], out=bqk[:, cs_], lhsT=kn[:, cs_], rhs=qn[:, cs_], start=True, stop=True)
                    yield
                    Lm, Lmk = BFP.get()
                    OP("dve", "tensor_tensor", [bkkk, W1k], [Lmk], out=Lm[:, 0:n], in0=bkk_[:, 0:n], in1=W1[:, 0:n], op=ALU.mult)
                    OP("dve", "tensor_tensor", [bqkk, EDTk], [(pk, "qkt")], out=P["qkt"][:, 0:npair, :], in0=v3(bqk), in1=v3(EDT), op=ALU.mult)

                    def mm_pairs(lhs, lhsk, rhs, rhsk, trans=False):
                        bi_ = prot.i % 6
                        bk2, bk2k = prot.get()
                        for pi in range(npair):
                            cs_ = slice(pi * 128, (pi + 1) * 128)
                            if trans:
                                OP("pe", "transpose", [lhsk, "identb"], [bk2k], out=banks_bf[bi_][:, cs_], in_=lhs[:, cs_], identity=identb[:])
                            else:
                                OP("pe", "matmul", [lhsk, rhsk], [bk2k], out=bk2[:, cs_], lhsT=lhs[:, cs_], rhs=rhs[:, cs_], start=True, stop=True)
                        return (banks_bf[bi_] if trans else bk2), bk2k

                    def evac_copy(src, srck, eng="act"):
                        t, tk = BFP.get()
                        if eng == "act":
                            OP("act", "activation", [srck], [tk], out=t[:, 0:n], in_=src[:, 0:n], func=AF.Identity)
                        else:
                            OP("dve", "tensor_copy", [srck], [tk], out=t[:, 0:n], in_=src[:, 0:n])
                        return t, tk

                    yield
                    bu, buk = mm_pairs(Lm, Lmk, None, None, trans=True)
                    yield
                    Um, Umk = evac_copy(bu, buk, "act")
                    Pt, Ptk = BFP.get()
                    OP("pool", "tensor_tensor", ["identb", Umk], [Ptk], out=v3(Pt), in0=identb[:].unsqueeze(1).to_broadcast([128, npair, 128]), in1=v3(Um), op=ALU.subtract)
                    Lp, Lpk, Up, Upk = Lm, Lmk, Um, Umk
                    for lvl in range(1, 6):
                        yield
                        b1, b1k = mm_pairs(Up, Upk, Lp, Lpk)
                        if lvl < 5:
                            b2, b2k = mm_pairs(Lp, Lpk, Up, Upk)
                        yield
                        Ln_, Lnk = evac_copy(b1, b1k, "act")
                        if lvl < 5:
                            Un_, Unk = evac_copy(b2, b2k, "dve")
                        yield
                        b3, b3k = mm_pairs(Ln_, Lnk, Pt, Ptk)
                        yield
                        Pn, Pnk = BFP.get()
                        OP("dve", "tensor_tensor", [b3k, Ptk], [Pnk], out=Pn[:, 0:n], in0=b3[:, 0:n], in1=Pt[:, 0:n], op=ALU.add)
                        Pt, Ptk = Pn, Pnk
                        Lp, Lpk = Ln_, Lnk
                        if lvl < 5:
                            Up, Upk = Un_, Unk
                    Tt, Ttk = Pt, Ptk
                    yield
                    bkt, bktk = mm_pairs(kn, knk, None, None, trans=True)
                    bvt, bvtk = mm_pairs(vT, vTk, None, None, trans=True)
                    yield
                    kbe, kbek = BFP.get()
                    for pi in range(npair):
                        cs_ = slice(pi * 128, (pi + 1) * 128)
                        OP("act", "activation", [bktk, "sc%dBEG" % d], [kbek], out=kbe[:, cs_], in_=bkt[:, cs_], func=AF.Identity, scale=BEG[:, p0 + pi, hd:hd + 1])
                        OP("act", "activation", [bktk, "sc%dEGR" % d], [(pk, "kd")], out=P["kd"][:, pi, :], in_=bkt[:, cs_], func=AF.Identity, scale=EGR[:, p0 + pi, hd:hd + 1])
                    vb, vbk = BFP.get()
                    for pi in range(npair):
                        cs_ = slice(pi * 128, (pi + 1) * 128)
                        OP("act", "activation", [bvtk, "sc%dB" % d], [vbk], out=vb[:, cs_], in_=bvt[:, cs_], func=AF.Identity, scale=Bt[:, p0 + pi, hd:hd + 1])
                    yield
                    bu2, bu2k = mm_pairs(Tt, Ttk, vb, vbk)
                    bw, bwk = mm_pairs(kbe, kbek, Tt, Ttk)
                    yield
                    OP("dve", "tensor_copy", [bu2k], [(pk, "u")], out=P["u"][:, 0:npair, :], in_=v3(bu2))
                    OP("act", "activation", [bwk], [(pk, "wT")], out=P["wT"][:, 0:n], in_=bw[:, 0:n], func=AF.Identity)

                def rec(gi, tpar):
                    o, n = groups[gi]
                    nch = n // 64
                    p0 = o // 128
                    is_lat = o >= S_CTX
                    for ci in range(nch):
                        pi, hf = ci // 2, ci % 2
                        Ps = slice(hf * 64, hf * 64 + 64)
                        for d in range(2):
                            P = pers[(d, tpar)]
                            pk = P["k"]
                            cur = hstate[d]
                            nxt = 1 - cur
                            hb_c, hb_n = hbf[d][cur], hbf[d][nxt]
                            bwh, bwhk = prot.get()
                            OP("pe", "matmul", [(pk, "wT"), ("hbf", par, d, cur)], [bwhk], out=bwh[Ps, 0:128], lhsT=P["wT"][:, ci * 64:(ci + 1) * 64], rhs=hb_c[:],
                               start=True, stop=True)
                            vn, vnk = VNP[d].get()
                            OP("dve", "tensor_tensor", [(pk, "u"), bwhk], [vnk], out=vn[Ps, :], in0=P["u"][Ps, pi, :], in1=bwh[Ps, 0:128], op=ALU.subtract)
                            if is_lat:
                                ob = banks[6 + d]
                                OP("pe", "matmul", [("hbf", par, d, cur), (pk, "qe")], [("obank", d)], out=ob[:, ci * 64:(ci + 1) * 64], lhsT=hb_c[:],
                                   rhs=P["qe"][:, ci * 64:(ci + 1) * 64], start=True, stop=False)
                                OP("pe", "matmul", [vnk, (pk, "qkt")], [("obank", d)], out=ob[:, ci * 64:(ci + 1) * 64], lhsT=vn[Ps, :],
                                   rhs=P["qkt"][Ps, pi, hf * 64:hf * 64 + 64], start=False, stop=True)
                            bh, bhk = prot.get()
                            OP("pe", "matmul", [(pk, "kd"), vnk], [bhk], out=bh[:, 0:128], lhsT=P["kd"][Ps, pi, :], rhs=vn[Ps, :], start=True, stop=True)
                            egl = EGLt[d][:, p0 + pi, hf, hd:hd + 1]
                            OP("dve", "scalar_tensor_tensor", [("h32", par, d), bhk, "sc%dEGL" % d], [("hbf", par, d, nxt)], out=hb_n[:], in0=h32[d][:], scalar=egl,
                               in1=bh[:, 0:128], op0=ALU.mult, op1=ALU.add)
                            OP("dve", "scalar_tensor_tensor", [("h32", par, d), bhk, "sc%dEGL" % d], [("h32", par, d)], out=h32[d][:], in0=h32[d][:], scalar=egl,
                               in1=bh[:, 0:128], op0=ALU.mult, op1=ALU.add)
                            hstate[d] = nxt
                            yield
                    if is_lat:
                        cl0 = (o - S_CTX) // 64
                        for d in range(2):
                            Ob, Obk = Obuf[0][d]
                            src = banks[6 + d][:, 0:512].rearrange("p (c i) -> p c i", i=64)
                            if d == 0:
                                OP("act", "activation", [("obank", d)], [Obk], out=Ob[:, cl0:cl0 + 8, :], in_=src[:, :, 0:32], func=AF.Identity)
                            else:
                                hi = 63 - cl0
                                OP("act", "activation", [("obank", d)], [Obk], out=Ob[:, hi - 7:hi + 1, :][:, ::-1, :], in_=src[:, :, 32:64][:, :, ::-1], func=AF.Identity)

                def finalize():
                  F32P, BFP = F32Pd[0], BFPd[0]
                  O0, O0k = Obuf[0][0]
                  O1, O1k = Obuf[0][1]
                  Of = O0[:].rearrange("p c r -> p (c r)")
                  OP("pool", "tensor_tensor", [O0k, O1k], [O0k], out=Of, in0=Of, in1=O1[:].rearrange("p c r -> p (c r)"), op=ALU.add)
                  for q4 in range(4):
                      cs_ = slice(q4 * 512, (q4 + 1) * 512)
                      SQ, SQk = BFP.get()
                      OP("pool", "tensor_tensor", [O0k], [SQk], out=SQ[:], in0=Of[:, cs_], in1=Of[:, cs_], op=ALU.mult)
                      bk, bkk = prot.get()
                      OP("pe", "matmul", ["onesb", SQk], [bkk], out=bk[:], lhsT=onesb[:], rhs=SQ[:], start=True, stop=True)
                      RS, RSk = F32P.get()
                      OP("act", "activation", [bkk], [RSk], out=RS[:], in_=bk[:], func=AF.Ln, scale=1.0 / 128, bias=EPS)
                      OP("act", "activation", [RSk], [RSk], out=RS[:], in_=RS[:], func=AF.Exp, scale=-0.5)
                      OP("dve", "scalar_tensor_tensor", [O0k, RSk, "gnorm"], [O1k], out=O1[:].rearrange("p c r -> p (c r)")[:, cs_], in0=Of[:, cs_],
                         scalar=gnorm[:, 0:1], in1=RS[:], op0=ALU.mult, op1=ALU.mult)
                  OP("pool", "tensor_tensor", [O1k, szk], [xok], out=xo[:].rearrange("p (r c) -> p r c", c=64), in0=O1[:].rearrange("p c r -> p r c"),
                     in1=sz[:].rearrange("p (r c) -> p r c", c=64), op=ALU.mult)
                  DMA(XDN[hd * 128:(hd + 1) * 128, :], xo[:], reads=[xok])

                pending = prev
                for gi in range(len(groups)):
                    tpar = tcount["t"] % 2
                    tcount["t"] += 1
                    gens = [prep(0, gi, tpar), prep(1, gi, tpar)]
                    if pending is not None:
                        gens.append(pending["rec"])
                    run_rr(gens)
                    if pending is not None and pending.get("fin") is not None:
                        pending["fin"]()
                    pending = dict(rec=rec(gi, tpar), fin=(finalize if gi == len(groups) - 1 else None))
                return pending

            pend = None
            for hd in dn_heads:
                pend = dn_head(hd, pend)
            run_rr([pend["rec"]])
            pend["fin"]()
            S.barrier()
            arena["off"] = mark0

        top = {"off": ARENA_WORDS}

        def sb_top(name, shape, dt=F32):
            shape = list(shape)
            elems = int(np.prod(shape[1:]))
            words = elems if dt == F32 else (elems + 1) // 2
            words += words % 2
            top["off"] -= words
            off = top["off"]
            ap = arena_t[0:shape[0], off:off + words]
            if dt != F32:
                ap = ap.bitcast(dt)[:, 0:elems]
            else:
                ap = ap[:, 0:elems]
            if len(shape) == 3:
                ap = ap.rearrange("p (a b) -> p a b", a=shape[1])
            return ap

        NT = OWN // 128
        if "tail" in phases:
            hfT = sb_top("hfT", [128, 8, OWN], BF16)
            CW = sb_top("CW", [128, NT, NE])
            G2P = sb_top("G2P", [128, D])
            prot = TP.__new__(TP)
            prot.t = [(banks[i], "bank%d" % i) for i in range(8)]
            prot.i = 0
            G1P = sb("G1P", [128, D])
            GP2 = sb("GP2", [128, D])
            SH2t = sb("SH2t", [128, D])
            rb = sb("rb", [128, NE])
            rw = sb("rw", [128, 8, NE])
            mark_t = arena["off"]
            MOD = sb("MOD", [128, 4, D])
            rows = sb("rows", [128, 3, D])
            csrep = sb("csrep", [128, 8, 128])
            adab_r = sb("adab_r", [128, 4 * D])
            DMA(adab_r[:], ada_b[:, 2048:6144].partition_broadcast(128), writes=["adab_r"])
            DMA(rows[:, 0, :], gpost.partition_broadcast(128), writes=["rows"])
            DMA(rows[:, 1, :], gfpre.partition_broadcast(128), writes=["rows"])
            DMA(rows[:, 2, :], gfpost.partition_broadcast(128), writes=["rows"])
            DMA(rb[:], router_b.partition_broadcast(128), writes=["rb"])
            DMA(rw[:], router_w.rearrange("(kc p) e -> p kc e", p=128), writes=["rw"])
            for kc in range(8):
                OP("dve", "tensor_copy", [], [("csrep", kc)], out=csrep[:, kc, :], in_=cs[:, kc, 0:1].to_broadcast([128, 128]))
            awp = TP("awp", 2, [128, 8, 512], F32)
            for pc_ in range(8):
                aw, awk = awp.get()
                for kc in range(8):
                    DMA(aw[:, kc, :], ada_w[kc * 128:(kc + 1) * 128, 2048 + pc_ * 512:2048 + (pc_ + 1) * 512], writes=[(awk, kc)])
                bk, bkk = prot.get()
                for kc in range(8):
                    OP("pe", "matmul", [(awk, kc), ("csrep", kc)], [bkk], out=bk[:], lhsT=csrep[:, kc, :], rhs=aw[:, kc, :], start=(kc == 0), stop=(kc == 7))
                OP("dve", "tensor_tensor", [bkk, "adab_r"], ["MOD"], out=MOD[:].rearrange("p j d -> p (j d)")[:, pc_ * 512:(pc_ + 1) * 512], in0=bk[:],
                   in1=adab_r[:, pc_ * 512:(pc_ + 1) * 512], op=ALU.add)
            OP("dve", "tensor_tensor", ["MOD", "rows"], ["G1P"], out=G1P[:], in0=MOD[:, 0, :], in1=rows[:, 0, :], op=ALU.mult)
            OP("dve", "scalar_tensor_tensor", ["MOD", "rows"], ["GP2"], out=GP2[:], in0=MOD[:, 2, :], scalar=1.0, in1=rows[:, 1, :], op0=ALU.add, op1=ALU.mult)
            OP("dve", "tensor_tensor", ["MOD", "rows"], ["G2P"], out=G2P[:], in0=MOD[:, 3, :], in1=rows[:, 2, :], op=ALU.mult)
            OP("pool", "tensor_copy", ["MOD"], ["SH2t"], out=SH2t[:], in_=MOD[:, 1, :])
            SH2 = SH2t[:]
            S.barrier()
            arena["off"] = mark_t
            n_blk = 4 if tail_stop >= 2 else 0
            Wrg = sb("Wrg", [128, 8, D], BF16)
            Wdn = sb("Wdn", [128, 8, D], BF16)
            Wou = sb("Wou", [128, 8, D], BF16)
            for (wt, src, wk) in ((Wrg, rg_w_o, "Wrg"), (Wdn, dn_w_o, "Wdn"), (Wou, w_out, "Wou")):
                for kc in range(8):
                    S.op("pool", lambda e, wt=wt, src=src, kc=kc: e.dma_start(out=wt[:, kc, :], in_=src[kc * 128:(kc + 1) * 128, :]), writes=[(wk, kc)], dma=True)
            xrb = sb("t_xrg", [128, 8, 512], BF16)
            xdb = sb("t_xdn", [128, 8, 512], BF16)
            s1b = sb("t_sg1", [128, 8, 512], BF16)
            s2b = sb("t_sg2", [128, 8, 512], BF16)
            mrg = sb("t_mrg", [128, 8, 512], BF16)
            F4 = TP("t_f4", 6, [128, D], F32)
            F2 = TP("t_f2", 4, [128, 512], F32)
            hf32 = TP("t_hf32", 2, [128, 8, 128], F32)
            sst = TP("t_ss", 4, [128, 8], F32)
            junk = sb("t_junk", [128, D], BF16)
            print("TAIL arena bottom", arena["off"], "top", top["off"])
            assert arena["off"] <= top["off"], "tail arena overlap"
            for blk in range(n_blk):
                t0 = blk * 512
                for kc in range(8):
                    DMA(xrb[:, kc, :], XRG[kc * 128:(kc + 1) * 128, t0:t0 + 512], writes=[("xrb", kc)])
                    DMA(xdb[:, kc, :], XDN[kc * 128:(kc + 1) * 128, t0:t0 + 512], writes=[("xdb", kc)])
                    DMA(s1b[:, kc, :], GATES[2048 + kc * 128:2048 + (kc + 1) * 128, t0:t0 + 512], writes=[("s1b", kc)])
                    DMA(s2b[:, kc, :], GATES[3072 + kc * 128:3072 + (kc + 1) * 128, t0:t0 + 512], writes=[("s2b", kc)])
                for m in range(8):
                    b1, b1k = prot.get()
                    b2, b2k = prot.get()
                    for kc in range(8):
                        OP("pe", "matmul", [("Wrg", kc), ("xrb", kc)], [b1k], out=b1[:], lhsT=Wrg[:, kc, m * 128:(m + 1) * 128], rhs=xrb[:, kc, :], start=(kc == 0), stop=(kc == 7))
                    for kc in range(8):
                        OP("pe", "matmul", [("Wdn", kc), ("xdb", kc)], [b2k], out=b2[:], lhsT=Wdn[:, kc, m * 128:(m + 1) * 128], rhs=xdb[:, kc, :], start=(kc == 0), stop=(kc == 7))
                    ta, tak = F2.get()
                    tb_, tbk = F2.get()
                    OP("dve", "tensor_tensor", [b1k, ("s1b", m)], [tak], out=ta[:], in0=b1[:], in1=s1b[:, m, :], op=ALU.mult)
                    OP("dve", "tensor_tensor", [b2k, ("s2b", m)], [tbk], out=tb_[:], in0=b2[:], in1=s2b[:, m, :], op=ALU.mult)
                    OP("pool", "tensor_tensor", [tak, tbk], [("mrg", m)], out=mrg[:, m, :], in0=ta[:], in1=tb_[:], op=ALU.add)
                for tt in range(4 if tail_stop >= 3 else 0):
                    gt_ = blk * 4 + tt
                    r0 = gt_ * 128
                    xt, xtk = F4.get()
                    DMA(xt[:], x[r0:r0 + 128, :], writes=[xtk])
                    ss, ssk = sst.get()
                    yb = []
                    for n2 in range(2):
                        bk, bkk = prot.get()
                        for kc in range(8):
                            OP("pe", "matmul", [("Wou", kc), ("mrg", kc)], [bkk], out=bk[:], lhsT=mrg[:, kc, tt * 128:(tt + 1) * 128], rhs=Wou[:, kc, n2 * 512:(n2 + 1) * 512],
                               start=(kc == 0), stop=(kc == 7))
                        OP("act", "activation", [bkk], ["t_junk", (ssk, n2)], out=junk[:, 0:512], in_=bk[:], func=AF.Square, accum_out=ss[:, n2:n2 + 1])
                        yb.append((bk, bkk))
                    if tail_stop < 3.2:
                        continue
                    OP("dve", "tensor_tensor", [(ssk, 0), (ssk, 1)], [(ssk, 2)], out=ss[:, 2:3], in0=ss[:, 0:1], in1=ss[:, 1:2], op=ALU.add)
                    OP("dve", "tensor_scalar", [(ssk, 2)], [(ssk, 2)], out=ss[:, 2:3], in0=ss[:, 2:3], scalar1=1.0 / D, scalar2=EPS, op0=ALU.mult, op1=ALU.add)
                    OP("act", "activation", [(ssk, 2)], [(ssk, 2)], out=ss[:, 2:3], in_=ss[:, 2:3], func=AF.Sqrt)
                    OP("dve", "reciprocal", [(ssk, 2)], [(ssk, 2)], out=ss[:, 2:3], in_=ss[:, 2:3])
                    xn, xnk = F4.get()
                    for n2 in range(2):
                        bk, bkk = yb[n2]
                        cs_ = slice(n2 * 512, (n2 + 1) * 512)
                        OP("dve", "scalar_tensor_tensor", [bkk, (ssk, 2), "G1P"], [(xnk, n2)], out=xn[:, cs_], in0=bk[:], scalar=ss[:, 2:3], in1=G1P[:, cs_], op0=ALU.mult, op1=ALU.mult)
                    OP("pool", "tensor_tensor", [(xnk, 0), (xnk, 1), xtk], [xnk, (xnk, 0), (xnk, 1)], out=xn[:], in0=xn[:], in1=xt[:], op=ALU.add)
                    if tail_stop < 3.4:
                        continue
                    DMA(XNEW[r0:r0 + 128, :], xn[:], reads=[xnk])
                    OP("act", "activation", [xnk], ["t_junk", (ssk, 3)], out=junk[:], in_=xn[:], func=AF.Square, accum_out=ss[:, 3:4])
                    OP("dve", "tensor_scalar", [(ssk, 3)], [(ssk, 3)], out=ss[:, 3:4], in0=ss[:, 3:4], scalar1=1.0 / D, scalar2=EPS, op0=ALU.mult, op1=ALU.add)
                    OP("act", "activation", [(ssk, 3)], [(ssk, 3)], out=ss[:, 3:4], in_=ss[:, 3:4], func=AF.Sqrt)
                    OP("dve", "reciprocal", [(ssk, 3)], [(ssk, 3)], out=ss[:, 3:4], in_=ss[:, 3:4])
                    if tail_stop < 3.6:
                        continue
                    hf, hfk = F4.get()
                    OP("dve", "scalar_tensor_tensor", [xnk, (ssk, 3), "GP2"], [hfk], out=hf[:], in0=xn[:], scalar=ss[:, 3:4], in1=GP2[:], op0=ALU.mult, op1=ALU.mult)
                    OP("pool", "tensor_tensor", [hfk, "SH2t"], [hfk], out=hf[:], in0=hf[:], in1=SH2, op=ALU.add)
                    if tail_stop < 3.8:
                        continue
                    h32t, h32k = hf32.get()
                    for half in range(2):
                        bk, bkk = prot.get()
                        for c4 in range(4):
                            c = half * 4 + c4
                            OP("pe", "transpose", [hfk, "ident"], [bkk], out=bk[:, c4 * 128:(c4 + 1) * 128], in_=hf[:, c * 128:(c + 1) * 128], identity=ident[:])
                        OP("dve", "tensor_copy", [bkk], [(h32k, half)], out=h32t[:, half * 4:half * 4 + 4, :], in_=bk[:].rearrange("p (c t) -> p c t", c=4))
                        OP("pool", "tensor_copy", [(h32k, half)], [("hfT", gt_, half)], out=hfT[:, half * 4:half * 4 + 4, r0:r0 + 128], in_=h32t[:, half * 4:half * 4 + 4, :])
                    if tail_stop < 4:
                        continue
                    bl, blk_ = prot.get()
                    for kc in range(8):
                        OP("pe", "matmul", [(h32k, kc // 4), "rw"], [blk_], out=bl[:, 0:NE], lhsT=h32t[:, kc, :], rhs=rw[:, kc, :], start=(kc == 0), stop=(kc == 7))
                    lg, lgk = F2.get()
                    OP("dve", "tensor_tensor", [blk_, "rb"], [lgk], out=lg[:, 0:NE], in0=bl[:, 0:NE], in1=rb[:], op=ALU.add)
                    OP("dve", "max", [lgk], [(lgk, "m8")], out=lg[:, 64:72], in_=lg[:, 0:NE])
                    OP("dve", "tensor_scalar", [lgk, (lgk, "m8")], [(lgk, "mask")], out=lg[:, 128:128 + NE], in0=lg[:, 0:NE], scalar1=lg[:, 67:68], scalar2=None, op0=ALU.is_ge)
                    OP("dve", "tensor_scalar", [(lgk, "m8")], [(lgk, "nm")], out=lg[:, 72:73], in0=lg[:, 64:65], scalar1=-1.0, scalar2=None, op0=ALU.mult)
                    OP("act", "activation", [lgk, (lgk, "nm")], [(lgk, "e")], out=lg[:, 192:192 + NE], in_=lg[:, 0:NE], func=AF.Exp, bias=lg[:, 72:73])
                    OP("dve", "tensor_tensor", [(lgk, "e"), (lgk, "mask")], [(lgk, "em")], out=lg[:, 256:256 + NE], in0=lg[:, 192:192 + NE], in1=lg[:, 128:128 + NE], op=ALU.mult)
                    OP("dve", "tensor_reduce", [(lgk, "em")], [(lgk, "sum")], out=lg[:, 73:74], in_=lg[:, 256:256 + NE], axis=mybir.AxisListType.X, op=ALU.add)
                    OP("dve", "reciprocal", [(lgk, "sum")], [(lgk, "sum")], out=lg[:, 73:74], in_=lg[:, 73:74])
                    OP("dve", "tensor_scalar", [(lgk, "em"), (lgk, "sum")], [("CW", gt_)], out=CW[:, gt_, :], in0=lg[:, 256:256 + NE], scalar1=lg[:, 73:74], scalar2=None, op0=ALU.mult)
            if debug:
                dbg("CW", CW[:], [("CW", i) for i in range(NT)])
                dbg("hfT", hfT[:, :, :], [("hfT", i, h) for i in range(NT) for h in range(2)])
            S.barrier()
            arena["off"] = mark0

        if "moe" in phases:
            prot = TP.__new__(TP)
            prot.t = [(banks[i], "bank%d" % i) for i in range(8)]
            prot.i = 0
            acc = sb("acc", [128, NT, D])
            b1T = sb("b1T", [128, NE, 16])
            mark_m = arena["off"]
            eb2 = sb("eb2", [NE, D])
            cwT = sb("cwT", [NE, NT, 128])
            DMA(b1T[:], e_b1T, writes=["b1T"])
            DMA(eb2[:], e_b2, writes=["eb2"])
            for tt in range(NT):
                bk, bkk = prot.get()
                OP("pe", "transpose", [], [bkk], out=bk[0:NE, 0:128], in_=CW[:, tt, :], identity=ident[:])
                OP("dve", "tensor_copy", [bkk], [("cwT", tt)], out=cwT[:, tt, :], in_=bk[0:NE, 0:128])
                for n2 in range(2):
                    bk2, bk2k = prot.get()
                    OP("pe", "matmul", [("cwT", tt), "eb2"], [bk2k], out=bk2[:], lhsT=cwT[:, tt, :], rhs=eb2[:, n2 * 512:(n2 + 1) * 512], start=True, stop=True)
                    OP("act", "activation", [bk2k], [("acc", tt, n2)], out=acc[:, tt, n2 * 512:(n2 + 1) * 512], in_=bk2[:], func=AF.Identity)
            S.barrier()
            arena["off"] = mark_m
            w1b = sb("w1b", [128, 8, 2 * D], BF16)
            w2b = sb("w2b", [128, 8, D], BF16)
            stg = TP("m_stg", 2, [128, 2 * D], F32)
            actT = sb("actT", [128, 8, OWN // 2], BF16)
            EF = TP("m_ef", 6, [128, 512], F32)
            n_exp = moe_experts
            castq = Rot(["pool", "pool", "act"])
            for e_ in range(n_exp):
                for kc in range(8):
                    sg, sgk = stg.get()
                    DMA(sg[:], e_w1[e_, kc * 128:(kc + 1) * 128, :], writes=[sgk])
                    OP("pool", "tensor_copy", [sgk], [("w1b", kc)], out=w1b[:, kc, :], in_=sg[:])
                for kc in range(0, 8, 2):
                    sg, sgk = stg.get()
                    DMA(sg[:].rearrange("p (a b) -> p a b", a=2), e_w2[e_, kc * 128:(kc + 2) * 128, :].rearrange("(a p) d -> p a d", p=128), writes=[sgk])
                    OP("pool", "tensor_copy", [sgk], [("w2b", kc), ("w2b", kc + 1)], out=w2b[:, kc:kc + 2, :], in_=sg[:].rearrange("p (a b) -> p a b", a=2))
                for half in range(2):
                    h0 = half * (OWN // 2)
                    for m in range(8):
                        for nt_ in range(2):
                            tk0 = h0 + nt_ * 512
                            bg, bgk = prot.get()
                            bl, blk_ = prot.get()
                            for kc in range(8):
                                OP("pe", "matmul", [("w1b", kc)], [bgk], out=bg[:], lhsT=w1b[:, kc, m * 128:(m + 1) * 128], rhs=hfT[:, kc, tk0:tk0 + 512], start=(kc == 0), stop=(kc == 7))
                            for kc in range(8):
                                OP("pe", "matmul", [("w1b", kc)], [blk_], out=bl[:], lhsT=w1b[:, kc, D + m * 128:D + (m + 1) * 128], rhs=hfT[:, kc, tk0:tk0 + 512], start=(kc == 0), stop=(kc == 7))
                            tg, tgk = EF.get()
                            ts, tsk = EF.get()
                            tl, tlk = EF.get()
                            OP("dve", "tensor_scalar", [bgk, "b1T"], [tgk], out=tg[:], in0=bg[:], scalar1=b1T[:, e_, m:m + 1], scalar2=7.0, op0=ALU.add, op1=ALU.min)
                            OP("act", "activation", [tgk], [tsk], out=ts[:], in_=tg[:], func=AF.Sigmoid, scale=1.702)
                            OP("dve", "tensor_scalar", [blk_, "b1T"], [tlk], out=tl[:], in0=bl[:], scalar1=b1T[:, e_, 8 + m:9 + m], scalar2=7.0, op0=ALU.add, op1=ALU.min)
                            OP("dve", "tensor_scalar", [tlk], [tlk], out=tl[:], in0=tl[:], scalar1=-7.0, scalar2=1.0, op0=ALU.max, op1=ALU.add)
                            OP("pool", "tensor_tensor", [tgk, tsk], [tgk], out=tg[:], in0=tg[:], in1=ts[:], op=ALU.mult)
                            OP("pool", "tensor_tensor", [tgk, tlk], [("actT", m, nt_)], out=actT[:, m, nt_ * 512:(nt_ + 1) * 512], in0=tg[:], in1=tl[:], op=ALU.mult)
                    for tt in range(8):
                        gt_ = half * 8 + tt
                        for n2 in range(2):
                            bk, bkk = prot.get()
                            for m in range(8):
                                OP("pe", "matmul", [("actT", m, tt // 4), ("w2b", m)], [bkk], out=bk[:], lhsT=actT[:, m, tt * 128:(tt + 1) * 128], rhs=w2b[:, m, n2 * 512:(n2 + 1) * 512],
                                   start=(m == 0), stop=(m == 7))
                            OP("dve", "scalar_tensor_tensor", [bkk, ("acc", gt_, n2)], [("acc", gt_, n2)], out=acc[:, gt_, n2 * 512:(n2 + 1) * 512], in0=bk[:],
                               scalar=CW[:, gt_, e_:e_ + 1], in1=acc[:, gt_, n2 * 512:(n2 + 1) * 512], op0=ALU.mult, op1=ALU.add)
            S.barrier()
            arena["off"] = mark_m
            FX = TP("m_fx", 3, [128, D], F32)
            FO = TP("m_fo", 3, [128, D], F32)
            sst2 = TP("m_ss", 3, [128, 2], F32)
            junk2 = sb("m_junk", [128, D], BF16)
            outs = []
            for tt in range(NT):
                r0 = tt * 128
                xn, xnk = FX.get()
                DMA(xn[:], XNEW[r0:r0 + 128, :], writes=[xnk])
                ss, ssk = sst2.get()
                OP("act", "activation", [("acc", tt, 0), ("acc", tt, 1)], ["m_junk", ssk], out=junk2[:], in_=acc[:, tt, :], func=AF.Square, accum_out=ss[:, 0:1])
                OP("dve", "tensor_scalar", [ssk], [ssk], out=ss[:, 0:1], in0=ss[:, 0:1], scalar1=1.0 / D, scalar2=EPS, op0=ALU.mult, op1=ALU.add)
                OP("act", "activation", [ssk], [ssk], out=ss[:, 0:1], in_=ss[:, 0:1], func=AF.Sqrt)
                OP("dve", "reciprocal", [ssk], [ssk], out=ss[:, 0:1], in_=ss[:, 0:1])
                fo, fok = FO.get()
                OP("dve", "scalar_tensor_tensor", [("acc", tt, 0), ("acc", tt, 1), ssk], [fok], out=fo[:], in0=acc[:, tt, :], scalar=ss[:, 0:1], in1=G2P[:], op0=ALU.mult, op1=ALU.mult)
                OP("pool", "tensor_tensor", [fok, xnk], [fok], out=fo[:], in0=fo[:], in1=xn[:], op=ALU.add)
                outs.append(S.op("sp", lambda e, fo=fo, r0=r0: e.dma_start(out=out[r0:r0 + 128, :], in_=fo[:]), reads=[fok], dma=True))
            S.barrier()

        S.emit(st)
    return nc


def prepare_core_inputs(inputs, b, half):
    f = np.ascontiguousarray
    flip = (half == 1)
    xs = inputs["x"][b]
    cs = inputs["ctx"][b]
    if flip:
        xs = xs[::-1]
        cs = cs[::-1]
    w_in = inputs["w_in"][0]
    if flip:
        w_in = w_in.copy()
        a0 = 6144
        w_in[:, a0:a0 + 8], w_in[:, a0 + 8:a0 + 16] = inputs["w_in"][0][:, a0 + 8:a0 + 16], inputs["w_in"][0][:, a0:a0 + 8]
        b0 = 6160
        w_in[:, b0:b0 + 8], w_in[:, b0 + 8:b0 + 16] = inputs["w_in"][0][:, b0 + 8:b0 + 16], inputs["w_in"][0][:, b0:b0 + 8]
    cvec = np.stack([inputs["c"][b], inputs["c_ctx"]], axis=-1)
    m = {
        "x": f(xs), "ctx": f(cs),
        "cc": f(cvec.reshape(8, 128, 2).transpose(1, 0, 2)),
        "ada_w": f(inputs["ada_w"][0]),
        "ada_bT": f(inputs["ada_b"][0].reshape(48, 128).T),
        "ada_b": f(inputs["ada_b"][0].reshape(1, -1)),
        "gpreT": f(inputs["mix_pre_g"][0].reshape(8, 128).T),
        "w_in": f(w_in),
    }
    dsel = (lambda a: a[::-1]) if flip else (lambda a: a)
    P = lambda k: dsel(inputs[k][0])
    rgp = np.concatenate([P("rg_conv_w").transpose(0, 2, 1),
                          P("rg_conv_b")[:, :, None], P("rg_ba")[:, :, None], P("rg_bi")[:, :, None], P("rg_lam")[:, :, None]], axis=2)
    m["rgp"] = f(rgp.reshape(2, 8, 128, 8).transpose(2, 1, 0, 3))
    m["rg_wa"] = f(P("rg_wa"))
    m["rg_wi"] = f(P("rg_wi"))
    cw = P("dn_conv_w")
    m["dncw"] = f(cw.reshape(2, 4, 24, 128).transpose(3, 2, 0, 1))
    m["dnc"] = f(np.concatenate([P("dn_a_log").reshape(-1), P("dn_dt_bias").reshape(-1)]).reshape(1, 32))
    m["dn_norm_g"] = f(inputs["dn_norm_g"][0].reshape(128, 1))
    m["gpost"] = f(inputs["mix_post_g"][0].reshape(1, -1))
    m["gfpre"] = f(inputs["ffn_pre_g"][0].reshape(1, -1))
    m["gfpost"] = f(inputs["ffn_post_g"][0].reshape(1, -1))
    for k in ("rg_w_o", "dn_w_o", "w_out", "router_w", "e_w1", "e_w2", "e_b2"):
        m[k] = f(inputs[k][0])
    m["router_b"] = f(inputs["router_b"][0].reshape(1, -1))
    m["e_b1T"] = f(inputs["e_b1"][0].reshape(NE, 16, 128).transpose(2, 0, 1))
    return {k: np.asarray(v, dtype=np.float32) for k, v in m.items()}


_PROG = {}


def kernel(**inputs):
    inputs = {k: np.asarray(v) for k, v in inputs.items()}
    if "nc" not in _PROG:
        _PROG["nc"] = build_program()
    nc = _PROG["nc"]
    in_maps = []
    for core in range(8):
        b, half = core // 2, core % 2
        in_maps.append(prepare_core_inputs(inputs, b, half))
    res = run_bass_kernel_spmd(nc, in_maps, core_ids=list(range(8)))
    B = inputs["x"].shape[0]
    outp = np.zeros((B, S_LAT, D), np.float32)
    for core in range(8):
        b, half = core // 2, core % 2
        o = np.asarray(res.results[core]["out"], dtype=np.float32)
        if half == 0:
            outp[b, 0:OWN] = o
        else:
            outp[b, S_LAT - OWN:] = o[::-1]
    return outp
```

```python
from contextlib import ExitStack
import numpy as np
import concourse.bass as bass
import concourse.mybir as mybir
from concourse.bass_utils import run_bass_kernel_spmd

F32 = mybir.dt.float32
BF16 = mybir.dt.bfloat16
AF = mybir.ActivationFunctionType
ALU = mybir.AluOpType

D = 1024
S_LAT = 4096
S_CTX = 256
S_ALL = S_LAT + S_CTX
OWN = 2048
EPS = 1e-6
NE = 32
N_DMA_SEM = 32
SEM_GEN = 30000


class Sched:
    ENGS = ("pe", "act", "dve", "pool", "sp")

    def __init__(self, nc):
        self.nc = nc
        self.ops = {e: [] for e in self.ENGS}
        self.cnt = {e: 0 for e in self.ENGS}
        self.known = {e: {} for e in self.ENGS}
        self.last_w = {}
        self.readers = {}
        self.dma_n = 0
        self.dma_slot_target = [0] * N_DMA_SEM
        self.semids = set()

    def _add_wait(self, eng, waits, tok, raw):
        semid, val, teng = tok
        if teng == eng:
            if eng == "pe" or not raw:
                return
        if self.known[eng].get(semid, 0) >= val:
            return
        waits[semid] = max(waits.get(semid, 0), val)

    def op(self, eng, fn, reads=(), writes=(), dma=False):
        waits = {}
        for k in reads:
            t = self.last_w.get(k)
            if t is not None:
                self._add_wait(eng, waits, t, True)
            if (isinstance(k, str) and k.startswith("bank")) or (isinstance(k, tuple) and k[0] == "obank"):
                for t in self.readers.get(k, ()):
                    self._add_wait(eng, waits, t, False)
        for k in writes:
            t = self.last_w.get(k)
            if t is not None:
                self._add_wait(eng, waits, t, False)
            for t in self.readers.get(k, ()):
                self._add_wait(eng, waits, t, False)
        if dma:
            slot = self.dma_n % N_DMA_SEM
            self.dma_n += 1
            prev = self.dma_slot_target[slot]
            if prev > 0:
                self._add_wait(eng, waits, ("q%d" % slot, prev, None), True)
            val = prev + 16
            self.dma_slot_target[slot] = val
            tok = ("q%d" % slot, val, None)
        else:
            self.cnt[eng] += 1
            gen, val = divmod(self.cnt[eng] - 1, SEM_GEN)
            tok = ("%s.%d" % (eng, gen), val + 1, eng)
            if gen > 0 and val == 0:
                pass
        self.semids.add(tok[0])
        for semid, val in waits.items():
            self.known[eng][semid] = val
        self.ops[eng].append((waits, fn, tok))
        for k in reads:
            self.readers.setdefault(k, []).append(tok)
        for k in writes:
            self.last_w[k] = tok
            self.readers[k] = []
        return tok

    def wait_tokens(self, eng, toks):
        waits = {}
        for t in toks:
            self._add_wait(eng, waits, t, True)
        for semid, val in waits.items():
            self.known[eng][semid] = val
        self.ops[eng].append((waits, None, None))

    def barrier(self):
        toks = []
        for e in self.ENGS:
            if self.cnt[e] > 0:
                gen, val = divmod(self.cnt[e] - 1, SEM_GEN)
                toks.append(("%s.%d" % (e, gen), val + 1, None))
        for slot in range(N_DMA_SEM):
            if self.dma_slot_target[slot] > 0:
                toks.append(("q%d" % slot, self.dma_slot_target[slot], None))
        for e in self.ENGS:
            self.wait_tokens(e, toks)
        self.last_w = {}
        self.readers = {}

    def emit(self, stack):
        nc = self.nc
        sems = {}
        for sid in sorted(self.semids):
            sems[sid] = stack.enter_context(nc.semaphore("s_" + sid.replace(".", "_")))
        block = stack.enter_context(nc.Block())

        def run(ename):
            def body(eng):
                for waits, fn, tok in self.ops[ename]:
                    for semid, val in waits.items():
                        eng.wait_ge(sems[semid], val)
                    if fn is None:
                        continue
                    inst = fn(eng)
                    semid, val, _ = tok
                    inst.then_inc(sems[semid], 16 if semid.startswith("q") else 1)
            return body

        block.tensor(run("pe"))
        block.scalar(run("act"))
        block.vector(run("dve"))
        block.gpsimd(run("pool"))
        block.sync(run("sp"))


class Rot:
    def __init__(self, items):
        self.items = items
        self.i = 0

    def next(self):
        it = self.items[self.i % len(self.items)]
        self.i += 1
        return it


def build_program(debug=False, phases=("pa", "pb", "rg", "dn", "tail", "moe"), dn_heads=tuple(range(8)), moe_experts=NE, tail_stop=99):
    nc = bass.Bass("TRN2", target_bir_lowering=False)
    dbg_kind = "ExternalOutput" if debug else "Internal"

    def din(name, shape, dt=F32):
        return nc.dram_tensor(name, list(shape), dt, kind="ExternalInput").ap()

    def dscr(name, shape, dt):
        return nc.dram_tensor(name, list(shape), dt, kind=dbg_kind).ap()

    x = din("x", [S_LAT, D])
    ctx = din("ctx", [S_CTX, D])
    cc = din("cc", [128, 8, 2])
    ada_w = din("ada_w", [D, 6 * D])
    ada_bT = din("ada_bT", [128, 48])
    ada_b = din("ada_b", [1, 6 * D])
    gpreT = din("gpreT", [128, 8])
    w_in = din("w_in", [D, 8224])
    rgp = din("rgp", [128, 8, 2, 8])
    rg_wa = din("rg_wa", [2, 16, 64, 64])
    rg_wi = din("rg_wi", [2, 16, 64, 64])
    dncw = din("dncw", [128, 24, 2, 4])
    dnc = din("dnc", [1, 32])
    dn_norm_g = din("dn_norm_g", [128, 1])
    gpost = din("gpost", [1, D])
    gfpre = din("gfpre", [1, D])
    gfpost = din("gfpost", [1, D])
    rg_w_o = din("rg_w_o", [D, D])
    dn_w_o = din("dn_w_o", [D, D])
    w_out = din("w_out", [D, D])
    router_w = din("router_w", [D, NE])
    router_b = din("router_b", [1, NE])
    e_w1 = din("e_w1", [NE, D, 2 * D])
    e_b1T = din("e_b1T", [128, NE, 16])
    e_w2 = din("e_w2", [NE, D, D])
    e_b2 = din("e_b2", [NE, D])
    out = nc.dram_tensor("out", [OWN, D], F32, kind="ExternalOutput").ap()

    PRG = dscr("PRG", [D, S_ALL], BF16)
    PQKV = dscr("PQKV", [3 * D, S_ALL], BF16)
    PAB = dscr("PAB", [S_ALL, 32], F32)
    GATES = dscr("GATES", [4 * D, OWN], BF16)
    XRG = dscr("XRG", [D, OWN], BF16)
    XDN = dscr("XDN", [D, OWN], BF16)
    XNEW = dscr("XNEW", [OWN, D], F32)

    with ExitStack() as st:
        E = st.enter_context
        S = Sched(nc)

        ARENA_WORDS = 53200
        arena_t = E(nc.sbuf_tensor("arena", [128, ARENA_WORDS], F32))
        arena = {"off": 0}

        def sb(name, shape, dt=F32):
            shape = list(shape)
            elems = int(np.prod(shape[1:]))
            words = elems if dt == F32 else (elems + 1) // 2
            words += words % 2
            off = arena["off"]
            assert off + words <= ARENA_WORDS, ("SBUF arena overflow", name, off, words)
            arena["off"] = off + words
            ap = arena_t[0:shape[0], off:off + words]
            if dt != F32:
                ap = ap.bitcast(dt)[:, 0:elems]
            else:
                ap = ap[:, 0:elems]
            if len(shape) == 3:
                ap = ap.rearrange("p (a b) -> p a b", a=shape[1])
            elif len(shape) == 4:
                ap = ap.rearrange("p (a b c) -> p a b c", a=shape[1], b=shape[2])
            return ap

        def ps(name):
            return E(nc.psum_tensor(name, [128, 512], F32))

        banks = [ps("bank%d" % i) for i in range(8)]

        def dbg(name, ap, reads):
            if not debug:
                return
            t = nc.dram_tensor("dbg_" + name, list(ap.shape), ap.dtype, kind="ExternalOutput").ap()
            S.op("sp", lambda e: e.dma_start(out=t, in_=ap), reads=reads, dma=True)

        ident = sb("ident", [128, 128])
        S.op("pool", lambda e: e.memset(ident[:], 0.0), writes=["ident"])
        S.op("pool", lambda e: e.affine_select(out=ident[:], in_=ident[:], pattern=[[-1, 128]],
                                               compare_op=ALU.not_equal, fill=1.0, base=0,
                                               channel_multiplier=1),
             reads=["ident"], writes=["ident"])

        cs = sb("cs", [128, 8, 2])
        modF = sb("modF", [128, 16, 2])
        Gm = sb("Gm", [128, 8, 2])
        Sh = sb("Sh", [128, 8, 2])
        adabT = sb("adabT", [128, 48])
        gpre = sb("gpre", [128, 8])
        S.op("sp", lambda e: e.dma_start(out=cs[:], in_=cc), writes=["cs"], dma=True)
        S.op("sp", lambda e: e.dma_start(out=adabT[:], in_=ada_bT), writes=["adabT"], dma=True)
        S.op("sp", lambda e: e.dma_start(out=gpre[:], in_=gpreT), writes=["gpre"], dma=True)
        S.op("act", lambda e: e.activation(out=cs[:], in_=cs[:], func=AF.Silu), reads=["cs"], writes=["cs"])
        mark0 = arena["off"]
        if True:
            adaw0 = sb("adaw0", [128, 8, 2048])
            for kc in range(8):
                S.op("sp", lambda e, kc=kc: e.dma_start(out=adaw0[:, kc, :], in_=ada_w[kc * 128:(kc + 1) * 128, 0:2048]),
                     writes=[("adaw0", kc)], dma=True)
            pm = banks[0]
            for j in range(16):
                for kc in range(8):
                    S.op("pe", lambda e, j=j, kc=kc: e.matmul(pm[:, 2 * j:2 * j + 2], lhsT=adaw0[:, kc, j * 128:(j + 1) * 128],
                                                              rhs=cs[:, kc, :], start=(kc == 0), stop=(kc == 7)),
                         reads=[("adaw0", kc), "cs"], writes=["bank0"])
            S.op("dve", lambda e: e.tensor_tensor(out=modF[:], in0=pm[:, 0:32].rearrange("p (j t) -> p j t", t=2),
                                                  in1=adabT[:, 0:16].unsqueeze(2).to_broadcast([128, 16, 2]), op=ALU.add),
                 reads=["bank0", "adabT"], writes=["modF"])
            S.op("dve", lambda e: e.tensor_scalar(out=Gm[:], in0=modF[:, 8:16, :], scalar1=1.0, scalar2=None, op0=ALU.add),
                 reads=["modF"], writes=["Gm"])
            S.op("dve", lambda e: e.tensor_tensor(out=Gm[:], in0=Gm[:], in1=gpre[:].unsqueeze(2).to_broadcast([128, 8, 2]), op=ALU.mult),
                 reads=["Gm", "gpre"], writes=["Gm"])
            S.op("dve", lambda e: e.tensor_copy(out=Sh[:], in_=modF[:, 0:8, :]), reads=["modF"], writes=["Sh"])
            S.barrier()
            arena["off"] = mark0

        def proj_pass(pname, Wb, wkey, blocks, chunk_fn, ab_cols=None):
            xts = Rot([(sb("%s_xt%d" % (pname, i), [128, D]), "%s_xt%d" % (pname, i)) for i in range(4)])
            xrs = [[(sb("%s_xr%d_%d" % (pname, b, i), [128, D]), "%s_xr%d_%d" % (pname, b, i)) for i in range(4)] for b in range(2)]
            hTs = [(sb("%s_hT%d" % (pname, i), [128, 8, 512], BF16), "%s_hT%d" % (pname, i)) for i in range(2)]
            junk = sb(pname + "_junk", [128, D], BF16)
            ssq = [(sb("%s_ss%d" % (pname, i), [128, 4]), "%s_ss%d" % (pname, i)) for i in range(2)]
            stg = Rot([(sb("%s_stg%d" % (pname, i), [128, 512], BF16), "%s_stg%d" % (pname, i)) for i in range(6)])
            tmp = Rot([(sb("%s_tmp%d" % (pname, i), [128, 512]), "%s_tmp%d" % (pname, i)) for i in range(4)])
            tb = Rot([(banks[0], "bank0"), (banks[1], "bank1")])
            pb = Rot([(banks[2 + i], "bank%d" % (2 + i)) for i in range(4)])
            abb = Rot([(banks[6], "bank6"), (banks[7], "bank7")])
            abst = Rot([(sb("%s_abst%d" % (pname, i), [128, 4, 32]), "%s_abst%d" % (pname, i)) for i in range(2)])
            evq = Rot(["act", "dve"])

            def stage1(bi):
                blk = blocks[bi]
                nt = blk["ntok"] // 128
                ss, ssk = ssq[bi % 2]
                xtl = []
                for j in range(nt):
                    xt, xk = xts.next()
                    for (ap, p0, npart) in blk["loads"][j]:
                        S.op("sp", lambda e, xt=xt, ap=ap, p0=p0, npart=npart: e.dma_start(out=xt[p0:p0 + npart, :], in_=ap),
                             writes=[xk], dma=True)
                    S.op("act", lambda e, xt=xt, ss=ss, j=j: e.activation(out=junk[:], in_=xt[:], func=AF.Square, accum_out=ss[:, j:j + 1]),
                         reads=[xk], writes=[pname + "_junk", ssk])
                    xtl.append((xt, xk))
                S.op("dve", lambda e, ss=ss, nt=nt: e.tensor_scalar(out=ss[:, 0:nt], in0=ss[:, 0:nt], scalar1=1.0 / D, scalar2=EPS,
                                                                   op0=ALU.mult, op1=ALU.add), reads=[ssk], writes=[ssk])
                S.op("act", lambda e, ss=ss, nt=nt: e.activation(out=ss[:, 0:nt], in_=ss[:, 0:nt], func=AF.Sqrt), reads=[ssk], writes=[ssk])
                S.op("dve", lambda e, ss=ss, nt=nt: e.reciprocal(out=ss[:, 0:nt], in_=ss[:, 0:nt]), reads=[ssk], writes=[ssk])
                xrl = xrs[bi % 2]
                for j in range(nt):
                    xt, xk = xtl[j]
                    xr, xrk = xrl[j]
                    S.op("pool", lambda e, xr=xr, xt=xt, ss=ss, j=j: e.tensor_scalar(out=xr[:], in0=xt[:], scalar1=ss[:, j:j + 1], scalar2=0.0,
                                                                                    op0=ALU.mult, op1=ALU.add), reads=[xk, ssk], writes=[xrk])
                hT, hk = hTs[bi % 2]
                m = blk["mod"]
                for c in range(8):
                    bk, bkk = tb.next()
                    for j in range(nt):
                        xr, xrk = xrl[j]
                        S.op("pe", lambda e, bk=bk, xr=xr, c=c, j=j: e.transpose(out=bk[:, j * 128:(j + 1) * 128], in_=xr[:, c * 128:(c + 1) * 128],
                                                                                 identity=ident[:]), reads=[xrk, "ident"], writes=[bkk])
                    S.op("act", lambda e, bk=bk, hT=hT, c=c, nt=nt, m=m: e.activation(out=hT[:, c, 0:nt * 128], in_=bk[:, 0:nt * 128], func=AF.Identity,
                                                                                   scale=Gm[:, c, m:m + 1], bias=Sh[:, c, m:m + 1]),
                         reads=[bkk, "Gm", "Sh"], writes=[(hk, c)])

            def stage2(bi):
                blk = blocks[bi]
                ntok = blk["ntok"]
                hT, hk = hTs[bi % 2]
                for spec in chunk_fn(blk):
                    col0, kind, dst = spec
                    bk, bkk = pb.next()
                    for kc in range(8):
                        S.op("pe", lambda e, bk=bk, kc=kc, col0=col0: e.matmul(bk[:, 0:ntok], lhsT=Wb[:, kc, col0:col0 + 128], rhs=hT[:, kc, 0:ntok],
                                                                               start=(kc == 0), stop=(kc == 7)),
                             reads=[(wkey, kc), (hk, kc)], writes=[bkk])
                    sg, sgk = stg.next()
                    if kind == "copy":
                        q = evq.next()
                        if q == "act":
                            S.op("act", lambda e, sg=sg, bk=bk: e.activation(out=sg[:, 0:ntok], in_=bk[:, 0:ntok], func=AF.Identity),
                                 reads=[bkk], writes=[sgk])
                        else:
                            S.op("dve", lambda e, sg=sg, bk=bk: e.tensor_copy(out=sg[:, 0:ntok], in_=bk[:, 0:ntok]), reads=[bkk], writes=[sgk])
                    elif kind == "silu":
                        S.op("act", lambda e, sg=sg, bk=bk: e.activation(out=sg[:, 0:ntok], in_=bk[:, 0:ntok], func=AF.Silu), reads=[bkk], writes=[sgk])
                    elif kind == "sigmoid":
                        S.op("act", lambda e, sg=sg, bk=bk: e.activation(out=sg[:, 0:ntok], in_=bk[:, 0:ntok], func=AF.Sigmoid), reads=[bkk], writes=[sgk])
                    elif kind == "gelu":
                        t1, t1k = tmp.next()
                        S.op("act", lambda e, t1=t1, bk=bk: e.activation(out=t1[:, 0:ntok], in_=bk[:, 0:ntok], func=AF.Square), reads=[bkk], writes=[t1k])
                        S.op("dve", lambda e, t1=t1: e.tensor_scalar(out=t1[:, 0:ntok], in0=t1[:, 0:ntok], scalar1=0.044715, scalar2=1.0,
                                                                    op0=ALU.mult, op1=ALU.add), reads=[t1k], writes=[t1k])
                        S.op("dve", lambda e, t1=t1, bk=bk: e.tensor_tensor(out=t1[:, 0:ntok], in0=t1[:, 0:ntok], in1=bk[:, 0:ntok], op=ALU.mult),
                             reads=[t1k, bkk], writes=[t1k])
                        S.op("act", lambda e, t1=t1: e.activation(out=t1[:, 0:ntok], in_=t1[:, 0:ntok], func=AF.Sigmoid, scale=1.5957691216057308),
                             reads=[t1k], writes=[t1k])
                        S.op("dve", lambda e, t1=t1, bk=bk, sg=sg: e.tensor_tensor(out=sg[:, 0:ntok], in0=t1[:, 0:ntok], in1=bk[:, 0:ntok], op=ALU.mult),
                             reads=[t1k, bkk], writes=[sgk])
                    S.op("sp", lambda e, sg=sg, dst=dst: e.dma_start(out=dst, in_=sg[:, 0:ntok]), reads=[sgk], dma=True)
                if ab_cols is not None:
                    nt = ntok // 128
                    bk, bkk = abb.next()
                    for j in range(nt):
                        for kc in range(8):
                            S.op("pe", lambda e, bk=bk, kc=kc, j=j: e.matmul(bk[:, j * 32:(j + 1) * 32], lhsT=hT[:, kc, j * 128:(j + 1) * 128],
                                                                             rhs=Wb[:, kc, ab_cols:ab_cols + 32], start=(kc == 0), stop=(kc == 7)),
                                 reads=[(wkey, kc), (hk, kc)], writes=[bkk])
                    ab, abk = abst.next()
                    S.op("dve", lambda e, ab=ab, bk=bk, nt=nt: e.tensor_copy(out=ab[:, 0:nt, :], in_=bk[:, 0:nt * 32].rearrange("p (j c) -> p j c", c=32)),
                         reads=[bkk], writes=[abk])
                    t0 = blk["tokoff"]
                    S.op("sp", lambda e, ab=ab, nt=nt, t0=t0: e.dma_start(out=PAB[t0:t0 + nt * 128, :].rearrange("(j p) c -> p j c", p=128),
                                                                         in_=ab[:, 0:nt, :]), reads=[abk], dma=True)

            stage1(0)
            for bi in range(len(blocks)):
                if bi + 1 < len(blocks):
                    stage1(bi + 1)
                stage2(bi)

        WA = sb("WA", [128, 8, 5120], BF16)
        srcA = [(0, 0, 1024), (1024, 1024, 1024), (2048, 5120, 1024), (3072, 6176, 1024), (4096, 7200, 1024)]
        for (d0, s0, n) in srcA:
            for kc in range(8):
                S.op("pool", lambda e, kc=kc, d0=d0, s0=s0, n=n: e.dma_start(out=WA[:, kc, d0:d0 + n], in_=w_in[kc * 128:(kc + 1) * 128, s0:s0 + n]),
                     writes=[("WA", kc)], dma=True)
        blocksA = [dict(ntok=256, mod=1, tokoff=0, own=False,
                        loads=[[(ctx[j * 128:(j + 1) * 128, :], 0, 128)] for j in range(2)])]
        for bi in range(8):
            blocksA.append(dict(ntok=512, mod=0, tokoff=S_CTX + bi * 512, own=(bi < 4), lat0=bi * 512,
                                loads=[[(x[bi * 512 + j * 128: bi * 512 + (j + 1) * 128, :], 0, 128)] for j in range(4)]))

        def chunksA(blk):
            ntok, t0 = blk["ntok"], blk["tokoff"]
            specs = [(c * 128, "copy", PRG[c * 128:(c + 1) * 128, t0:t0 + ntok]) for c in range(8)]
            if blk["own"]:
                l0 = blk["lat0"]
                for gi, kind in enumerate(["gelu", "silu", "sigmoid", "sigmoid"]):
                    for c in range(8):
                        specs.append((1024 + gi * 1024 + c * 128, kind, GATES[gi * 1024 + c * 128: gi * 1024 + (c + 1) * 128, l0:l0 + ntok]))
            return specs

        if "pa" in phases:
            proj_pass("pa", WA, "WA", blocksA, chunksA)
        S.barrier()
        arena["off"] = mark0

        WB = sb("WB", [128, 8, 3104], BF16)
        srcB = [(0, 2048, 1024), (1024, 3072, 1024), (2048, 4096, 1024), (3072, 6144, 32)]
        for (d0, s0, n) in srcB:
            for kc in range(8):
                S.op("pool", lambda e, kc=kc, d0=d0, s0=s0, n=n: e.dma_start(out=WB[:, kc, d0:d0 + n], in_=w_in[kc * 128:(kc + 1) * 128, s0:s0 + n]),
                     writes=[("WB", kc)], dma=True)
        xcm = x.rearrange("(r c) d -> c r d", c=64)
        blocksB = [dict(ntok=256, mod=1, tokoff=0,
                        loads=[[(ctx[j * 128:(j + 1) * 128, :], 0, 128)] for j in range(2)])]
        for bi in range(8):
            blocksB.append(dict(ntok=512, mod=0, tokoff=S_CTX + bi * 512,
                                loads=[[(xcm[bi * 8 + j * 2 + h], h * 64, 64) for h in range(2)] for j in range(4)]))

        def chunksB(blk):
            ntok, t0 = blk["ntok"], blk["tokoff"]
            return [(c * 128, "copy", PQKV[c * 128:(c + 1) * 128, t0:t0 + ntok]) for c in range(24)]

        if "pb" in phases:
            proj_pass("pb", WB, "WB", blocksB, chunksB, ab_cols=3072)

        S.barrier()
        arena["off"] = mark0

        if "rg" in phases:
            XW = 4 + S_CTX + 4 + S_LAT + 4
            rgp_t = sb("rgp_t", [128, 8, 2, 8])
            S.op("sp", lambda e: e.dma_start(out=rgp_t[:], in_=rgp), writes=["rgp"], dma=True)
            c8 = sb("c8", [128, 8, 2])
            S.op("act", lambda e: e.activation(out=c8[:], in_=rgp_t[:, :, :, 7], func=AF.Exp, scale=-1.0), reads=["rgp"], writes=["c8"])
            S.op("act", lambda e: e.activation(out=c8[:], in_=c8[:], func=AF.Ln, bias=1.0), reads=["c8"], writes=["c8"])
            S.op("dve", lambda e: e.tensor_scalar(out=c8[:], in0=c8[:], scalar1=-8.0, scalar2=None, op0=ALU.mult), reads=["c8"], writes=["c8"])
            NSET = 2
            sets = []
            for si in range(NSET):
                sets.append(dict(
                    xc=sb("rg_xc%d" % si, [128, S_ALL]), ra=sb("rg_ra%d" % si, [128, S_ALL]),
                    ib=sb("rg_ib%d" % si, [128, S_ALL]), mh=sb("rg_mh%d" % si, [128, S_ALL]),
                    wa=sb("rg_wa%d" % si, [128, 128]), wi=sb("rg_wi%d" % si, [128, 128]), k="rgs%d" % si))
            xps = [(sb("rg_xp%d" % i, [128, XW], BF16), "rg_xp%d" % i) for i in range(2)]
            gts = [(sb("rg_gt%d" % i, [128, OWN], BF16), "rg_gt%d" % i) for i in range(2)]
            accs = [(sb("rg_acc%d" % i, [128, OWN]), "rg_acc%d" % i) for i in range(2)]
            xos = [(sb("rg_xo%d" % i, [128, OWN], BF16), "rg_xo%d" % i) for i in range(2)]
            pbk = Rot([(banks[i], "bank%d" % i) for i in range(8)])
            def rg_one(g, d, st_, xp, xpk, gt, gtk, acc, acck, xo, xok):
                sk = st_["k"]
                xc, ra, ib, mh, wa_t, wi_t = st_["xc"], st_["ra"], st_["ib"], st_["mh"], st_["wa"], st_["wi"]
                prm = rgp_t[:, g, d, :]
                for wt, src, wk in ((wa_t, rg_wa, sk + "wa"), (wi_t, rg_wi, sk + "wi")):
                    S.op("pool", lambda e, wt=wt: e.memset(wt[:], 0.0), writes=[wk])
                    for h in range(2):
                        S.op("sp", lambda e, wt=wt, src=src, h=h, g=g, d=d: e.dma_start(out=wt[h * 64:(h + 1) * 64, h * 64:(h + 1) * 64],
                                                                                   in_=src[d, 2 * g + h]), writes=[wk], dma=True)
                V = xp if d == 0 else xp[:, ::-1]
                ctx_off, lat_off = (4, 264) if d == 0 else (4104, 4)
                n_lat = OWN if d == 0 else S_LAT
                segs = [(ctx_off, 0, S_CTX)] + [(lat_off + i * 512, S_CTX + i * 512, 512) for i in range(n_lat // 512)]
                ntot = S_CTX + n_lat
                for (vo, o, n) in segs:
                    kx = (sk + "xc", o)
                    S.op("dve", lambda e, vo=vo, o=o, n=n: e.tensor_scalar(out=xc[:, o:o + n], in0=V[:, vo:vo + n], scalar1=prm[:, 3:4], scalar2=prm[:, 4:5],
                                                                          op0=ALU.mult, op1=ALU.add), reads=[xpk, "rgp"], writes=[kx])
                    for j in (2, 1, 0):
                        sh = 3 - j
                        S.op("dve", lambda e, vo=vo, o=o, n=n, j=j, sh=sh: e.scalar_tensor_tensor(out=xc[:, o:o + n], in0=V[:, vo - sh:vo - sh + n], scalar=prm[:, j:j + 1],
                                                                                                  in1=xc[:, o:o + n], op0=ALU.mult, op1=ALU.add),
                             reads=[xpk, "rgp", kx], writes=[kx])
                    b1, b1k = pbk.next()
                    b2, b2k = pbk.next()
                    S.op("pe", lambda e, b1=b1, o=o, n=n: e.matmul(b1[:, 0:n], lhsT=wa_t[:], rhs=xc[:, o:o + n], start=True, stop=True),
                         reads=[sk + "wa", kx], writes=[b1k])
                    S.op("pe", lambda e, b2=b2, o=o, n=n: e.matmul(b2[:, 0:n], lhsT=wi_t[:], rhs=xc[:, o:o + n], start=True, stop=True),
                         reads=[sk + "wi", kx], writes=[b2k])
                    S.op("act", lambda e, b1=b1, o=o, n=n: e.activation(out=ra[:, o:o + n], in_=b1[:, 0:n], func=AF.Sigmoid, bias=prm[:, 5:6]),
                         reads=[b1k, "rgp"], writes=[(sk + "ra", o)])
                    S.op("act", lambda e, b2=b2, o=o, n=n: e.activation(out=ib[:, o:o + n], in_=b2[:, 0:n], func=AF.Sigmoid, bias=prm[:, 6:7]),
                         reads=[b2k, "rgp"], writes=[(sk + "ib", o)])
                allr = [(sk + "ra", o) for (_, o, _) in segs]
                alli = [(sk + "ib", o) for (_, o, _) in segs]
                allx = [(sk + "xc", o) for (_, o, _) in segs]
                allm = [sk + "mh"]
                S.op("act", lambda e, g=g, d=d: e.activation(out=ra[:, 0:ntot], in_=ra[:, 0:ntot], func=AF.Exp, scale=c8[:, g, d:d + 1]),
                     reads=allr + ["c8"], writes=allr)
                S.op("pool", lambda e: e.tensor_tensor(out=mh[:, 0:ntot], in0=ra[:, 0:ntot], in1=ra[:, 0:ntot], op=ALU.mult),
                     reads=allr, writes=allm)
                S.op("act", lambda e: e.activation(out=mh[:, 0:ntot], in_=mh[:, 0:ntot], func=AF.Sqrt, scale=-1.0, bias=1.0),
                     reads=allm, writes=allm)
                S.op("pool", lambda e: e.memset(mh[:, 0:1], 1.0), reads=allm, writes=allm)
                S.op("pool", lambda e: e.tensor_tensor(out=ib[:, 0:ntot], in0=ib[:, 0:ntot], in1=mh[:, 0:ntot], op=ALU.mult),
                     reads=alli + allm, writes=alli)
                S.op("pool", lambda e: e.tensor_tensor(out=ib[:, 0:ntot], in0=ib[:, 0:ntot], in1=xc[:, 0:ntot], op=ALU.mult),
                     reads=alli + allx, writes=alli)
                S.op("dve", lambda e: e.tensor_tensor_scan(out=mh[:, 0:S_CTX], data0=ra[:, 0:S_CTX], data1=ib[:, 0:S_CTX], initial=0.0,
                                                           op0=ALU.mult, op1=ALU.add), reads=allr + alli + allm, writes=allm)
                for o in range(S_CTX, ntot, 1024):
                    S.op("dve", lambda e, o=o: e.tensor_tensor_scan(out=mh[:, o:o + 1024], data0=ra[:, o:o + 1024], data1=ib[:, o:o + 1024],
                                                                    initial=mh[:, o - 1:o], op0=ALU.mult, op1=ALU.add),
                         reads=allr + alli + allm, writes=allm)
                if g == 0:
                    if d == 0:
                        dbg("xp", xp[:, :], [xpk])
                    dbg("xc%d" % d, xc[:, 0:ntot], allx)
                    dbg("a%d" % d, ra[:, 0:ntot], allr)
                    dbg("b%d" % d, ib[:, 0:ntot], alli)
                    dbg("h%d" % d, mh[:, 0:ntot], allm)
                    dbg("c8_%d" % d, c8[:, 0, :], ["c8"])
                if d == 0:
                    S.op("pool", lambda e, acc=acc: e.tensor_copy(out=acc[:], in_=mh[:, S_CTX:S_CTX + OWN]), reads=allm, writes=[acck])
                else:
                    S.op("pool", lambda e, acc=acc: e.tensor_tensor(out=acc[:], in0=acc[:], in1=mh[:, S_CTX + OWN:S_ALL][:, ::-1], op=ALU.add),
                         reads=allm + [acck], writes=[acck])
                    S.op("pool", lambda e, acc=acc, gt=gt, xo=xo: e.tensor_tensor(out=xo[:], in0=acc[:], in1=gt[:], op=ALU.mult),
                         reads=[acck, gtk], writes=[xok])
                    S.op("sp", lambda e, xo=xo, g=g: e.dma_start(out=XRG[g * 128:(g + 1) * 128, :], in_=xo[:]), reads=[xok], dma=True)

            seti = 0
            for g in range(8):
                xp, xpk = xps[g % 2]
                gt, gtk = gts[g % 2]
                acc, acck = accs[g % 2]
                xo, xok = xos[g % 2]
                S.op("pool", lambda e, xp=xp: e.memset(xp[:, 0:4], 0.0), writes=[xpk])
                S.op("pool", lambda e, xp=xp: e.memset(xp[:, 260:264], 0.0), writes=[xpk])
                S.op("pool", lambda e, xp=xp: e.memset(xp[:, XW - 4:XW], 0.0), writes=[xpk])
                S.op("sp", lambda e, xp=xp, g=g: e.dma_start(out=xp[:, 4:260], in_=PRG[g * 128:(g + 1) * 128, 0:S_CTX]), writes=[xpk], dma=True)
                S.op("sp", lambda e, xp=xp, g=g: e.dma_start(out=xp[:, 264:264 + S_LAT], in_=PRG[g * 128:(g + 1) * 128, S_CTX:S_ALL]), writes=[xpk], dma=True)
                S.op("sp", lambda e, gt=gt, g=g: e.dma_start(out=gt[:], in_=GATES[g * 128:(g + 1) * 128, :]), writes=[gtk], dma=True)
                for d in range(2):
                    rg_one(g, d, sets[seti % NSET], xp, xpk, gt, gtk, acc, acck, xo, xok)
                    seti += 1
            S.barrier()
            arena["off"] = mark0

        def OP(eng, method, reads, writes, **kw):
            S.op(eng, lambda e: getattr(e, method)(**kw), reads=reads, writes=writes)

        def DMA(outap, inap, reads=(), writes=()):
            S.op("sp", lambda e: e.dma_start(out=outap, in_=inap), reads=reads, writes=writes, dma=True)

        class TP:
            def __init__(self, name, n, shape, dt):
                self.t = [(sb("%s%d" % (name, i), shape, dt), "%s%d" % (name, i)) for i in range(n)]
                self.i = 0

            def get(self):
                r = self.t[self.i % len(self.t)]
                self.i += 1
                return r

        if "dn" in phases:
            NEG = -30000.0
            XW = 4 + S_CTX + 4 + S_LAT + 4
            onesb = sb("onesb", [128, 128], BF16)
            onesf = sb("onesf", [128, 128])
            identb = sb("identb", [128, 128], BF16)
            LOW = sb("LOW", [128, 128])
            UPI = sb("UPI", [128, 128])
            NEGL = sb("NEGL", [128, 128])
            NEGU = sb("NEGU", [128, 128])
            H0 = sb("H0", [128, 128])
            H1 = sb("H1", [128, 128])
            OP("pool", "memset", [], ["onesb"], ap=onesb[:], constant=1.0)
            OP("pool", "memset", [], ["onesf"], ap=onesf[:], constant=1.0)
            OP("pool", "tensor_copy", ["ident"], ["identb"], out=identb[:], in_=ident[:])
            OP("pool", "affine_select", ["onesf"], ["LOW"], out=LOW[:], in_=onesf[:], pattern=[[-1, 128]], compare_op=ALU.is_gt, fill=0.0,
               base=0, channel_multiplier=1)
            OP("pool", "memset", ["LOW"], ["LOW"], ap=LOW[64:128, 0:64], constant=0.0)
            OP("pool", "affine_select", ["onesf"], ["UPI"], out=UPI[:], in_=onesf[:], pattern=[[1, 128]], compare_op=ALU.is_ge, fill=0.0,
               base=0, channel_multiplier=-1)
            OP("pool", "memset", ["UPI"], ["UPI"], ap=UPI[0:64, 64:128], constant=0.0)
            OP("dve", "tensor_scalar", ["LOW"], ["NEGL"], out=NEGL[:], in0=LOW[:], scalar1=-1.0, scalar2=-NEG, op0=ALU.add, op1=ALU.mult)
            OP("dve", "tensor_scalar", ["UPI"], ["NEGU"], out=NEGU[:], in0=UPI[:], scalar1=-1.0, scalar2=-NEG, op0=ALU.add, op1=ALU.mult)
            OP("pool", "memset", [], ["H0"], ap=H0[:], constant=0.0)
            OP("pool", "memset", ["H0"], ["H0"], ap=H0[0:64, :], constant=1.0)
            OP("pool", "memset", [], ["H1"], ap=H1[:], constant=0.0)
            OP("pool", "memset", ["H1"], ["H1"], ap=H1[64:128, :], constant=1.0)
            dncw_t = sb("dncw_t", [128, 24, 2, 4])
            dnc_t = sb("dnc_t", [128, 32])
            gnorm = sb("gnorm", [128, 1])
            negA = sb("negA", [128, 16])
            DMA(dncw_t[:], dncw, writes=["dncw"])
            DMA(dnc_t[:], dnc.partition_broadcast(128), writes=["dnc"])
            DMA(gnorm[:], dn_norm_g, writes=["gnorm"])
            OP("act", "activation", ["dnc"], ["negA"], out=negA[:], in_=dnc_t[:, 0:16], func=AF.Exp)
            OP("dve", "tensor_scalar", ["negA"], ["negA"], out=negA[:], in0=negA[:], scalar1=-1.0, scalar2=None, op0=ALU.mult)

            NP = S_ALL // 128
            GCt, Btt, EGLt, EGRt, BEGt = [], [], [], [], []
            with_tmp = arena["off"]
            ABt = sb("ABt", [128, NP, 32])
            ABr = sb("ABr", [128, NP, 32])
            Jm = sb("Jm", [128, 128])
            OP("pool", "memset", [], ["Jm"], ap=Jm[:], constant=0.0)
            OP("pool", "affine_select", ["Jm"], ["Jm"], out=Jm[:], in_=Jm[:], pattern=[[1, 128]], compare_op=ALU.not_equal, fill=1.0,
               base=-127, channel_multiplier=1)
            Zt = sb("Zt", [128, NP, 8])
            Lt = sb("Lt", [128, NP, 8])
            Gt = sb("Gt", [128, NP, 8])
            GLtok = sb("GLtok", [128, NP, 8])
            for d in range(2):
                GC = sb("GC%d" % d, [128, NP, 8]); Bt = sb("Bt%d" % d, [128, NP, 8]); EGL = sb("EGL%d" % d, [128, NP, 2, 8])
                EGR = sb("EGR%d" % d, [128, NP, 8]); BEG = sb("BEG%d" % d, [128, NP, 8])
                GCt.append(GC); Btt.append(Bt); EGLt.append(EGL); EGRt.append(EGR); BEGt.append(BEG)
            mark_dn = arena["off"]
            for d in range(2):
                GC, Bt, EGL, EGR, BEG = GCt[d], Btt[d], EGLt[d], EGRt[d], BEGt[d]
                k = "sc%d" % d
                if d == 0:
                    DMA(ABt[:, 0:2, :], PAB[0:S_CTX, :].rearrange("(n p) c -> p n c", p=128), writes=["ABt"])
                    DMA(ABt[:, 2:NP, :], PAB[S_CTX:S_ALL, :].rearrange("(n p) c -> p n c", p=128), writes=["ABt"])
                    ABs, ABk = ABt, "ABt"
                else:
                    ABf = ABt[:].rearrange("p n c -> p (n c)")
                    for bi_, (s0, s1) in enumerate(((0, 16), (16, 32), (32, 34))):
                        OP("pe", "matmul", ["Jm", "ABt"], ["bank%d" % (3 + bi_)], out=banks[3 + bi_][:, 0:(s1 - s0) * 32], lhsT=Jm[:], rhs=ABf[:, s0 * 32:s1 * 32],
                           start=True, stop=True)
                    b3v = banks[3][:, 0:512].rearrange("p (n c) -> p n c", c=32)
                    b4v = banks[4][:, 0:512].rearrange("p (n c) -> p n c", c=32)
                    b5v = banks[5][:, 0:64].rearrange("p (n c) -> p n c", c=32)
                    OP("dve", "tensor_copy", ["bank3"], ["ABr"], out=ABr[:, 0:2, :], in_=b3v[:, 0:2, :][:, ::-1, :])
                    OP("dve", "tensor_copy", ["bank3"], ["ABr"], out=ABr[:, 20:34, :], in_=b3v[:, 2:16, :][:, ::-1, :])
                    OP("dve", "tensor_copy", ["bank4"], ["ABr"], out=ABr[:, 4:20, :], in_=b4v[:, 0:16, :][:, ::-1, :])
                    OP("dve", "tensor_copy", ["bank5"], ["ABr"], out=ABr[:, 2:4, :], in_=b5v[:, 0:2, :][:, ::-1, :])
                    ABs, ABk = ABr, "ABr"
                dtb = dnc_t[:, 16 + d * 8:24 + d * 8].unsqueeze(1).to_broadcast([128, NP, 8])
                nab = negA[:, d * 8:(d + 1) * 8].unsqueeze(1).to_broadcast([128, NP, 8])
                OP("dve", "tensor_tensor", [ABk, "dnc"], ["Zt"], out=Zt[:], in0=ABs[:, :, d * 8:(d + 1) * 8], in1=dtb, op=ALU.add)
                OP("dve", "scalar_tensor_tensor", ["Zt"], ["Lt"], out=Lt[:].rearrange("p n c -> p (n c)"), in0=Zt[:].rearrange("p n c -> p (n c)"), scalar=-1.0,
                   in1=Zt[:].rearrange("p n c -> p (n c)"), op0=ALU.mult, op1=ALU.max)
                OP("act", "activation", ["Lt"], ["Lt"], out=Lt[:], in_=Lt[:], func=AF.Exp, scale=-1.0)
                OP("act", "activation", ["Lt"], ["Lt"], out=Lt[:], in_=Lt[:], func=AF.Ln, bias=1.0)
                OP("dve", "scalar_tensor_tensor", ["Zt", "Lt"], ["Gt"], out=Gt[:].rearrange("p n c -> p (n c)"), in0=Zt[:].rearrange("p n c -> p (n c)"),
                   scalar=0.0, in1=Lt[:].rearrange("p n c -> p (n c)"), op0=ALU.max, op1=ALU.add)
                OP("dve", "tensor_tensor", ["Gt", "negA"], ["Gt"], out=Gt[:], in0=Gt[:], in1=nab, op=ALU.mult)
                OP("act", "activation", [ABk], [k + "B"], out=Bt[:], in_=ABs[:, :, 16 + d * 8:24 + d * 8], func=AF.Sigmoid)
                Gf = Gt[:].rearrange("p n c -> p (n c)")
                W = NP * 8
                OP("pe", "matmul", ["UPI", "Gt"], ["bank0"], out=banks[0][:, 0:W], lhsT=UPI[:], rhs=Gf, start=True, stop=True)
                OP("pe", "matmul", ["H0", "Gt"], ["bank1"], out=banks[1][:, 0:W], lhsT=H0[:], rhs=Gf, start=True, stop=True)
                OP("pe", "matmul", ["H1", "Gt"], ["bank2"], out=banks[2][:, 0:W], lhsT=H1[:], rhs=Gf, start=True, stop=True)
                OP("dve", "tensor_copy", ["bank0"], [k + "GC"], out=GC[:].rearrange("p n c -> p (n c)"), in_=banks[0][:, 0:W])
                OP("act", "activation", ["bank1"], [k + "EGL"], out=EGL[:, :, 0, :], in_=banks[1][:, 0:W].rearrange("p (n c) -> p n c", c=8), func=AF.Exp)
                OP("act", "activation", ["bank2"], [k + "EGL"], out=EGL[:, :, 1, :], in_=banks[2][:, 0:W].rearrange("p (n c) -> p n c", c=8), func=AF.Exp)
                OP("dve", "tensor_copy", ["bank1"], ["GLtok"], out=GLtok[0:64].rearrange("p n c -> p (n c)"), in_=banks[1][0:64, 0:W])
                OP("dve", "tensor_copy", ["bank2", "GLtok"], ["GLtok"], out=GLtok[64:128].rearrange("p n c -> p (n c)"), in_=banks[2][64:128, 0:W])
                OP("dve", "tensor_tensor", ["GLtok", k + "GC"], ["GLtok"], out=GLtok[:], in0=GLtok[:], in1=GC[:], op=ALU.subtract)
                OP("act", "activation", ["GLtok"], [k + "EGR"], out=EGR[:], in_=GLtok[:], func=AF.Exp)
                OP("act", "activation", [k + "GC"], [k + "BEG"], out=BEG[:], in_=GC[:], func=AF.Exp)
                OP("dve", "tensor_tensor", [k + "BEG", k + "B"], [k + "BEG"], out=BEG[:], in0=BEG[:], in1=Bt[:], op=ALU.mult)
            S.barrier()
            arena["off"] = mark_dn

            xin = [[(sb("dn_x%d_%d" % (par, part), [128, XW], BF16), "dn_x%d_%d" % (par, part)) for part in range(3)] for par in range(1)]
            szs = [(sb("dn_sz%d" % i, [128, OWN], BF16), "dn_sz%d" % i) for i in range(2)]
            Obuf = [[(sb("dn_O%d_%d" % (par, d), [128, 64, 32]), "dn_O%d_%d" % (par, d)) for d in range(2)] for par in range(1)]
            xos = [(sb("dn_xo%d" % i, [128, OWN], BF16), "dn_xo%d" % i) for i in range(2)]
            F32Pd = [TP("dn_f%d" % d, 7, [128, 512], F32) for d in range(2)]
            BFPd = [TP("dn_b%d" % d, 16, [128, 512], BF16) for d in range(2)]
            VNP = [TP("dn_vn%d" % d, 4, [128, 128], BF16) for d in range(2)]
            h32s = [[sb("dn_h32_%d%d" % (hp, d), [128, 128]) for d in range(2)] for hp in range(2)]
            hbfs = [[[sb("dn_hbf%d%d_%d" % (hp, d, i), [128, 128], BF16) for i in range(2)] for d in range(2)] for hp in range(2)]
            pers = {}
            for d in range(2):
                for par in range(2):
                    pers[(d, par)] = dict(
                        qe=sb("dn_qe%d%d" % (d, par), [128, 512], BF16), qkt=sb("dn_qkt%d%d" % (d, par), [128, 4, 128], BF16),
                        u=sb("dn_u%d%d" % (d, par), [128, 4, 128]), wT=sb("dn_wT%d%d" % (d, par), [128, 512], BF16),
                        kd=sb("dn_kd%d%d" % (d, par), [128, 4, 128], BF16), k="dnp%d%d" % (d, par),
                        qn=sb("dn_qn%d%d" % (d, par), [128, 512], BF16), kn=sb("dn_kn%d%d" % (d, par), [128, 512], BF16),
                        vT=sb("dn_vT%d%d" % (d, par), [128, 512], BF16))
            dgs = [sb("dn_dg%d" % hp, [128, 24, 128], BF16) for hp in range(2)]
            print("DN arena used", arena["off"])
            prot = TP.__new__(TP)
            prot.t = [(banks[i], "bank%d" % i) for i in range(6)]
            prot.i = 0
            banks_bf = [bk.bitcast(BF16) for bk in banks]
            LNQ = -0.5 * float(np.log(128.0))

            tcount = {"t": 0}

            def run_rr(gens):
                gens = list(gens)
                while gens:
                    for g_ in list(gens):
                        try:
                            next(g_)
                        except StopIteration:
                            gens.remove(g_)

            def dn_head(hd, prev):
                par = hd % 2
                h32 = h32s[par]
                hbf = hbfs[par]
                xb = xin[0]
                sz, szk = szs[par]
                xo, xok = xos[par]
                for part in range(3):
                    xp, xpk = xb[part]
                    r0 = part * 1024 + hd * 128
                    OP("pool", "memset", [], [xpk], ap=xp[:, 0:4], constant=0.0)
                    OP("pool", "memset", [], [xpk], ap=xp[:, 260:264], constant=0.0)
                    OP("pool", "memset", [], [xpk], ap=xp[:, XW - 4:XW], constant=0.0)
                    DMA(xp[:, 4:260], PQKV[r0:r0 + 128, 0:S_CTX], writes=[xpk])
                    DMA(xp[:, 264:264 + S_LAT], PQKV[r0:r0 + 128, S_CTX:S_ALL], writes=[xpk])
                DMA(sz[:], GATES[1024 + hd * 128:1024 + (hd + 1) * 128, :], writes=[szk])
                dg = dgs[par]
                for part in range(3):
                    for d in range(2):
                        for j in range(4):
                            OP("pool", "tensor_scalar", ["identb", "dncw"], [("dg", par, part, d)], out=dg[:, (part * 2 + d) * 4 + j, :], in0=identb[:],
                               scalar1=dncw_t[:, part * 8 + hd, d, j:j + 1], scalar2=None, op0=ALU.mult)
                hstate = {}
                for d in range(2):
                    OP("pool", "memset", [], [("h32", par, d)], ap=h32[d][:], constant=0.0)
                    OP("pool", "memset", [], [("hbf", par, d, 0)], ap=hbf[d][0][:], constant=0.0)
                    hstate[d] = 0

                groups = [(0, 256)] + [(S_CTX + i * 512, 512) for i in range(8)]

                def prep(d, gi, tpar):
                    F32P, BFP = F32Pd[d], BFPd[d]
                    o, n = groups[gi]
                    npair = n // 128
                    p0 = o // 128
                    P = pers[(d, tpar)]
                    pk = P["k"]
                    GC, Bt, EGR, BEG = GCt[d], Btt[d], EGRt[d], BEGt[d]
                    ctx_off, lat_off = (4, 264) if d == 0 else (4104, 4)
                    base = (ctx_off + o) if o < S_CTX else (lat_off + o - S_CTX)
                    res = {}
                    for part in range(3):
                        xp, xpk = xb[part]
                        V = xp if d == 0 else xp[:, ::-1]
                        T1, T1k = prot.get()
                        for j in range(4):
                            sh = 3 - j
                            OP("pe", "matmul", [xpk, ("dg", par, part, d)], [T1k], out=T1[:, 0:n], lhsT=dg[:, (part * 2 + d) * 4 + j, :],
                               rhs=V[:, base - sh:base - sh + n], start=(j == 0), stop=(j == 3))
                        yield
                        if part == 2:
                            vT, vTk = P["vT"], (pk, "vT")
                            OP("act", "activation", [T1k], [vTk], out=vT[:, 0:n], in_=T1[:, 0:n], func=AF.Silu)
                            res["v"] = (vT, vTk)
                            continue
                        T2, T2k = F32P.get()
                        OP("act", "activation", [T1k], [T2k], out=T2[:, 0:n], in_=T1[:, 0:n], func=AF.Silu)
                        SQ, SQk = BFP.get()
                        OP("pool", "tensor_tensor", [T2k], [SQk], out=SQ[:, 0:n], in0=T2[:, 0:n], in1=T2[:, 0:n], op=ALU.mult)
                        bk, bkk = prot.get()
                        OP("pe", "matmul", ["onesb", SQk], [bkk], out=bk[:, 0:n], lhsT=onesb[:], rhs=SQ[:, 0:n], start=True, stop=True)
                        yield
                        RS, RSk = F32P.get()
                        OP("act", "activation", [bkk], [RSk], out=RS[:, 0:n], in_=bk[:, 0:n], func=AF.Ln, bias=EPS)
                        OP("act", "activation", [RSk], [RSk], out=RS[:, 0:n], in_=RS[:, 0:n], func=AF.Exp, scale=-0.5, bias=(LNQ if part == 0 else 0.0))
                        NN, NNk = (P["qn"], (pk, "qn")) if part == 0 else (P["kn"], (pk, "kn"))
                        OP("pool", "tensor_tensor", [T2k, RSk], [NNk], out=NN[:, 0:n], in0=T2[:, 0:n], in1=RS[:, 0:n], op=ALU.mult)
                        res["qk"[part]] = (NN, NNk)
                        yield
                    qn, qnk = res["q"]
                    kn, knk = res["k"]
                    vT, vTk = res["v"]
                    gcb = GC[:, p0:p0 + npair, hd].unsqueeze(2).to_broadcast([128, npair, 128])
                    v3 = lambda t: t[:, 0:n].rearrange("p (a b) -> p a b", b=128)
                    Rt, Rtk = F32P.get()
                    OP("dve", "tensor_tensor", ["ident", "sc%dGC" % d], [Rtk], out=v3(Rt), in0=ident[:].unsqueeze(1).to_broadcast([128, npair, 128]), in1=gcb, op=ALU.mult)
                    bg, bgk = prot.get()
                    OP("pe", "matmul", ["onesf", Rtk], [bgk], out=bg[:, 0:n], lhsT=onesf[:], rhs=Rt[:, 0:n], start=True, stop=True)
                    yield
                    EB, EBk = F32P.get()
                    OP("act", "activation", [bgk], [EBk], out=EB[:, 0:n], in_=bg[:, 0:n], func=AF.Exp)
                    OP("pool", "tensor_tensor", [qnk, EBk], [(pk, "qe")], out=P["qe"][:, 0:n], in0=qn[:, 0:n], in1=EB[:, 0:n], op=ALU.mult)
                    Dl, Dlk = F32P.get()
                    OP("dve", "tensor_tensor", ["sc%dGC" % d, bgk], [Dlk], out=v3(Dl), in0=gcb, in1=v3(bg), op=ALU.subtract)
                    OP("pool", "tensor_tensor", [Dlk, "NEGL"], [Dlk], out=v3(Dl), in0=v3(Dl), in1=NEGL[:].unsqueeze(1).to_broadcast([128, npair, 128]), op=ALU.add)
                    OP("act", "activation", [Dlk], [Dlk], out=Dl[:, 0:n], in_=Dl[:, 0:n], func=AF.Exp)
                    W1, W1k = BFP.get()
                    OP("pool", "tensor_tensor", [Dlk, "sc%dB" % d], [W1k], out=v3(W1), in0=v3(Dl),
                       in1=Bt[:, p0:p0 + npair, hd].unsqueeze(2).to_broadcast([128, npair, 128]), op=ALU.mult)
                    yield
                    Du, Duk = F32P.get()
                    OP("dve", "tensor_tensor", ["sc%dGC" % d, bgk], [Duk], out=v3(Du), in0=v3(bg), in1=gcb, op=ALU.subtract)
                    OP("pool", "tensor_tensor", [Duk, "NEGU"], [Duk], out=v3(Du), in0=v3(Du), in1=NEGU[:].unsqueeze(1).to_broadcast([128, npair, 128]), op=ALU.add)
                    EDT, EDTk = BFP.get()
                    OP("act", "activation", [Duk], [EDTk], out=EDT[:, 0:n], in_=Du[:, 0:n], func=AF.Exp)
                    yield
                    bkk_, bkkk = prot.get()
                    bqk, bqkk = prot.get()
                    for pi in range(npair):
                        cs_ = slice(pi * 128, (pi + 1) * 128)
                        OP("pe", "matmul", [knk], [bkkk], out=bkk_[:, cs_], lhsT=kn[:, cs_], rhs=kn[:, cs_], start=True, stop=True)
                        OP("pe", "matmul", [knk, qnk], [bqkk], out=bqk[:, cs_], lhsT=kn[:, cs_], rhs=qn[:, cs_], start=True, stop=True)
                    yield
                    Lm, Lmk = BFP.get()
                    OP("dve", "tensor_tensor", [bkkk, W1k], [Lmk], out=Lm[:, 0:n], in0=bkk_[:, 0:n], in1=W1[:, 0:n], op=ALU.mult)
                    OP("dve", "tensor_tensor", [bqkk, EDTk], [(pk, "qkt")], out=P["qkt"][:, 0:npair, :], in0=v3(bqk), in1=v3(EDT), op=ALU.mult)

                    def mm_pairs(lhs, lhsk, rhs, rhsk, trans=False):
                        bi_ = prot.i % 6
                        bk2, bk2k = prot.get()
                        for pi in range(npair):
                            cs_ = slice(pi * 128, (pi + 1) * 128)
                            if trans:
                                OP("pe", "transpose", [lhsk, "identb"], [bk2k], out=banks_bf[bi_][:, cs_], in_=lhs[:, cs_], identity=identb[:])
                            else:
                                OP("pe", "matmul", [lhsk, rhsk], [bk2k], out=bk2[:, cs_], lhsT=lhs[:, cs_], rhs=rhs[:, cs_], start=True, stop=True)
                        return (banks_bf[bi_] if trans else bk2), bk2k

                    def evac_copy(src, srck, eng="act"):
                        t, tk = BFP.get()
                        if eng == "act":
                            OP("act", "activation", [srck], [tk], out=t[:, 0:n], in_=src[:, 0:n], func=AF.Identity)
                        else:
                            OP("dve", "tensor_copy", [srck], [tk], out=t[:, 0:n], in_=src[:, 0:n])
                        return t, tk

                    yield
                    bu, buk = mm_pairs(Lm, Lmk, None, None, trans=True)
                    yield
                    Um, Umk = evac_copy(bu, buk, "act")
                    Pt, Ptk = BFP.get()
                    OP("pool", "tensor_tensor", ["identb", Umk], [Ptk], out=v3(Pt), in0=identb[:].unsqueeze(1).to_broadcast([128, npair, 128]), in1=v3(Um), op=ALU.subtract)
                    Lp, Lpk, Up, Upk = Lm, Lmk, Um, Umk
                    for lvl in range(1, 6):
                        yield
                        b1, b1k = mm_pairs(Up, Upk, Lp, Lpk)
                        if lvl < 5:
                            b2, b2k = mm_pairs(Lp, Lpk, Up, Upk)
                        yield
                        Ln_, Lnk = evac_copy(b1, b1k, "act")
                        if lvl < 5:
                            Un_, Unk = evac_copy(b2, b2k, "dve")
                        yield
                        b3, b3k = mm_pairs(Ln_, Lnk, Pt, Ptk)
                        yield
                        Pn, Pnk = BFP.get()
                        OP("dve", "tensor_tensor", [b3k, Ptk], [Pnk], out=Pn[:, 0:n], in0=b3[:, 0:n], in1=Pt[:, 0:n], op=ALU.add)
                        Pt, Ptk = Pn, Pnk
                        Lp, Lpk = Ln_, Lnk
                        if lvl < 5:
                            Up, Upk = Un_, Unk
                    Tt, Ttk = Pt, Ptk
                    yield
                    bkt, bktk = mm_pairs(kn, knk, None, None, trans=True)
                    bvt, bvtk = mm_pairs(vT, vTk, None, None, trans=True)
                    yield
                    kbe, kbek = BFP.get()
                    for pi in range(npair):
                        cs_ = slice(pi * 128, (pi + 1) * 128)
                        OP("act", "activation", [bktk, "sc%dBEG" % d], [kbek], out=kbe[:, cs_], in_=bkt[:, cs_], func=AF.Identity, scale=BEG[:, p0 + pi, hd:hd + 1])
                        OP("act", "activation", [bktk, "sc%dEGR" % d], [(pk, "kd")], out=P["kd"][:, pi, :], in_=bkt[:, cs_], func=AF.Identity, scale=EGR[:, p0 + pi, hd:hd + 1])
                    vb, vbk = BFP.get()
                    for pi in range(npair):
                        cs_ = slice(pi * 128, (pi + 1) * 128)
                        OP("act", "activation", [bvtk, "sc%dB" % d], [vbk], out=vb[:, cs_], in_=bvt[:, cs_], func=AF.Identity, scale=Bt[:, p0 + pi, hd:hd + 1])
                    yield
                    bu2, bu2k = mm_pairs(Tt, Ttk, vb, vbk)
                    bw, bwk = mm_pairs(kbe, kbek, Tt, Ttk)
                    yield
                    OP("dve", "tensor_copy", [bu2k], [(pk, "u")], out=P["u"][:, 0:npair, :], in_=v3(bu2))
                    OP("act", "activation", [bwk], [(pk, "wT")], out=P["wT"][:, 0:n], in_=bw[:, 0:n], func=AF.Identity)

                def rec(gi, tpar):
                    o, n = groups[gi]
                    nch = n // 64
                    p0 = o // 128
                    is_lat = o >= S_CTX
                    for ci in range(nch):
                        pi, hf = ci // 2, ci % 2
                        Ps = slice(hf * 64, hf * 64 + 64)
                        for d in range(2):
                            P = pers[(d, tpar)]
                            pk = P["k"]
                            cur = hstate[d]
                            nxt = 1 - cur
                            hb_c, hb_n = hbf[d][cur], hbf[d][nxt]
                            bwh, bwhk = prot.get()
                            OP("pe", "matmul", [(pk, "wT"), ("hbf", par, d, cur)], [bwhk], out=bwh[Ps, 0:128], lhsT=P["wT"][:, ci * 64:(ci + 1) * 64], rhs=hb_c[:],
                               start=True, stop=True)
                            vn, vnk = VNP[d].get()
                            OP("dve", "tensor_tensor", [(pk, "u"), bwhk], [vnk], out=vn[Ps, :], in0=P["u"][Ps, pi, :], in1=bwh[Ps, 0:128], op=ALU.subtract)
                            if is_lat:
                                ob = banks[6 + d]
                                OP("pe", "matmul", [("hbf", par, d, cur), (pk, "qe")], [("obank", d)], out=ob[:, ci * 64:(ci + 1) * 64], lhsT=hb_c[:],
                                   rhs=P["qe"][:, ci * 64:(ci + 1) * 64], start=True, stop=False)
                                OP("pe", "matmul", [vnk, (pk, "qkt")], [("obank", d)], out=ob[:, ci * 64:(ci + 1) * 64], lhsT=vn[Ps, :],
                                   rhs=P["qkt"][Ps, pi, hf * 64:hf * 64 + 64], start=False, stop=True)
                            bh, bhk = prot.get()
                            OP("pe", "matmul", [(pk, "kd"), vnk], [bhk], out=bh[:, 0:128], lhsT=P["kd"][Ps, pi, :], rhs=vn[Ps, :], start=True, stop=True)
                            egl = EGLt[d][:, p0 + pi, hf, hd:hd + 1]
                            OP("dve", "scalar_tensor_tensor", [("h32", par, d), bhk, "sc%dEGL" % d], [("hbf", par, d, nxt)], out=hb_n[:], in0=h32[d][:], scalar=egl,
                               in1=bh[:, 0:128], op0=ALU.mult, op1=ALU.add)
                            OP("dve", "scalar_tensor_tensor", [("h32", par, d), bhk, "sc%dEGL" % d], [("h32", par, d)], out=h32[d][:], in0=h32[d][:], scalar=egl,
                               in1=bh[:, 0:128], op0=ALU.mult, op1=ALU.add)
                            hstate[d] = nxt
                            yield
                    if is_lat:
                        cl0 = (o - S_CTX) // 64
                        for d in range(2):
                            Ob, Obk = Obuf[0][d]
                            src = banks[6 + d][:, 0:512].rearrange("p (c i) -> p c i", i=64)
                            if d == 0:
                                OP("act", "activation", [("obank", d)], [Obk], out=Ob[:, cl0:cl0 + 8, :], in_=src[:, :, 0:32], func=AF.Identity)
                            else:
                                hi = 63 - cl0
                                OP("act", "activation", [("obank", d)], [Obk], out=Ob[:, hi - 7:hi + 1, :][:, ::-1, :], in_=src[:, :, 32:64][:, :, ::-1], func=AF.Identity)

                def finalize():
                  F32P, BFP = F32Pd[0], BFPd[0]
                  O0, O0k = Obuf[0][0]
                  O1, O1k = Obuf[0][1]
                  Of = O0[:].rearrange("p c r -> p (c r)")
                  OP("pool", "tensor_tensor", [O0k, O1k], [O0k], out=Of, in0=Of, in1=O1[:].rearrange("p c r -> p (c r)"), op=ALU.add)
                  for q4 in range(4):
                      cs_ = slice(q4 * 512, (q4 + 1) * 512)
                      SQ, SQk = BFP.get()
                      OP("pool", "tensor_tensor", [O0k], [SQk], out=SQ[:], in0=Of[:, cs_], in1=Of[:, cs_], op=ALU.mult)
                      bk, bkk = prot.get()
                      OP("pe", "matmul", ["onesb", SQk], [bkk], out=bk[:], lhsT=onesb[:], rhs=SQ[:], start=True, stop=True)
                      RS, RSk = F32P.get()
                      OP("act", "activation", [bkk], [RSk], out=RS[:], in_=bk[:], func=AF.Ln, scale=1.0 / 128, bias=EPS)
                      OP("act", "activation", [RSk], [RSk], out=RS[:], in_=RS[:], func=AF.Exp, scale=-0.5)
                      OP("dve", "scalar_tensor_tensor", [O0k, RSk, "gnorm"], [O1k], out=O1[:].rearrange("p c r -> p (c r)")[:, cs_], in0=Of[:, cs_],
                         scalar=gnorm[:, 0:1], in1=RS[:], op0=ALU.mult, op1=ALU.mult)
                  OP("pool", "tensor_tensor", [O1k, szk], [xok], out=xo[:].rearrange("p (r c) -> p r c", c=64), in0=O1[:].rearrange("p c r -> p r c"),
                     in1=sz[:].rearrange("p (r c) -> p r c", c=64), op=ALU.mult)
                  DMA(XDN[hd * 128:(hd + 1) * 128, :], xo[:], reads=[xok])

                pending = prev
                for gi in range(len(groups)):
                    tpar = tcount["t"] % 2
                    tcount["t"] += 1
                    gens = [prep(0, gi, tpar), prep(1, gi, tpar)]
                    if pending is not None:
                        gens.append(pending["rec"])
                    run_rr(gens)
                    if pending is not None and pending.get("fin") is not None:
                        pending["fin"]()
                    pending = dict(rec=rec(gi, tpar), fin=(finalize if gi == len(groups) - 1 else None))
                return pending

            pend = None
            for hd in dn_heads:
                pend = dn_head(hd, pend)
            run_rr([pend["rec"]])
            pend["fin"]()
            S.barrier()
            arena["off"] = mark0

        top = {"off": ARENA_WORDS}

        def sb_top(name, shape, dt=F32):
            shape = list(shape)
            elems = int(np.prod(shape[1:]))
            words = elems if dt == F32 else (elems + 1) // 2
            words += words % 2
            top["off"] -= words
            off = top["off"]
            ap = arena_t[0:shape[0], off:off + words]
            if dt != F32:
                ap = ap.bitcast(dt)[:, 0:elems]
            else:
                ap = ap[:, 0:elems]
            if len(shape) == 3:
                ap = ap.rearrange("p (a b) -> p a b", a=shape[1])
            return ap

        NT = OWN // 128
        if "tail" in phases:
            hfT = sb_top("hfT", [128, 8, OWN], BF16)
            CW = sb_top("CW", [128, NT, NE])
            G2P = sb_top("G2P", [128, D])
            prot = TP.__new__(TP)
            prot.t = [(banks[i], "bank%d" % i) for i in range(8)]
            prot.i = 0
            G1P = sb("G1P", [128, D])
            GP2 = sb("GP2", [128, D])
            SH2t = sb("SH2t", [128, D])
            rb = sb("rb", [128, NE])
            rw = sb("rw", [128, 8, NE])
            mark_t = arena["off"]
            MOD = sb("MOD", [128, 4, D])
            rows = sb("rows", [128, 3, D])
            csrep = sb("csrep", [128, 8, 128])
            adab_r = sb("adab_r", [128, 4 * D])
            DMA(adab_r[:], ada_b[:, 2048:6144].partition_broadcast(128), writes=["adab_r"])
            DMA(rows[:, 0, :], gpost.partition_broadcast(128), writes=["rows"])
            DMA(rows[:, 1, :], gfpre.partition_broadcast(128), writes=["rows"])
            DMA(rows[:, 2, :], gfpost.partition_broadcast(128), writes=["rows"])
            DMA(rb[:], router_b.partition_broadcast(128), writes=["rb"])
            DMA(rw[:], router_w.rearrange("(kc p) e -> p kc e", p=128), writes=["rw"])
            for kc in range(8):
                OP("dve", "tensor_copy", [], [("csrep", kc)], out=csrep[:, kc, :], in_=cs[:, kc, 0:1].to_broadcast([128, 128]))
            awp = TP("awp", 2, [128, 8, 512], F32)
            for pc_ in range(8):
                aw, awk = awp.get()
                for kc in range(8):
                    DMA(aw[:, kc, :], ada_w[kc * 128:(kc + 1) * 128, 2048 + pc_ * 512:2048 + (pc_ + 1) * 512], writes=[(awk, kc)])
                bk, bkk = prot.get()
                for kc in range(8):
                    OP("pe", "matmul", [(awk, kc), ("csrep", kc)], [bkk], out=bk[:], lhsT=csrep[:, kc, :], rhs=aw[:, kc, :], start=(kc == 0), stop=(kc == 7))
                OP("dve", "tensor_tensor", [bkk, "adab_r"], ["MOD"], out=MOD[:].rearrange("p j d -> p (j d)")[:, pc_ * 512:(pc_ + 1) * 512], in0=bk[:],
                   in1=adab_r[:, pc_ * 512:(pc_ + 1) * 512], op=ALU.add)
            OP("dve", "tensor_tensor", ["MOD", "rows"], ["G1P"], out=G1P[:], in0=MOD[:, 0, :], in1=rows[:, 0, :], op=ALU.mult)
            OP("dve", "scalar_tensor_tensor", ["MOD", "rows"], ["GP2"], out=GP2[:], in0=MOD[:, 2, :], scalar=1.0, in1=rows[:, 1, :], op0=ALU.add, op1=ALU.mult)
            OP("dve", "tensor_tensor", ["MOD", "rows"], ["G2P"], out=G2P[:], in0=MOD[:, 3, :], in1=rows[:, 2, :], op=ALU.mult)
            OP("pool", "tensor_copy", ["MOD"], ["SH2t"], out=SH2t[:], in_=MOD[:, 1, :])
            SH2 = SH2t[:]
            S.barrier()
            arena["off"] = mark_t
            n_blk = 4 if tail_stop >= 2 else 0
            Wrg = sb("Wrg", [128, 8, D], BF16)
            Wdn = sb("Wdn", [128, 8, D], BF16)
            Wou = sb("Wou", [128, 8, D], BF16)
            for (wt, src, wk) in ((Wrg, rg_w_o, "Wrg"), (Wdn, dn_w_o, "Wdn"), (Wou, w_out, "Wou")):
                for kc in range(8):
                    S.op("pool", lambda e, wt=wt, src=src, kc=kc: e.dma_start(out=wt[:, kc, :], in_=src[kc * 128:(kc + 1) * 128, :]), writes=[(wk, kc)], dma=True)
            xrb = sb("t_xrg", [128, 8, 512], BF16)
            xdb = sb("t_xdn", [128, 8, 512], BF16)
            s1b = sb("t_sg1", [128, 8, 512], BF16)
            s2b = sb("t_sg2", [128, 8, 512], BF16)
            mrg = sb("t_mrg", [128, 8, 512], BF16)
            F4 = TP("t_f4", 6, [128, D], F32)
            F2 = TP("t_f2", 4, [128, 512], F32)
            hf32 = TP("t_hf32", 2, [128, 8, 128], F32)
            sst = TP("t_ss", 4, [128, 8], F32)
            junk = sb("t_junk", [128, D], BF16)
            print("TAIL arena bottom", arena["off"], "top", top["off"])
            assert arena["off"] <= top["off"], "tail arena overlap"
            for blk in range(n_blk):
                t0 = blk * 512
                for kc in range(8):
                    DMA(xrb[:, kc, :], XRG[kc * 128:(kc + 1) * 128, t0:t0 + 512], writes=[("xrb", kc)])
                    DMA(xdb[:, kc, :], XDN[kc * 128:(kc + 1) * 128, t0:t0 + 512], writes=[("xdb", kc)])
                    DMA(s1b[:, kc, :], GATES[2048 + kc * 128:2048 + (kc + 1) * 128, t0:t0 + 512], writes=[("s1b", kc)])
                    DMA(s2b[:, kc, :], GATES[3072 + kc * 128:3072 + (kc + 1) * 128, t0:t0 + 512], writes=[("s2b", kc)])
                for m in range(8):
                    b1, b1k = prot.get()
                    b2, b2k = prot.get()
                    for kc in range(8):
                        OP("pe", "matmul", [("Wrg", kc), ("xrb", kc)], [b1k], out=b1[:], lhsT=Wrg[:, kc, m * 128:(m + 1) * 128], rhs=xrb[:, kc, :], start=(kc == 0), stop=(kc == 7))
                    for kc in range(8):
                        OP("pe", "matmul", [("Wdn", kc), ("xdb", kc)], [b2k], out=b2[:], lhsT=Wdn[:, kc, m * 128:(m + 1) * 128], rhs=xdb[:, kc, :], start=(kc == 0), stop=(kc == 7))
                    ta, tak = F2.get()
                    tb_, tbk = F2.get()
                    OP("dve", "tensor_tensor", [b1k, ("s1b", m)], [tak], out=ta[:], in0=b1[:], in1=s1b[:, m, :], op=ALU.mult)
                    OP("dve", "tensor_tensor", [b2k, ("s2b", m)], [tbk], out=tb_[:], in0=b2[:], in1=s2b[:, m, :], op=ALU.mult)
                    OP("pool", "tensor_tensor", [tak, tbk], [("mrg", m)], out=mrg[:, m, :], in0=ta[:], in1=tb_[:], op=ALU.add)
                for tt in range(4 if tail_stop >= 3 else 0):
                    gt_ = blk * 4 + tt
                    r0 = gt_ * 128
                    xt, xtk = F4.get()
                    DMA(xt[:], x[r0:r0 + 128, :], writes=[xtk])
                    ss, ssk = sst.get()
                    yb = []
                    for n2 in range(2):
                        bk, bkk = prot.get()
                        for kc in range(8):
                            OP("pe", "matmul", [("Wou", kc), ("mrg", kc)], [bkk], out=bk[:], lhsT=mrg[:, kc, tt * 128:(tt + 1) * 128], rhs=Wou[:, kc, n2 * 512:(n2 + 1) * 512],
                               start=(kc == 0), stop=(kc == 7))
                        OP("act", "activation", [bkk], ["t_junk", (ssk, n2)], out=junk[:, 0:512], in_=bk[:], func=AF.Square, accum_out=ss[:, n2:n2 + 1])
                        yb.append((bk, bkk))
                    if tail_stop < 3.2:
                        continue
                    OP("dve", "tensor_tensor", [(ssk, 0), (ssk, 1)], [(ssk, 2)], out=ss[:, 2:3], in0=ss[:, 0:1], in1=ss[:, 1:2], op=ALU.add)
                    OP("dve", "tensor_scalar", [(ssk, 2)], [(ssk, 2)], out=ss[:, 2:3], in0=ss[:, 2:3], scalar1=1.0 / D, scalar2=EPS, op0=ALU.mult, op1=ALU.add)
                    OP("act", "activation", [(ssk, 2)], [(ssk, 2)], out=ss[:, 2:3], in_=ss[:, 2:3], func=AF.Sqrt)
                    OP("dve", "reciprocal", [(ssk, 2)], [(ssk, 2)], out=ss[:, 2:3], in_=ss[:, 2:3])
                    xn, xnk = F4.get()
                    for n2 in range(2):
                        bk, bkk = yb[n2]
                        cs_ = slice(n2 * 512, (n2 + 1) * 512)
                        OP("dve", "scalar_tensor_tensor", [bkk, (ssk, 2), "G1P"], [(xnk, n2)], out=xn[:, cs_], in0=bk[:], scalar=ss[:, 2:3], in1=G1P[:, cs_], op0=ALU.mult, op1=ALU.mult)
                    OP("pool", "tensor_tensor", [(xnk, 0), (xnk, 1), xtk], [xnk, (xnk, 0), (xnk, 1)], out=xn[:], in0=xn[:], in1=xt[:], op=ALU.add)
                    if tail_stop < 3.4:
                        continue
                    DMA(XNEW[r0:r0 + 128, :], xn[:], reads=[xnk])
                    OP("act", "activation", [xnk], ["t_junk", (ssk, 3)], out=junk[:], in_=xn[:], func=AF.Square, accum_out=ss[:, 3:4])
                    OP("dve", "tensor_scalar", [(ssk, 3)], [(ssk, 3)], out=ss[:, 3:4], in0=ss[:, 3:4], scalar1=1.0 / D, scalar2=EPS, op0=ALU.mult, op1=ALU.add)
                    OP("act", "activation", [(ssk, 3)], [(ssk, 3)], out=ss[:, 3:4], in_=ss[:, 3:4], func=AF.Sqrt)
                    OP("dve", "reciprocal", [(ssk, 3)], [(ssk, 3)], out=ss[:, 3:4], in_=ss[:, 3:4])
                    if tail_stop < 3.6:
                        continue
                    hf, hfk = F4.get()
                    OP("dve", "scalar_tensor_tensor", [xnk, (ssk, 3), "GP2"], [hfk], out=hf[:], in0=xn[:], scalar=ss[:, 3:4], in1=GP2[:], op0=ALU.mult, op1=ALU.mult)
                    OP("pool", "tensor_tensor", [hfk, "SH2t"], [hfk], out=hf[:], in0=hf[:], in1=SH2, op=ALU.add)
                    if tail_stop < 3.8:
                        continue
                    h32t, h32k = hf32.get()
                    for half in range(2):
                        bk, bkk = prot.get()
                        for c4 in range(4):
                            c = half * 4 + c4
                            OP("pe", "transpose", [hfk, "ident"], [bkk], out=bk[:, c4 * 128:(c4 + 1) * 128], in_=hf[:, c * 128:(c + 1) * 128], identity=ident[:])
                        OP("dve", "tensor_copy", [bkk], [(h32k, half)], out=h32t[:, half * 4:half * 4 + 4, :], in_=bk[:].rearrange("p (c t) -> p c t", c=4))
                        OP("pool", "tensor_copy", [(h32k, half)], [("hfT", gt_, half)], out=hfT[:, half * 4:half * 4 + 4, r0:r0 + 128], in_=h32t[:, half * 4:half * 4 + 4, :])
                    if tail_stop < 4:
                        continue
                    bl, blk_ = prot.get()
                    for kc in range(8):
                        OP("pe", "matmul", [(h32k, kc // 4), "rw"], [blk_], out=bl[:, 0:NE], lhsT=h32t[:, kc, :], rhs=rw[:, kc, :], start=(kc == 0), stop=(kc == 7))
                    lg, lgk = F2.get()
                    OP("dve", "tensor_tensor", [blk_, "rb"], [lgk], out=lg[:, 0:NE], in0=bl[:, 0:NE], in1=rb[:], op=ALU.add)
                    OP("dve", "max", [lgk], [(lgk, "m8")], out=lg[:, 64:72], in_=lg[:, 0:NE])
                    OP("dve", "tensor_scalar", [lgk, (lgk, "m8")], [(lgk, "mask")], out=lg[:, 128:128 + NE], in0=lg[:, 0:NE], scalar1=lg[:, 67:68], scalar2=None, op0=ALU.is_ge)
                    OP("dve", "tensor_scalar", [(lgk, "m8")], [(lgk, "nm")], out=lg[:, 72:73], in0=lg[:, 64:65], scalar1=-1.0, scalar2=None, op0=ALU.mult)
                    OP("act", "activation", [lgk, (lgk, "nm")], [(lgk, "e")], out=lg[:, 192:192 + NE], in_=lg[:, 0:NE], func=AF.Exp, bias=lg[:, 72:73])
                    OP("dve", "tensor_tensor", [(lgk, "e"), (lgk, "mask")], [(lgk, "em")], out=lg[:, 256:256 + NE], in0=lg[:, 192:192 + NE], in1=lg[:, 128:128 + NE], op=ALU.mult)
                    OP("dve", "tensor_reduce", [(lgk, "em")], [(lgk, "sum")], out=lg[:, 73:74], in_=lg[:, 256:256 + NE], axis=mybir.AxisListType.X, op=ALU.add)
                    OP("dve", "reciprocal", [(lgk, "sum")], [(lgk, "sum")], out=lg[:, 73:74], in_=lg[:, 73:74])
                    OP("dve", "tensor_scalar", [(lgk, "em"), (lgk, "sum")], [("CW", gt_)], out=CW[:, gt_, :], in0=lg[:, 256:256 + NE], scalar1=lg[:, 73:74], scalar2=None, op0=ALU.mult)
            if debug:
                dbg("CW", CW[:], [("CW", i) for i in range(NT)])
                dbg("hfT", hfT[:, :, :], [("hfT", i, h) for i in range(NT) for h in range(2)])
            S.barrier()
            arena["off"] = mark0

        if "moe" in phases:
            prot = TP.__new__(TP)
            prot.t = [(banks[i], "bank%d" % i) for i in range(8)]
            prot.i = 0
            acc = sb("acc", [128, NT, D])
            b1T = sb("b1T", [128, NE, 16])
            mark_m = arena["off"]
            eb2 = sb("eb2", [NE, D])
            cwT = sb("cwT", [NE, NT, 128])
            DMA(b1T[:], e_b1T, writes=["b1T"])
            DMA(eb2[:], e_b2, writes=["eb2"])
            for tt in range(NT):
                bk, bkk = prot.get()
                OP("pe", "transpose", [], [bkk], out=bk[0:NE, 0:128], in_=CW[:, tt, :], identity=ident[:])
                OP("dve", "tensor_copy", [bkk], [("cwT", tt)], out=cwT[:, tt, :], in_=bk[0:NE, 0:128])
                for n2 in range(2):
                    bk2, bk2k = prot.get()
                    OP("pe", "matmul", [("cwT", tt), "eb2"], [bk2k], out=bk2[:], lhsT=cwT[:, tt, :], rhs=eb2[:, n2 * 512:(n2 + 1) * 512], start=True, stop=True)
                    OP("act", "activation", [bk2k], [("acc", tt, n2)], out=acc[:, tt, n2 * 512:(n2 + 1) * 512], in_=bk2[:], func=AF.Identity)
            S.barrier()
            arena["off"] = mark_m
            w1b = sb("w1b", [128, 8, 2 * D], BF16)
            w2b = sb("w2b", [128, 8, D], BF16)
            stg = TP("m_stg", 4, [128, D], F32)
            stg2 = TP("m_stg2", 2, [128, D], F32)
            actT = sb("actT", [128, 8, OWN // 2], BF16)
            EF = TP("m_ef", 6, [128, 512], F32)
            print("MOE arena bottom", arena["off"], "top", top["off"])
            assert arena["off"] <= top["off"], "moe arena overlap"
            n_exp = moe_experts
            w1v = w1b[:].rearrange("p kc (g m c) -> p kc g m c", g=2, m=8)
            w1_stage = {}
            w1_next = {}

            def w1_dma(e_):
                u = w1_next.get(e_, 0)
                if u >= 16:
                    return
                w1_next[e_] = u + 1
                m, g = u // 2, u % 2
                sg, sgk = stg.get()
                src = e_w1[e_].rearrange("(kc p) (g m c) -> p kc g m c", p=128, g=2, m=8)[:, :, g, m, :]
                DMA(sg[:].rearrange("p (kc c) -> p kc c", kc=8), src, writes=[sgk])
                w1_stage[(e_, u)] = (sg, sgk)

            def w1_cast(e_, u):
                m, g = u // 2, u % 2
                sg, sgk = w1_stage.pop((e_, u))
                OP("act", "activation", [sgk], [("w1b", m, g)], out=w1v[:, :, g, m, :], in_=sg[:].rearrange("p (kc c) -> p kc c", kc=8),
                   func=AF.Identity)

            def w2_load(e_):
                for kc in range(8):
                    sg, sgk = stg2.get()
                    DMA(sg[:], e_w2[e_, kc * 128:(kc + 1) * 128, :], writes=[sgk])
                    OP("pool", "tensor_copy", [sgk], [("w2b", kc)], out=w2b[:, kc, :], in_=sg[:])

            for u in range(16):
                w1_dma(0)
                w1_cast(0, u)
            for e_ in range(n_exp):
                w2_load(e_)
                nxt = e_ + 1 if e_ + 1 < n_exp else None
                for half in range(2):
                    h0 = half * (OWN // 2)
                    for m in range(8):
                        for nt_ in range(2):
                            tk0 = h0 + nt_ * 512
                            bg, bgk = prot.get()
                            bl, blk_ = prot.get()
                            for kc in range(8):
                                OP("pe", "matmul", [("w1b", m, 0)], [bgk], out=bg[:], lhsT=w1b[:, kc, m * 128:(m + 1) * 128], rhs=hfT[:, kc, tk0:tk0 + 512], start=(kc == 0), stop=(kc == 7))
                            for kc in range(8):
                                OP("pe", "matmul", [("w1b", m, 1)], [blk_], out=bl[:], lhsT=w1b[:, kc, D + m * 128:D + (m + 1) * 128], rhs=hfT[:, kc, tk0:tk0 + 512], start=(kc == 0), stop=(kc == 7))
                            tg, tgk = EF.get()
                            ts, tsk = EF.get()
                            tl, tlk = EF.get()
                            OP("dve", "tensor_scalar", [bgk, "b1T"], [tgk], out=tg[:], in0=bg[:], scalar1=b1T[:, e_, m:m + 1], scalar2=7.0, op0=ALU.add, op1=ALU.min)
                            OP("act", "activation", [tgk], [tsk], out=ts[:], in_=tg[:], func=AF.Sigmoid, scale=1.702)
                            OP("dve", "tensor_scalar", [blk_, "b1T"], [tlk], out=tl[:], in0=bl[:], scalar1=b1T[:, e_, 8 + m:9 + m], scalar2=7.0, op0=ALU.add, op1=ALU.min)
                            OP("dve", "tensor_scalar", [tlk], [tlk], out=tl[:], in0=tl[:], scalar1=-7.0, scalar2=1.0, op0=ALU.max, op1=ALU.add)
                            OP("pool", "tensor_tensor", [tgk, tsk], [tgk], out=tg[:], in0=tg[:], in1=ts[:], op=ALU.mult)
                            OP("pool", "tensor_tensor", [tgk, tlk], [("actT", m, nt_)], out=actT[:, m, nt_ * 512:(nt_ + 1) * 512], in0=tg[:], in1=tl[:], op=ALU.mult)
                        if nxt is not None:
                            if half == 0 and m == 7:
                                for _ in range(3):
                                    w1_dma(nxt)
                            if half == 1:
                                w1_cast(nxt, 2 * m)
                                w1_dma(nxt)
                                w1_cast(nxt, 2 * m + 1)
                                w1_dma(nxt)
                    for tt in range(8):
                        gt_ = half * 8 + tt
                        for n2 in range(2):
                            bk, bkk = prot.get()
                            for m in range(8):
                                OP("pe", "matmul", [("actT", m, tt // 4), ("w2b", m)], [bkk], out=bk[:], lhsT=actT[:, m, tt * 128:(tt + 1) * 128], rhs=w2b[:, m, n2 * 512:(n2 + 1) * 512],
                                   start=(m == 0), stop=(m == 7))
                            OP("dve", "scalar_tensor_tensor", [bkk, ("acc", gt_, n2)], [("acc", gt_, n2)], out=acc[:, gt_, n2 * 512:(n2 + 1) * 512], in0=bk[:],
                               scalar=CW[:, gt_, e_:e_ + 1], in1=acc[:, gt_, n2 * 512:(n2 + 1) * 512], op0=ALU.mult, op1=ALU.add)
            S.barrier()
            arena["off"] = mark_m
            FX = TP("m_fx", 3, [128, D], F32)
            FO = TP("m_fo", 3, [128, D], F32)
            sst2 = TP("m_ss", 3, [128, 2], F32)
            junk2 = sb("m_junk", [128, D], BF16)
            outs = []
            for tt in range(NT):
                r0 = tt * 128
                xn, xnk = FX.get()
                DMA(xn[:], XNEW[r0:r0 + 128, :], writes=[xnk])
                ss, ssk = sst2.get()
                OP("act", "activation", [("acc", tt, 0), ("acc", tt, 1)], ["m_junk", ssk], out=junk2[:], in_=acc[:, tt, :], func=AF.Square, accum_out=ss[:, 0:1])
                OP("dve", "tensor_scalar", [ssk], [ssk], out=ss[:, 0:1], in0=ss[:, 0:1], scalar1=1.0 / D, scalar2=EPS, op0=ALU.mult, op1=ALU.add)
                OP("act", "activation", [ssk], [ssk], out=ss[:, 0:1], in_=ss[:, 0:1], func=AF.Sqrt)
                OP("dve", "reciprocal", [ssk], [ssk], out=ss[:, 0:1], in_=ss[:, 0:1])
                fo, fok = FO.get()
                OP("dve", "scalar_tensor_tensor", [("acc", tt, 0), ("acc", tt, 1), ssk], [fok], out=fo[:], in0=acc[:, tt, :], scalar=ss[:, 0:1], in1=G2P[:], op0=ALU.mult, op1=ALU.mult)
                OP("pool", "tensor_tensor", [fok, xnk], [fok], out=fo[:], in0=fo[:], in1=xn[:], op=ALU.add)
                outs.append(S.op("sp", lambda e, fo=fo, r0=r0: e.dma_start(out=out[r0:r0 + 128, :], in_=fo[:]), reads=[fok], dma=True))
            S.barrier()

        S.emit(st)
    return nc


def prepare_core_inputs(inputs, b, half):
    f = np.ascontiguousarray
    flip = (half == 1)
    xs = inputs["x"][b]
    cs = inputs["ctx"][b]
    if flip:
        xs = xs[::-1]
        cs = cs[::-1]
    w_in = inputs["w_in"][0]
    if flip:
        w_in = w_in.copy()
        a0 = 6144
        w_in[:, a0:a0 + 8], w_in[:, a0 + 8:a0 + 16] = inputs["w_in"][0][:, a0 + 8:a0 + 16], inputs["w_in"][0][:, a0:a0 + 8]
        b0 = 6160
        w_in[:, b0:b0 + 8], w_in[:, b0 + 8:b0 + 16] = inputs["w_in"][0][:, b0 + 8:b0 + 16], inputs["w_in"][0][:, b0:b0 + 8]
    cvec = np.stack([inputs["c"][b], inputs["c_ctx"]], axis=-1)
    m = {
        "x": f(xs), "ctx": f(cs),
        "cc": f(cvec.reshape(8, 128, 2).transpose(1, 0, 2)),
        "ada_w": f(inputs["ada_w"][0]),
        "ada_bT": f(inputs["ada_b"][0].reshape(48, 128).T),
        "ada_b": f(inputs["ada_b"][0].reshape(1, -1)),
        "gpreT": f(inputs["mix_pre_g"][0].reshape(8, 128).T),
        "w_in": f(w_in),
    }
    dsel = (lambda a: a[::-1]) if flip else (lambda a: a)
    P = lambda k: dsel(inputs[k][0])
    rgp = np.concatenate([P("rg_conv_w").transpose(0, 2, 1),
                          P("rg_conv_b")[:, :, None], P("rg_ba")[:, :, None], P("rg_bi")[:, :, None], P("rg_lam")[:, :, None]], axis=2)
    m["rgp"] = f(rgp.reshape(2, 8, 128, 8).transpose(2, 1, 0, 3))
    m["rg_wa"] = f(P("rg_wa"))
    m["rg_wi"] = f(P("rg_wi"))
    cw = P("dn_conv_w")
    m["dncw"] = f(cw.reshape(2, 4, 24, 128).transpose(3, 2, 0, 1))
    m["dnc"] = f(np.concatenate([P("dn_a_log").reshape(-1), P("dn_dt_bias").reshape(-1)]).reshape(1, 32))
    m["dn_norm_g"] = f(inputs["dn_norm_g"][0].reshape(128, 1))
    m["gpost"] = f(inputs["mix_post_g"][0].reshape(1, -1))
    m["gfpre"] = f(inputs["ffn_pre_g"][0].reshape(1, -1))
    m["gfpost"] = f(inputs["ffn_post_g"][0].reshape(1, -1))
    for k in ("rg_w_o", "dn_w_o", "w_out", "router_w", "e_w1", "e_w2", "e_b2"):
        m[k] = f(inputs[k][0])
    m["router_b"] = f(inputs["router_b"][0].reshape(1, -1))
    m["e_b1T"] = f(inputs["e_b1"][0].reshape(NE, 16, 128).transpose(2, 0, 1))
    return {k: np.asarray(v, dtype=np.float32) for k, v in m.items()}


_PROG = {}


def kernel(**inputs):
    inputs = {k: np.asarray(v) for k, v in inputs.items()}
    if "nc" not in _PROG:
        _PROG["nc"] = build_program()
    nc = _PROG["nc"]
    in_maps = []
    for core in range(8):
        b, half = core // 2, core % 2
        in_maps.append(prepare_core_inputs(inputs, b, half))
    res = run_bass_kernel_spmd(nc, in_maps, core_ids=list(range(8)))
    B = inputs["x"].shape[0]
    outp = np.zeros((B, S_LAT, D), np.float32)
    for core in range(8):
        b, half = core // 2, core % 2
        o = np.asarray(res.results[core]["out"], dtype=np.float32)
        if half == 0:
            outp[b, 0:OWN] = o
        else:
            outp[b, S_LAT - OWN:] = o[::-1]
    return outp
```

```python
from contextlib import ExitStack
import numpy as np
import concourse.bass as bass
import concourse.mybir as mybir
from concourse.bass_utils import run_bass_kernel_spmd

F32 = mybir.dt.float32
BF16 = mybir.dt.bfloat16
AF = mybir.ActivationFunctionType
ALU = mybir.AluOpType

D = 1024
S_LAT = 4096
S_CTX = 256
S_ALL = S_LAT + S_CTX
OWN = 2048
EPS = 1e-6
NE = 32
N_DMA_SEM = 32
SEM_GEN = 30000


class Sched:
    ENGS = ("pe", "act", "dve", "pool", "sp")

    def __init__(self, nc):
        self.nc = nc
        self.ops = {e: [] for e in self.ENGS}
        self.cnt = {e: 0 for e in self.ENGS}
        self.known = {e: {} for e in self.ENGS}
        self.last_w = {}
        self.readers = {}
        self.dma_n = 0
        self.dma_slot_target = [0] * N_DMA_SEM
        self.semids = set()

    def _add_wait(self, eng, waits, tok, raw):
        semid, val, teng = tok
        if teng == eng:
            if eng == "pe" or not raw:
                return
        if self.known[eng].get(semid, 0) >= val:
            return
        waits[semid] = max(waits.get(semid, 0), val)

    def op(self, eng, fn, reads=(), writes=(), dma=False):
        waits = {}
        for k in reads:
            t = self.last_w.get(k)
            if t is not None:
                self._add_wait(eng, waits, t, True)
            if (isinstance(k, str) and k.startswith("bank")) or (isinstance(k, tuple) and k[0] == "obank"):
                for t in self.readers.get(k, ()):
                    self._add_wait(eng, waits, t, False)
        for k in writes:
            t = self.last_w.get(k)
            if t is not None:
                self._add_wait(eng, waits, t, False)
            for t in self.readers.get(k, ()):
                self._add_wait(eng, waits, t, False)
        if dma:
            slot = self.dma_n % N_DMA_SEM
            self.dma_n += 1
            prev = self.dma_slot_target[slot]
            if prev > 0:
                self._add_wait(eng, waits, ("q%d" % slot, prev, None), True)
            val = prev + 16
            self.dma_slot_target[slot] = val
            tok = ("q%d" % slot, val, None)
        else:
            self.cnt[eng] += 1
            gen, val = divmod(self.cnt[eng] - 1, SEM_GEN)
            tok = ("%s.%d" % (eng, gen), val + 1, eng)
            if gen > 0 and val == 0:
                pass
        self.semids.add(tok[0])
        for semid, val in waits.items():
            self.known[eng][semid] = val
        self.ops[eng].append((waits, fn, tok))
        for k in reads:
            self.readers.setdefault(k, []).append(tok)
        for k in writes:
            self.last_w[k] = tok
            self.readers[k] = []
        return tok

    def wait_tokens(self, eng, toks):
        waits = {}
        for t in toks:
            self._add_wait(eng, waits, t, True)
        for semid, val in waits.items():
            self.known[eng][semid] = val
        self.ops[eng].append((waits, None, None))

    def barrier(self):
        toks = []
        for e in self.ENGS:
            if self.cnt[e] > 0:
                gen, val = divmod(self.cnt[e] - 1, SEM_GEN)
                toks.append(("%s.%d" % (e, gen), val + 1, None))
        for slot in range(N_DMA_SEM):
            if self.dma_slot_target[slot] > 0:
                toks.append(("q%d" % slot, self.dma_slot_target[slot], None))
        for e in self.ENGS:
            self.wait_tokens(e, toks)
        self.last_w = {}
        self.readers = {}

    def emit(self, stack):
        nc = self.nc
        sems = {}
        for sid in sorted(self.semids):
            sems[sid] = stack.enter_context(nc.semaphore("s_" + sid.replace(".", "_")))
        block = stack.enter_context(nc.Block())

        def run(ename):
            def body(eng):
                for waits, fn, tok in self.ops[ename]:
                    for semid, val in waits.items():
                        eng.wait_ge(sems[semid], val)
                    if fn is None:
                        continue
                    inst = fn(eng)
                    semid, val, _ = tok
                    inst.then_inc(sems[semid], 16 if semid.startswith("q") else 1)
            return body

        block.tensor(run("pe"))
        block.scalar(run("act"))
        block.vector(run("dve"))
        block.gpsimd(run("pool"))
        block.sync(run("sp"))


class Rot:
    def __init__(self, items):
        self.items = items
        self.i = 0

    def next(self):
        it = self.items[self.i % len(self.items)]
        self.i += 1
        return it


def build_program(debug=False, phases=("pa", "pb", "rg", "dn", "tail", "moe"), dn_heads=tuple(range(8)), moe_experts=NE, tail_stop=99):
    nc = bass.Bass("TRN2", target_bir_lowering=False)
    dbg_kind = "ExternalOutput" if debug else "Internal"

    def din(name, shape, dt=F32):
        return nc.dram_tensor(name, list(shape), dt, kind="ExternalInput").ap()

    def dscr(name, shape, dt):
        return nc.dram_tensor(name, list(shape), dt, kind=dbg_kind).ap()

    x = din("x", [S_LAT, D])
    ctx = din("ctx", [S_CTX, D])
    cc = din("cc", [128, 8, 2])
    ada_w = din("ada_w", [D, 6 * D])
    ada_bT = din("ada_bT", [128, 48])
    ada_b = din("ada_b", [1, 6 * D])
    gpreT = din("gpreT", [128, 8])
    w_in = din("w_in", [D, 8224])
    rgp = din("rgp", [128, 8, 2, 8])
    rg_wa = din("rg_wa", [2, 16, 64, 64])
    rg_wi = din("rg_wi", [2, 16, 64, 64])
    dncw = din("dncw", [128, 24, 2, 4])
    dnc = din("dnc", [1, 32])
    dn_norm_g = din("dn_norm_g", [128, 1])
    gpost = din("gpost", [1, D])
    gfpre = din("gfpre", [1, D])
    gfpost = din("gfpost", [1, D])
    rg_w_o = din("rg_w_o", [D, D])
    dn_w_o = din("dn_w_o", [D, D])
    w_out = din("w_out", [D, D])
    router_w = din("router_w", [D, NE])
    router_b = din("router_b", [1, NE])
    e_w1 = din("e_w1", [NE, D, 2 * D])
    e_b1T = din("e_b1T", [128, NE, 16])
    e_w2 = din("e_w2", [NE, D, D])
    e_b2 = din("e_b2", [NE, D])
    out = nc.dram_tensor("out", [OWN, D], F32, kind="ExternalOutput").ap()

    PRG = dscr("PRG", [D, S_ALL], BF16)
    PQKV = dscr("PQKV", [3 * D, S_ALL], BF16)
    PAB = dscr("PAB", [S_ALL, 32], F32)
    GATES = dscr("GATES", [4 * D, OWN], BF16)
    XRG = dscr("XRG", [D, OWN], BF16)
    XDN = dscr("XDN", [D, OWN], BF16)
    XNEW = dscr("XNEW", [OWN, D], F32)

    with ExitStack() as st:
        E = st.enter_context
        S = Sched(nc)

        ARENA_WORDS = 53200
        arena_t = E(nc.sbuf_tensor("arena", [128, ARENA_WORDS], F32))
        arena = {"off": 0}

        def sb(name, shape, dt=F32):
            shape = list(shape)
            elems = int(np.prod(shape[1:]))
            words = elems if dt == F32 else (elems + 1) // 2
            words += words % 2
            off = arena["off"]
            assert off + words <= ARENA_WORDS, ("SBUF arena overflow", name, off, words)
            arena["off"] = off + words
            ap = arena_t[0:shape[0], off:off + words]
            if dt != F32:
                ap = ap.bitcast(dt)[:, 0:elems]
            else:
                ap = ap[:, 0:elems]
            if len(shape) == 3:
                ap = ap.rearrange("p (a b) -> p a b", a=shape[1])
            elif len(shape) == 4:
                ap = ap.rearrange("p (a b c) -> p a b c", a=shape[1], b=shape[2])
            return ap

        def ps(name):
            return E(nc.psum_tensor(name, [128, 512], F32))

        banks = [ps("bank%d" % i) for i in range(8)]

        def dbg(name, ap, reads):
            if not debug:
                return
            t = nc.dram_tensor("dbg_" + name, list(ap.shape), ap.dtype, kind="ExternalOutput").ap()
            S.op("sp", lambda e: e.dma_start(out=t, in_=ap), reads=reads, dma=True)

        ident = sb("ident", [128, 128])
        S.op("pool", lambda e: e.memset(ident[:], 0.0), writes=["ident"])
        S.op("pool", lambda e: e.affine_select(out=ident[:], in_=ident[:], pattern=[[-1, 128]],
                                               compare_op=ALU.not_equal, fill=1.0, base=0,
                                               channel_multiplier=1),
             reads=["ident"], writes=["ident"])

        cs = sb("cs", [128, 8, 2])
        modF = sb("modF", [128, 16, 2])
        Gm = sb("Gm", [128, 8, 2])
        Sh = sb("Sh", [128, 8, 2])
        adabT = sb("adabT", [128, 48])
        gpre = sb("gpre", [128, 8])
        S.op("sp", lambda e: e.dma_start(out=cs[:], in_=cc), writes=["cs"], dma=True)
        S.op("sp", lambda e: e.dma_start(out=adabT[:], in_=ada_bT), writes=["adabT"], dma=True)
        S.op("sp", lambda e: e.dma_start(out=gpre[:], in_=gpreT), writes=["gpre"], dma=True)
        S.op("act", lambda e: e.activation(out=cs[:], in_=cs[:], func=AF.Silu), reads=["cs"], writes=["cs"])
        mark0 = arena["off"]
        if True:
            adaw0 = sb("adaw0", [128, 8, 2048])
            for kc in range(8):
                S.op("sp", lambda e, kc=kc: e.dma_start(out=adaw0[:, kc, :], in_=ada_w[kc * 128:(kc + 1) * 128, 0:2048]),
                     writes=[("adaw0", kc)], dma=True)
            pm = banks[0]
            for j in range(16):
                for kc in range(8):
                    S.op("pe", lambda e, j=j, kc=kc: e.matmul(pm[:, 2 * j:2 * j + 2], lhsT=adaw0[:, kc, j * 128:(j + 1) * 128],
                                                              rhs=cs[:, kc, :], start=(kc == 0), stop=(kc == 7)),
                         reads=[("adaw0", kc), "cs"], writes=["bank0"])
            S.op("dve", lambda e: e.tensor_tensor(out=modF[:], in0=pm[:, 0:32].rearrange("p (j t) -> p j t", t=2),
                                                  in1=adabT[:, 0:16].unsqueeze(2).to_broadcast([128, 16, 2]), op=ALU.add),
                 reads=["bank0", "adabT"], writes=["modF"])
            S.op("dve", lambda e: e.tensor_scalar(out=Gm[:], in0=modF[:, 8:16, :], scalar1=1.0, scalar2=None, op0=ALU.add),
                 reads=["modF"], writes=["Gm"])
            S.op("dve", lambda e: e.tensor_tensor(out=Gm[:], in0=Gm[:], in1=gpre[:].unsqueeze(2).to_broadcast([128, 8, 2]), op=ALU.mult),
                 reads=["Gm", "gpre"], writes=["Gm"])
            S.op("dve", lambda e: e.tensor_copy(out=Sh[:], in_=modF[:, 0:8, :]), reads=["modF"], writes=["Sh"])
            S.barrier()
            arena["off"] = mark0

        def proj_pass(pname, Wb, wkey, blocks, chunk_fn, ab_cols=None):
            xts = Rot([(sb("%s_xt%d" % (pname, i), [128, D]), "%s_xt%d" % (pname, i)) for i in range(4)])
            xrs = [[(sb("%s_xr%d_%d" % (pname, b, i), [128, D]), "%s_xr%d_%d" % (pname, b, i)) for i in range(4)] for b in range(2)]
            hTs = [(sb("%s_hT%d" % (pname, i), [128, 8, 512], BF16), "%s_hT%d" % (pname, i)) for i in range(2)]
            junk = sb(pname + "_junk", [128, D], BF16)
            ssq = [(sb("%s_ss%d" % (pname, i), [128, 4]), "%s_ss%d" % (pname, i)) for i in range(2)]
            stg = Rot([(sb("%s_stg%d" % (pname, i), [128, 512], BF16), "%s_stg%d" % (pname, i)) for i in range(6)])
            tmp = Rot([(sb("%s_tmp%d" % (pname, i), [128, 512]), "%s_tmp%d" % (pname, i)) for i in range(4)])
            tb = Rot([(banks[0], "bank0"), (banks[1], "bank1")])
            pb = Rot([(banks[2 + i], "bank%d" % (2 + i)) for i in range(4)])
            abb = Rot([(banks[6], "bank6"), (banks[7], "bank7")])
            abst = Rot([(sb("%s_abst%d" % (pname, i), [128, 4, 32]), "%s_abst%d" % (pname, i)) for i in range(2)])
            evq = Rot(["act", "dve"])

            def stage1(bi):
                blk = blocks[bi]
                nt = blk["ntok"] // 128
                ss, ssk = ssq[bi % 2]
                xtl = []
                for j in range(nt):
                    xt, xk = xts.next()
                    for (ap, p0, npart) in blk["loads"][j]:
                        S.op("sp", lambda e, xt=xt, ap=ap, p0=p0, npart=npart: e.dma_start(out=xt[p0:p0 + npart, :], in_=ap),
                             writes=[xk], dma=True)
                    S.op("act", lambda e, xt=xt, ss=ss, j=j: e.activation(out=junk[:], in_=xt[:], func=AF.Square, accum_out=ss[:, j:j + 1]),
                         reads=[xk], writes=[pname + "_junk", ssk])
                    xtl.append((xt, xk))
                S.op("dve", lambda e, ss=ss, nt=nt: e.tensor_scalar(out=ss[:, 0:nt], in0=ss[:, 0:nt], scalar1=1.0 / D, scalar2=EPS,
                                                                   op0=ALU.mult, op1=ALU.add), reads=[ssk], writes=[ssk])
                S.op("act", lambda e, ss=ss, nt=nt: e.activation(out=ss[:, 0:nt], in_=ss[:, 0:nt], func=AF.Sqrt), reads=[ssk], writes=[ssk])
                S.op("dve", lambda e, ss=ss, nt=nt: e.reciprocal(out=ss[:, 0:nt], in_=ss[:, 0:nt]), reads=[ssk], writes=[ssk])
                xrl = xrs[bi % 2]
                for j in range(nt):
                    xt, xk = xtl[j]
                    xr, xrk = xrl[j]
                    S.op("pool", lambda e, xr=xr, xt=xt, ss=ss, j=j: e.tensor_scalar(out=xr[:], in0=xt[:], scalar1=ss[:, j:j + 1], scalar2=0.0,
                                                                                    op0=ALU.mult, op1=ALU.add), reads=[xk, ssk], writes=[xrk])
                hT, hk = hTs[bi % 2]
                m = blk["mod"]
                for c in range(8):
                    bk, bkk = tb.next()
                    for j in range(nt):
                        xr, xrk = xrl[j]
                        S.op("pe", lambda e, bk=bk, xr=xr, c=c, j=j: e.transpose(out=bk[:, j * 128:(j + 1) * 128], in_=xr[:, c * 128:(c + 1) * 128],
                                                                                 identity=ident[:]), reads=[xrk, "ident"], writes=[bkk])
                    S.op("act", lambda e, bk=bk, hT=hT, c=c, nt=nt, m=m: e.activation(out=hT[:, c, 0:nt * 128], in_=bk[:, 0:nt * 128], func=AF.Identity,
                                                                                   scale=Gm[:, c, m:m + 1], bias=Sh[:, c, m:m + 1]),
                         reads=[bkk, "Gm", "Sh"], writes=[(hk, c)])

            def stage2(bi):
                blk = blocks[bi]
                ntok = blk["ntok"]
                hT, hk = hTs[bi % 2]
                for spec in chunk_fn(blk):
                    col0, kind, dst = spec
                    bk, bkk = pb.next()
                    for kc in range(8):
                        S.op("pe", lambda e, bk=bk, kc=kc, col0=col0: e.matmul(bk[:, 0:ntok], lhsT=Wb[:, kc, col0:col0 + 128], rhs=hT[:, kc, 0:ntok],
                                                                               start=(kc == 0), stop=(kc == 7)),
                             reads=[(wkey, kc), (hk, kc)], writes=[bkk])
                    sg, sgk = stg.next()
                    if kind == "copy":
                        q = evq.next()
                        if q == "act":
                            S.op("act", lambda e, sg=sg, bk=bk: e.activation(out=sg[:, 0:ntok], in_=bk[:, 0:ntok], func=AF.Identity),
                                 reads=[bkk], writes=[sgk])
                        else:
                            S.op("dve", lambda e, sg=sg, bk=bk: e.tensor_copy(out=sg[:, 0:ntok], in_=bk[:, 0:ntok]), reads=[bkk], writes=[sgk])
                    elif kind == "silu":
                        S.op("act", lambda e, sg=sg, bk=bk: e.activation(out=sg[:, 0:ntok], in_=bk[:, 0:ntok], func=AF.Silu), reads=[bkk], writes=[sgk])
                    elif kind == "sigmoid":
                        S.op("act", lambda e, sg=sg, bk=bk: e.activation(out=sg[:, 0:ntok], in_=bk[:, 0:ntok], func=AF.Sigmoid), reads=[bkk], writes=[sgk])
                    elif kind == "gelu":
                        t1, t1k = tmp.next()
                        S.op("act", lambda e, t1=t1, bk=bk: e.activation(out=t1[:, 0:ntok], in_=bk[:, 0:ntok], func=AF.Square), reads=[bkk], writes=[t1k])
                        S.op("dve", lambda e, t1=t1: e.tensor_scalar(out=t1[:, 0:ntok], in0=t1[:, 0:ntok], scalar1=0.044715, scalar2=1.0,
                                                                    op0=ALU.mult, op1=ALU.add), reads=[t1k], writes=[t1k])
                        S.op("dve", lambda e, t1=t1, bk=bk: e.tensor_tensor(out=t1[:, 0:ntok], in0=t1[:, 0:ntok], in1=bk[:, 0:ntok], op=ALU.mult),
                             reads=[t1k, bkk], writes=[t1k])
                        S.op("act", lambda e, t1=t1: e.activation(out=t1[:, 0:ntok], in_=t1[:, 0:ntok], func=AF.Sigmoid, scale=1.5957691216057308),
                             reads=[t1k], writes=[t1k])
                        S.op("dve", lambda e, t1=t1, bk=bk, sg=sg: e.tensor_tensor(out=sg[:, 0:ntok], in0=t1[:, 0:ntok], in1=bk[:, 0:ntok], op=ALU.mult),
                             reads=[t1k, bkk], writes=[sgk])
                    S.op("sp", lambda e, sg=sg, dst=dst: e.dma_start(out=dst, in_=sg[:, 0:ntok]), reads=[sgk], dma=True)
                if ab_cols is not None:
                    nt = ntok // 128
                    bk, bkk = abb.next()
                    for j in range(nt):
                        for kc in range(8):
                            S.op("pe", lambda e, bk=bk, kc=kc, j=j: e.matmul(bk[:, j * 32:(j + 1) * 32], lhsT=hT[:, kc, j * 128:(j + 1) * 128],
                                                                             rhs=Wb[:, kc, ab_cols:ab_cols + 32], start=(kc == 0), stop=(kc == 7)),
                                 reads=[(wkey, kc), (hk, kc)], writes=[bkk])
                    ab, abk = abst.next()
                    S.op("dve", lambda e, ab=ab, bk=bk, nt=nt: e.tensor_copy(out=ab[:, 0:nt, :], in_=bk[:, 0:nt * 32].rearrange("p (j c) -> p j c", c=32)),
                         reads=[bkk], writes=[abk])
                    t0 = blk["tokoff"]
                    S.op("sp", lambda e, ab=ab, nt=nt, t0=t0: e.dma_start(out=PAB[t0:t0 + nt * 128, :].rearrange("(j p) c -> p j c", p=128),
                                                                         in_=ab[:, 0:nt, :]), reads=[abk], dma=True)

            stage1(0)
            for bi in range(len(blocks)):
                if bi + 1 < len(blocks):
                    stage1(bi + 1)
                stage2(bi)

        WA = sb("WA", [128, 8, 5120], BF16)
        srcA = [(0, 0, 1024), (1024, 1024, 1024), (2048, 5120, 1024), (3072, 6176, 1024), (4096, 7200, 1024)]
        for (d0, s0, n) in srcA:
            for kc in range(8):
                S.op("pool", lambda e, kc=kc, d0=d0, s0=s0, n=n: e.dma_start(out=WA[:, kc, d0:d0 + n], in_=w_in[kc * 128:(kc + 1) * 128, s0:s0 + n]),
                     writes=[("WA", kc)], dma=True)
        blocksA = [dict(ntok=256, mod=1, tokoff=0, own=False,
                        loads=[[(ctx[j * 128:(j + 1) * 128, :], 0, 128)] for j in range(2)])]
        for bi in range(8):
            blocksA.append(dict(ntok=512, mod=0, tokoff=S_CTX + bi * 512, own=(bi < 4), lat0=bi * 512,
                                loads=[[(x[bi * 512 + j * 128: bi * 512 + (j + 1) * 128, :], 0, 128)] for j in range(4)]))

        def chunksA(blk):
            ntok, t0 = blk["ntok"], blk["tokoff"]
            specs = [(c * 128, "copy", PRG[c * 128:(c + 1) * 128, t0:t0 + ntok]) for c in range(8)]
            if blk["own"]:
                l0 = blk["lat0"]
                for gi, kind in enumerate(["gelu", "silu", "sigmoid", "sigmoid"]):
                    for c in range(8):
                        specs.append((1024 + gi * 1024 + c * 128, kind, GATES[gi * 1024 + c * 128: gi * 1024 + (c + 1) * 128, l0:l0 + ntok]))
            return specs

        if "pa" in phases:
            proj_pass("pa", WA, "WA", blocksA, chunksA)
        S.barrier()
        arena["off"] = mark0

        WB = sb("WB", [128, 8, 3104], BF16)
        srcB = [(0, 2048, 1024), (1024, 3072, 1024), (2048, 4096, 1024), (3072, 6144, 32)]
        for (d0, s0, n) in srcB:
            for kc in range(8):
                S.op("pool", lambda e, kc=kc, d0=d0, s0=s0, n=n: e.dma_start(out=WB[:, kc, d0:d0 + n], in_=w_in[kc * 128:(kc + 1) * 128, s0:s0 + n]),
                     writes=[("WB", kc)], dma=True)
        xcm = x.rearrange("(r c) d -> c r d", c=64)
        blocksB = [dict(ntok=256, mod=1, tokoff=0,
                        loads=[[(ctx[j * 128:(j + 1) * 128, :], 0, 128)] for j in range(2)])]
        for bi in range(8):
            blocksB.append(dict(ntok=512, mod=0, tokoff=S_CTX + bi * 512,
                                loads=[[(xcm[bi * 8 + j * 2 + h], h * 64, 64) for h in range(2)] for j in range(4)]))

        def chunksB(blk):
            ntok, t0 = blk["ntok"], blk["tokoff"]
            return [(c * 128, "copy", PQKV[c * 128:(c + 1) * 128, t0:t0 + ntok]) for c in range(24)]

        if "pb" in phases:
            proj_pass("pb", WB, "WB", blocksB, chunksB, ab_cols=3072)

        S.barrier()
        arena["off"] = mark0

        if "rg" in phases:
            XW = 4 + S_CTX + 4 + S_LAT + 4
            rgp_t = sb("rgp_t", [128, 8, 2, 8])
            S.op("sp", lambda e: e.dma_start(out=rgp_t[:], in_=rgp), writes=["rgp"], dma=True)
            c8 = sb("c8", [128, 8, 2])
            S.op("act", lambda e: e.activation(out=c8[:], in_=rgp_t[:, :, :, 7], func=AF.Exp, scale=-1.0), reads=["rgp"], writes=["c8"])
            S.op("act", lambda e: e.activation(out=c8[:], in_=c8[:], func=AF.Ln, bias=1.0), reads=["c8"], writes=["c8"])
            S.op("dve", lambda e: e.tensor_scalar(out=c8[:], in0=c8[:], scalar1=-8.0, scalar2=None, op0=ALU.mult), reads=["c8"], writes=["c8"])
            NSET = 3
            sets = []
            for si in range(NSET):
                sets.append(dict(
                    xc=sb("rg_xc%d" % si, [128, S_ALL]), ra=sb("rg_ra%d" % si, [128, S_ALL]),
                    ib=sb("rg_ib%d" % si, [128, S_ALL]),
                    wa=sb("rg_wa%d" % si, [128, 128]), wi=sb("rg_wi%d" % si, [128, 128]), k="rgs%d" % si))
            xps = [(sb("rg_xp%d" % i, [128, XW], BF16), "rg_xp%d" % i) for i in range(2)]
            gts = [(sb("rg_gt%d" % i, [128, OWN], BF16), "rg_gt%d" % i) for i in range(2)]
            accs = [(sb("rg_acc%d" % i, [128, OWN]), "rg_acc%d" % i) for i in range(2)]
            identb_r = sb("identb_r", [128, 128], BF16)
            S.op("pool", lambda e: e.tensor_copy(out=identb_r[:], in_=ident[:]), reads=["ident"], writes=["identb_r"])
            dgr = [sb("rg_dg%d" % i, [128, 4, 128], BF16) for i in range(NSET)]
            print("RG arena used", arena["off"])
            pbk = Rot([(banks[i], "bank%d" % i) for i in range(8)])

            def rg_load(g):
                xp, xpk = xps[g % 2]
                gt, gtk = gts[g % 2]
                S.op("pool", lambda e, xp=xp: e.memset(xp[:, 0:4], 0.0), writes=[xpk])
                S.op("pool", lambda e, xp=xp: e.memset(xp[:, 260:264], 0.0), writes=[xpk])
                S.op("pool", lambda e, xp=xp: e.memset(xp[:, XW - 4:XW], 0.0), writes=[xpk])
                S.op("sp", lambda e, xp=xp, g=g: e.dma_start(out=xp[:, 4:260], in_=PRG[g * 128:(g + 1) * 128, 0:S_CTX]), writes=[xpk], dma=True)
                S.op("sp", lambda e, xp=xp, g=g: e.dma_start(out=xp[:, 264:264 + S_LAT], in_=PRG[g * 128:(g + 1) * 128, S_CTX:S_ALL]), writes=[xpk], dma=True)
                S.op("sp", lambda e, gt=gt, g=g: e.dma_start(out=gt[:], in_=GATES[g * 128:(g + 1) * 128, :]), writes=[gtk], dma=True)

            def rg_geom(d):
                ctx_off, lat_off = (4, 264) if d == 0 else (4104, 4)
                n_lat = OWN if d == 0 else S_LAT
                segs = [(ctx_off, 0, S_CTX)] + [(lat_off + i * 512, S_CTX + i * 512, 512) for i in range(n_lat // 512)]
                return segs, S_CTX + n_lat

            def rg_A(g, d, st_):
                xp, xpk = xps[g % 2]
                sk = st_["k"]
                xc, ra, ib, wa_t, wi_t = st_["xc"], st_["ra"], st_["ib"], st_["wa"], st_["wi"]
                prm = rgp_t[:, g, d, :]

                for wt, src, wk in ((wa_t, rg_wa, sk + "wa"), (wi_t, rg_wi, sk + "wi")):
                    S.op("pool", lambda e, wt=wt: e.memset(wt[:], 0.0), writes=[wk])
                    for h in range(2):
                        S.op("sp", lambda e, wt=wt, src=src, h=h, g=g, d=d: e.dma_start(out=wt[h * 64:(h + 1) * 64, h * 64:(h + 1) * 64],
                                                                                   in_=src[d, 2 * g + h]), writes=[wk], dma=True)
                V = xp if d == 0 else xp[:, ::-1]
                segs, ntot = rg_geom(d)
                def conv_seg(vo, o, n):
                    kx = (sk + "xc", o)
                    S.op("dve", lambda e, vo=vo, o=o, n=n: e.tensor_scalar(out=xc[:, o:o + n], in0=V[:, vo:vo + n], scalar1=prm[:, 3:4], scalar2=prm[:, 4:5],
                                                                          op0=ALU.mult, op1=ALU.add), reads=[xpk, "rgp"], writes=[kx])
                    for j in (2, 1, 0):
                        sh = 3 - j
                        S.op("dve", lambda e, vo=vo, o=o, n=n, j=j, sh=sh: e.scalar_tensor_tensor(out=xc[:, o:o + n], in0=V[:, vo - sh:vo - sh + n], scalar=prm[:, j:j + 1],
                                                                                                  in1=xc[:, o:o + n], op0=ALU.mult, op1=ALU.add),
                             reads=[xpk, "rgp", kx], writes=[kx])

                def gate_seg(vo, o, n):
                    kx = (sk + "xc", o)
                    b1, b1k = pbk.next()
                    b2, b2k = pbk.next()
                    S.op("pe", lambda e, b1=b1, o=o, n=n: e.matmul(b1[:, 0:n], lhsT=wa_t[:], rhs=xc[:, o:o + n], start=True, stop=True),
                         reads=[sk + "wa", kx], writes=[b1k])
                    S.op("pe", lambda e, b2=b2, o=o, n=n: e.matmul(b2[:, 0:n], lhsT=wi_t[:], rhs=xc[:, o:o + n], start=True, stop=True),
                         reads=[sk + "wi", kx], writes=[b2k])
                    S.op("act", lambda e, b1=b1, o=o, n=n: e.activation(out=ra[:, o:o + n], in_=b1[:, 0:n], func=AF.Sigmoid, bias=prm[:, 5:6]),
                         reads=[b1k, "rgp"], writes=[(sk + "ra", o)])
                    S.op("act", lambda e, b2=b2, o=o, n=n: e.activation(out=ib[:, o:o + n], in_=b2[:, 0:n], func=AF.Sigmoid, bias=prm[:, 6:7]),
                         reads=[b2k, "rgp"], writes=[(sk + "ib", o)])

                LEAD = 1
                for si_ in range(len(segs) + LEAD):
                    if si_ < len(segs):
                        conv_seg(*segs[si_])
                    if si_ >= LEAD:
                        gate_seg(*segs[si_ - LEAD])

            def rg_B(g, d, st_, part):
                gt, gtk = gts[g % 2]
                acc, acck = accs[g % 2]
                xo, xok = gt, gtk
                sk = st_["k"]
                xc, ra, ib = st_["xc"], st_["ra"], st_["ib"]
                segs, ntot = rg_geom(d)
                allr = [(sk + "ra", o) for (_, o, _) in segs]
                alli = [(sk + "ib", o) for (_, o, _) in segs]
                allx = [(sk + "xc", o) for (_, o, _) in segs]
                if part == 0:
                    S.op("act", lambda e, g=g, d=d: e.activation(out=ra[:, 0:ntot], in_=ra[:, 0:ntot], func=AF.Exp, scale=c8[:, g, d:d + 1]),
                         reads=allr + ["c8"], writes=allr)
                    S.op("pool", lambda e: e.tensor_tensor(out=ib[:, 0:ntot], in0=ib[:, 0:ntot], in1=xc[:, 0:ntot], op=ALU.mult),
                         reads=alli + allx, writes=alli)
                    S.op("act", lambda e: e.activation(out=xc[:, 0:ntot], in_=ra[:, 0:ntot], func=AF.Square),
                         reads=allr + alli, writes=allx)
                    S.op("act", lambda e: e.activation(out=xc[:, 0:ntot], in_=xc[:, 0:ntot], func=AF.Sqrt, scale=-1.0, bias=1.0),
                         reads=allx, writes=allx)
                    S.op("pool", lambda e: e.memset(xc[:, 0:1], 1.0), reads=allx, writes=allx)
                    S.op("pool", lambda e: e.tensor_tensor(out=ib[:, 0:ntot], in0=ib[:, 0:ntot], in1=xc[:, 0:ntot], op=ALU.mult),
                         reads=alli + allx, writes=alli)
                    return
                S.op("dve", lambda e: e.tensor_tensor_scan(out=xc[:, 0:S_CTX], data0=ra[:, 0:S_CTX], data1=ib[:, 0:S_CTX], initial=0.0,
                                                           op0=ALU.mult, op1=ALU.add), reads=allr + alli + allx, writes=allx)
                for o in range(S_CTX, ntot, 1024):
                    S.op("dve", lambda e, o=o: e.tensor_tensor_scan(out=xc[:, o:o + 1024], data0=ra[:, o:o + 1024], data1=ib[:, o:o + 1024],
                                                                    initial=xc[:, o - 1:o], op0=ALU.mult, op1=ALU.add),
                         reads=allr + alli + allx, writes=allx)
                if g == 0:
                    dbg("a%d" % d, ra[:, 0:ntot], allr)
                    dbg("b%d" % d, ib[:, 0:ntot], alli)
                    dbg("h%d" % d, xc[:, 0:ntot], allx)
                if d == 0:
                    S.op("pool", lambda e, acc=acc: e.tensor_copy(out=acc[:], in_=xc[:, S_CTX:S_CTX + OWN]), reads=allx, writes=[acck])
                else:
                    S.op("pool", lambda e, acc=acc: e.tensor_tensor(out=acc[:], in0=acc[:], in1=xc[:, S_CTX + OWN:S_ALL][:, ::-1], op=ALU.add),
                         reads=allx + [acck], writes=[acck])
                    S.op("pool", lambda e, acc=acc, gt=gt, xo=xo: e.tensor_tensor(out=xo[:], in0=acc[:], in1=gt[:], op=ALU.mult),
                         reads=[acck, gtk], writes=[xok])
                    S.op("sp", lambda e, xo=xo, g=g: e.dma_start(out=XRG[g * 128:(g + 1) * 128, :], in_=xo[:]), reads=[xok], dma=True)

            units = [(g, d) for g in range(8) for d in range(2)]
            rg_load(0)
            prev_u = None
            for ui, (g, d) in enumerate(units):
                st_ = sets[ui % NSET]
                if prev_u is not None:
                    rg_B(*prev_u, 0)
                rg_A(g, d, st_)
                if prev_u is not None:
                    rg_B(*prev_u, 1)
                prev_u = (g, d, st_)
                if d == 0 and g + 1 < 8:
                    rg_load(g + 1)
            rg_B(*prev_u, 0)
            rg_B(*prev_u, 1)
            S.barrier()
            arena["off"] = mark0

        def OP(eng, method, reads, writes, **kw):
            S.op(eng, lambda e: getattr(e, method)(**kw), reads=reads, writes=writes)

        def DMA(outap, inap, reads=(), writes=()):
            S.op("sp", lambda e: e.dma_start(out=outap, in_=inap), reads=reads, writes=writes, dma=True)

        class TP:
            def __init__(self, name, n, shape, dt):
                self.t = [(sb("%s%d" % (name, i), shape, dt), "%s%d" % (name, i)) for i in range(n)]
                self.i = 0

            def get(self):
                r = self.t[self.i % len(self.t)]
                self.i += 1
                return r

        if "dn" in phases:
            NEG = -30000.0
            XW = 4 + S_CTX + 4 + S_LAT + 4
            onesb = sb("onesb", [128, 128], BF16)
            onesf = sb("onesf", [128, 128])
            identb = sb("identb", [128, 128], BF16)
            LOW = sb("LOW", [128, 128])
            UPI = sb("UPI", [128, 128])
            NEGL = sb("NEGL", [128, 128])
            NEGU = sb("NEGU", [128, 128])
            H0 = sb("H0", [128, 128])
            H1 = sb("H1", [128, 128])
            OP("pool", "memset", [], ["onesb"], ap=onesb[:], constant=1.0)
            OP("pool", "memset", [], ["onesf"], ap=onesf[:], constant=1.0)
            OP("pool", "tensor_copy", ["ident"], ["identb"], out=identb[:], in_=ident[:])
            OP("pool", "affine_select", ["onesf"], ["LOW"], out=LOW[:], in_=onesf[:], pattern=[[-1, 128]], compare_op=ALU.is_gt, fill=0.0,
               base=0, channel_multiplier=1)
            OP("pool", "memset", ["LOW"], ["LOW"], ap=LOW[64:128, 0:64], constant=0.0)
            OP("pool", "affine_select", ["onesf"], ["UPI"], out=UPI[:], in_=onesf[:], pattern=[[1, 128]], compare_op=ALU.is_ge, fill=0.0,
               base=0, channel_multiplier=-1)
            OP("pool", "memset", ["UPI"], ["UPI"], ap=UPI[0:64, 64:128], constant=0.0)
            OP("dve", "tensor_scalar", ["LOW"], ["NEGL"], out=NEGL[:], in0=LOW[:], scalar1=-1.0, scalar2=-NEG, op0=ALU.add, op1=ALU.mult)
            OP("dve", "tensor_scalar", ["UPI"], ["NEGU"], out=NEGU[:], in0=UPI[:], scalar1=-1.0, scalar2=-NEG, op0=ALU.add, op1=ALU.mult)
            OP("pool", "memset", [], ["H0"], ap=H0[:], constant=0.0)
            OP("pool", "memset", ["H0"], ["H0"], ap=H0[0:64, :], constant=1.0)
            OP("pool", "memset", [], ["H1"], ap=H1[:], constant=0.0)
            OP("pool", "memset", ["H1"], ["H1"], ap=H1[64:128, :], constant=1.0)
            dncw_t = sb("dncw_t", [128, 24, 2, 4])
            dnc_t = sb("dnc_t", [128, 32])
            gnorm = sb("gnorm", [128, 1])
            negA = sb("negA", [128, 16])
            DMA(dncw_t[:], dncw, writes=["dncw"])
            DMA(dnc_t[:], dnc.partition_broadcast(128), writes=["dnc"])
            DMA(gnorm[:], dn_norm_g, writes=["gnorm"])
            OP("act", "activation", ["dnc"], ["negA"], out=negA[:], in_=dnc_t[:, 0:16], func=AF.Exp)
            OP("dve", "tensor_scalar", ["negA"], ["negA"], out=negA[:], in0=negA[:], scalar1=-1.0, scalar2=None, op0=ALU.mult)

            NP = S_ALL // 128
            GCt, Btt, EGLt, EGRt, BEGt = [], [], [], [], []
            with_tmp = arena["off"]
            ABt = sb("ABt", [128, NP, 32])
            ABr = sb("ABr", [128, NP, 32])
            Jm = sb("Jm", [128, 128])
            OP("pool", "memset", [], ["Jm"], ap=Jm[:], constant=0.0)
            OP("pool", "affine_select", ["Jm"], ["Jm"], out=Jm[:], in_=Jm[:], pattern=[[1, 128]], compare_op=ALU.not_equal, fill=1.0,
               base=-127, channel_multiplier=1)
            Zt = sb("Zt", [128, NP, 8])
            Lt = sb("Lt", [128, NP, 8])
            Gt = sb("Gt", [128, NP, 8])
            GLtok = sb("GLtok", [128, NP, 8])
            for d in range(2):
                GC = sb("GC%d" % d, [128, NP, 8]); Bt = sb("Bt%d" % d, [128, NP, 8]); EGL = sb("EGL%d" % d, [128, NP, 2, 8])
                EGR = sb("EGR%d" % d, [128, NP, 8]); BEG = sb("BEG%d" % d, [128, NP, 8])
                GCt.append(GC); Btt.append(Bt); EGLt.append(EGL); EGRt.append(EGR); BEGt.append(BEG)
            mark_dn = arena["off"]
            for d in range(2):
                GC, Bt, EGL, EGR, BEG = GCt[d], Btt[d], EGLt[d], EGRt[d], BEGt[d]
                k = "sc%d" % d
                if d == 0:
                    DMA(ABt[:, 0:2, :], PAB[0:S_CTX, :].rearrange("(n p) c -> p n c", p=128), writes=["ABt"])
                    DMA(ABt[:, 2:NP, :], PAB[S_CTX:S_ALL, :].rearrange("(n p) c -> p n c", p=128), writes=["ABt"])
                    ABs, ABk = ABt, "ABt"
                else:
                    ABf = ABt[:].rearrange("p n c -> p (n c)")
                    for bi_, (s0, s1) in enumerate(((0, 16), (16, 32), (32, 34))):
                        OP("pe", "matmul", ["Jm", "ABt"], ["bank%d" % (3 + bi_)], out=banks[3 + bi_][:, 0:(s1 - s0) * 32], lhsT=Jm[:], rhs=ABf[:, s0 * 32:s1 * 32],
                           start=True, stop=True)
                    b3v = banks[3][:, 0:512].rearrange("p (n c) -> p n c", c=32)
                    b4v = banks[4][:, 0:512].rearrange("p (n c) -> p n c", c=32)
                    b5v = banks[5][:, 0:64].rearrange("p (n c) -> p n c", c=32)
                    OP("dve", "tensor_copy", ["bank3"], ["ABr"], out=ABr[:, 0:2, :], in_=b3v[:, 0:2, :][:, ::-1, :])
                    OP("dve", "tensor_copy", ["bank3"], ["ABr"], out=ABr[:, 20:34, :], in_=b3v[:, 2:16, :][:, ::-1, :])
                    OP("dve", "tensor_copy", ["bank4"], ["ABr"], out=ABr[:, 4:20, :], in_=b4v[:, 0:16, :][:, ::-1, :])
                    OP("dve", "tensor_copy", ["bank5"], ["ABr"], out=ABr[:, 2:4, :], in_=b5v[:, 0:2, :][:, ::-1, :])
                    ABs, ABk = ABr, "ABr"
                dtb = dnc_t[:, 16 + d * 8:24 + d * 8].unsqueeze(1).to_broadcast([128, NP, 8])
                nab = negA[:, d * 8:(d + 1) * 8].unsqueeze(1).to_broadcast([128, NP, 8])
                OP("dve", "tensor_tensor", [ABk, "dnc"], ["Zt"], out=Zt[:], in0=ABs[:, :, d * 8:(d + 1) * 8], in1=dtb, op=ALU.add)
                OP("dve", "scalar_tensor_tensor", ["Zt"], ["Lt"], out=Lt[:].rearrange("p n c -> p (n c)"), in0=Zt[:].rearrange("p n c -> p (n c)"), scalar=-1.0,
                   in1=Zt[:].rearrange("p n c -> p (n c)"), op0=ALU.mult, op1=ALU.max)
                OP("act", "activation", ["Lt"], ["Lt"], out=Lt[:], in_=Lt[:], func=AF.Exp, scale=-1.0)
                OP("act", "activation", ["Lt"], ["Lt"], out=Lt[:], in_=Lt[:], func=AF.Ln, bias=1.0)
                OP("dve", "scalar_tensor_tensor", ["Zt", "Lt"], ["Gt"], out=Gt[:].rearrange("p n c -> p (n c)"), in0=Zt[:].rearrange("p n c -> p (n c)"),
                   scalar=0.0, in1=Lt[:].rearrange("p n c -> p (n c)"), op0=ALU.max, op1=ALU.add)
                OP("dve", "tensor_tensor", ["Gt", "negA"], ["Gt"], out=Gt[:], in0=Gt[:], in1=nab, op=ALU.mult)
                OP("act", "activation", [ABk], [k + "B"], out=Bt[:], in_=ABs[:, :, 16 + d * 8:24 + d * 8], func=AF.Sigmoid)
                Gf = Gt[:].rearrange("p n c -> p (n c)")
                W = NP * 8
                OP("pe", "matmul", ["UPI", "Gt"], ["bank0"], out=banks[0][:, 0:W], lhsT=UPI[:], rhs=Gf, start=True, stop=True)
                OP("pe", "matmul", ["H0", "Gt"], ["bank1"], out=banks[1][:, 0:W], lhsT=H0[:], rhs=Gf, start=True, stop=True)
                OP("pe", "matmul", ["H1", "Gt"], ["bank2"], out=banks[2][:, 0:W], lhsT=H1[:], rhs=Gf, start=True, stop=True)
                OP("dve", "tensor_copy", ["bank0"], [k + "GC"], out=GC[:].rearrange("p n c -> p (n c)"), in_=banks[0][:, 0:W])
                OP("act", "activation", ["bank1"], [k + "EGL"], out=EGL[:, :, 0, :], in_=banks[1][:, 0:W].rearrange("p (n c) -> p n c", c=8), func=AF.Exp)
                OP("act", "activation", ["bank2"], [k + "EGL"], out=EGL[:, :, 1, :], in_=banks[2][:, 0:W].rearrange("p (n c) -> p n c", c=8), func=AF.Exp)
                OP("dve", "tensor_copy", ["bank1"], ["GLtok"], out=GLtok[0:64].rearrange("p n c -> p (n c)"), in_=banks[1][0:64, 0:W])
                OP("dve", "tensor_copy", ["bank2", "GLtok"], ["GLtok"], out=GLtok[64:128].rearrange("p n c -> p (n c)"), in_=banks[2][64:128, 0:W])
                OP("dve", "tensor_tensor", ["GLtok", k + "GC"], ["GLtok"], out=GLtok[:], in0=GLtok[:], in1=GC[:], op=ALU.subtract)
                OP("act", "activation", ["GLtok"], [k + "EGR"], out=EGR[:], in_=GLtok[:], func=AF.Exp)
                OP("act", "activation", [k + "GC"], [k + "BEG"], out=BEG[:], in_=GC[:], func=AF.Exp)
                OP("dve", "tensor_tensor", [k + "BEG", k + "B"], [k + "BEG"], out=BEG[:], in0=BEG[:], in1=Bt[:], op=ALU.mult)
            S.barrier()
            arena["off"] = mark_dn

            xin = [[(sb("dn_x%d_%d" % (par, part), [128, XW], BF16), "dn_x%d_%d" % (par, part)) for part in range(3)] for par in range(1)]
            szs = [(sb("dn_sz%d" % i, [128, OWN], BF16), "dn_sz%d" % i) for i in range(2)]
            Obuf = [[(sb("dn_O%d_%d" % (par, d), [128, 64, 32]), "dn_O%d_%d" % (par, d)) for d in range(2)] for par in range(1)]
            xos = [(sb("dn_xo%d" % i, [128, OWN], BF16), "dn_xo%d" % i) for i in range(2)]
            F32Pd = [TP("dn_f%d" % d, 7, [128, 512], F32) for d in range(2)]
            BFPd = [TP("dn_b%d" % d, 16, [128, 512], BF16) for d in range(2)]
            VNP = [TP("dn_vn%d" % d, 4, [128, 128], BF16) for d in range(2)]
            h32s = [[sb("dn_h32_%d%d" % (hp, d), [128, 128]) for d in range(2)] for hp in range(2)]
            hbfs = [[[sb("dn_hbf%d%d_%d" % (hp, d, i), [128, 128], BF16) for i in range(2)] for d in range(2)] for hp in range(2)]
            pers = {}
            for d in range(2):
                for par in range(2):
                    pers[(d, par)] = dict(
                        qe=sb("dn_qe%d%d" % (d, par), [128, 512], BF16), qkt=sb("dn_qkt%d%d" % (d, par), [128, 4, 128], BF16),
                        u=sb("dn_u%d%d" % (d, par), [128, 4, 128]), wT=sb("dn_wT%d%d" % (d, par), [128, 512], BF16),
                        kd=sb("dn_kd%d%d" % (d, par), [128, 4, 128], BF16), k="dnp%d%d" % (d, par),
                        qn=sb("dn_qn%d%d" % (d, par), [128, 512], BF16), kn=sb("dn_kn%d%d" % (d, par), [128, 512], BF16),
                        vT=sb("dn_vT%d%d" % (d, par), [128, 512], BF16))
            dgs = [sb("dn_dg%d" % hp, [128, 24, 128], BF16) for hp in range(2)]
            print("DN arena used", arena["off"])
            prot = TP.__new__(TP)
            prot.t = [(banks[i], "bank%d" % i) for i in range(6)]
            prot.i = 0
            banks_bf = [bk.bitcast(BF16) for bk in banks]
            LNQ = -0.5 * float(np.log(128.0))

            tcount = {"t": 0}

            def run_rr(gens):
                gens = list(gens)
                while gens:
                    for g_ in list(gens):
                        try:
                            next(g_)
                        except StopIteration:
                            gens.remove(g_)

            def dn_head(hd, prev):
                par = hd % 2
                h32 = h32s[par]
                hbf = hbfs[par]
                xb = xin[0]
                sz, szk = szs[par]
                xo, xok = xos[par]
                for part in range(3):
                    xp, xpk = xb[part]
                    r0 = part * 1024 + hd * 128
                    OP("pool", "memset", [], [xpk], ap=xp[:, 0:4], constant=0.0)
                    OP("pool", "memset", [], [xpk], ap=xp[:, 260:264], constant=0.0)
                    OP("pool", "memset", [], [xpk], ap=xp[:, XW - 4:XW], constant=0.0)
                    DMA(xp[:, 4:260], PQKV[r0:r0 + 128, 0:S_CTX], writes=[xpk])
                    DMA(xp[:, 264:264 + S_LAT], PQKV[r0:r0 + 128, S_CTX:S_ALL], writes=[xpk])
                DMA(sz[:], GATES[1024 + hd * 128:1024 + (hd + 1) * 128, :], writes=[szk])
                dg = dgs[par]
                for part in range(3):
                    for d in range(2):
                        for j in range(4):
                            OP("pool", "tensor_scalar", ["identb", "dncw"], [("dg", par, part, d)], out=dg[:, (part * 2 + d) * 4 + j, :], in0=identb[:],
                               scalar1=dncw_t[:, part * 8 + hd, d, j:j + 1], scalar2=None, op0=ALU.mult)
                hstate = {}
                for d in range(2):
                    OP("pool", "memset", [], [("h32", par, d)], ap=h32[d][:], constant=0.0)
                    OP("pool", "memset", [], [("hbf", par, d, 0)], ap=hbf[d][0][:], constant=0.0)
                    hstate[d] = 0

                groups = [(0, 256)] + [(S_CTX + i * 512, 512) for i in range(8)]

                def prep(d, gi, tpar):
                    F32P, BFP = F32Pd[d], BFPd[d]
                    o, n = groups[gi]
                    npair = n // 128
                    p0 = o // 128
                    P = pers[(d, tpar)]
                    pk = P["k"]
                    GC, Bt, EGR, BEG = GCt[d], Btt[d], EGRt[d], BEGt[d]
                    ctx_off, lat_off = (4, 264) if d == 0 else (4104, 4)
                    base = (ctx_off + o) if o < S_CTX else (lat_off + o - S_CTX)
                    res = {}
                    for part in range(3):
                        xp, xpk = xb[part]
                        V = xp if d == 0 else xp[:, ::-1]
                        T1, T1k = prot.get()
                        for j in range(4):
                            sh = 3 - j
                            OP("pe", "matmul", [xpk, ("dg", par, part, d)], [T1k], out=T1[:, 0:n], lhsT=dg[:, (part * 2 + d) * 4 + j, :],
                               rhs=V[:, base - sh:base - sh + n], start=(j == 0), stop=(j == 3))
                        yield
                        if part == 2:
                            vT, vTk = P["vT"], (pk, "vT")
                            OP("act", "activation", [T1k], [vTk], out=vT[:, 0:n], in_=T1[:, 0:n], func=AF.Silu)
                            res["v"] = (vT, vTk)
                            continue
                        T2, T2k = F32P.get()
                        OP("act", "activation", [T1k], [T2k], out=T2[:, 0:n], in_=T1[:, 0:n], func=AF.Silu)
                        SQ, SQk = BFP.get()
                        OP("pool", "tensor_tensor", [T2k], [SQk], out=SQ[:, 0:n], in0=T2[:, 0:n], in1=T2[:, 0:n], op=ALU.mult)
                        bk, bkk = prot.get()
                        OP("pe", "matmul", ["onesb", SQk], [bkk], out=bk[:, 0:n], lhsT=onesb[:], rhs=SQ[:, 0:n], start=True, stop=True)
                        yield
                        RS, RSk = F32P.get()
                        OP("act", "activation", [bkk], [RSk], out=RS[:, 0:n], in_=bk[:, 0:n], func=AF.Ln, bias=EPS)
                        OP("act", "activation", [RSk], [RSk], out=RS[:, 0:n], in_=RS[:, 0:n], func=AF.Exp, scale=-0.5, bias=(LNQ if part == 0 else 0.0))
                        NN, NNk = (P["qn"], (pk, "qn")) if part == 0 else (P["kn"], (pk, "kn"))
                        OP("pool", "tensor_tensor", [T2k, RSk], [NNk], out=NN[:, 0:n], in0=T2[:, 0:n], in1=RS[:, 0:n], op=ALU.mult)
                        res["qk"[part]] = (NN, NNk)
                        yield
                    qn, qnk = res["q"]
                    kn, knk = res["k"]
                    vT, vTk = res["v"]
                    gcb = GC[:, p0:p0 + npair, hd].unsqueeze(2).to_broadcast([128, npair, 128])
                    v3 = lambda t: t[:, 0:n].rearrange("p (a b) -> p a b", b=128)
                    Rt, Rtk = F32P.get()
                    OP("pool", "tensor_tensor", ["ident", "sc%dGC" % d], [Rtk], out=v3(Rt), in0=ident[:].unsqueeze(1).to_broadcast([128, npair, 128]), in1=gcb, op=ALU.mult)
                    bg, bgk = prot.get()
                    OP("pe", "matmul", ["onesf", Rtk], [bgk], out=bg[:, 0:n], lhsT=onesf[:], rhs=Rt[:, 0:n], start=True, stop=True)
                    yield
                    EB, EBk = F32P.get()
                    OP("act", "activation", [bgk], [EBk], out=EB[:, 0:n], in_=bg[:, 0:n], func=AF.Exp)
                    OP("pool", "tensor_tensor", [qnk, EBk], [(pk, "qe")], out=P["qe"][:, 0:n], in0=qn[:, 0:n], in1=EB[:, 0:n], op=ALU.mult)
                    Dl, Dlk = F32P.get()
                    OP("dve", "tensor_tensor", ["sc%dGC" % d, bgk], [Dlk], out=v3(Dl), in0=gcb, in1=v3(bg), op=ALU.subtract)
                    OP("pool", "tensor_tensor", [Dlk, "NEGL"], [Dlk], out=v3(Dl), in0=v3(Dl), in1=NEGL[:].unsqueeze(1).to_broadcast([128, npair, 128]), op=ALU.add)
                    OP("act", "activation", [Dlk], [Dlk], out=Dl[:, 0:n], in_=Dl[:, 0:n], func=AF.Exp)
                    W1, W1k = BFP.get()
                    OP("pool", "tensor_tensor", [Dlk, "sc%dB" % d], [W1k], out=v3(W1), in0=v3(Dl),
                       in1=Bt[:, p0:p0 + npair, hd].unsqueeze(2).to_broadcast([128, npair, 128]), op=ALU.mult)
                    yield
                    Du, Duk = F32P.get()
                    OP("dve", "tensor_tensor", ["sc%dGC" % d, bgk], [Duk], out=v3(Du), in0=v3(bg), in1=gcb, op=ALU.subtract)
                    OP("pool", "tensor_tensor", [Duk, "NEGU"], [Duk], out=v3(Du), in0=v3(Du), in1=NEGU[:].unsqueeze(1).to_broadcast([128, npair, 128]), op=ALU.add)
                    EDT, EDTk = BFP.get()
                    OP("act", "activation", [Duk], [EDTk], out=EDT[:, 0:n], in_=Du[:, 0:n], func=AF.Exp)
                    yield
                    bkk_, bkkk = prot.get()
                    bqk, bqkk = prot.get()
                    for pi in range(npair):
                        cs_ = slice(pi * 128, (pi + 1) * 128)
                        OP("pe", "matmul", [knk], [bkkk], out=bkk_[:, cs_], lhsT=kn[:, cs_], rhs=kn[:, cs_], start=True, stop=True)
                        OP("pe", "matmul", [knk, qnk], [bqkk], out=bqk[:, cs_], lhsT=kn[:, cs_], rhs=qn[:, cs_], start=True, stop=True)
                    yield
                    Lm, Lmk = BFP.get()
                    OP("dve", "tensor_tensor", [bkkk, W1k], [Lmk], out=Lm[:, 0:n], in0=bkk_[:, 0:n], in1=W1[:, 0:n], op=ALU.mult)
                    OP("dve", "tensor_tensor", [bqkk, EDTk], [(pk, "qkt")], out=P["qkt"][:, 0:npair, :], in0=v3(bqk), in1=v3(EDT), op=ALU.mult)

                    def mm_pairs(lhs, lhsk, rhs, rhsk, trans=False):
                        bi_ = prot.i % 6
                        bk2, bk2k = prot.get()
                        for pi in range(npair):
                            cs_ = slice(pi * 128, (pi + 1) * 128)
                            if trans:
                                OP("pe", "transpose", [lhsk, "identb"], [bk2k], out=banks_bf[bi_][:, cs_], in_=lhs[:, cs_], identity=identb[:])
                            else:
                                OP("pe", "matmul", [lhsk, rhsk], [bk2k], out=bk2[:, cs_], lhsT=lhs[:, cs_], rhs=rhs[:, cs_], start=True, stop=True)
                        return (banks_bf[bi_] if trans else bk2), bk2k

                    def evac_copy(src, srck, eng="act"):
                        t, tk = BFP.get()
                        if eng == "act":
                            OP("act", "activation", [srck], [tk], out=t[:, 0:n], in_=src[:, 0:n], func=AF.Identity)
                        else:
                            OP("dve", "tensor_copy", [srck], [tk], out=t[:, 0:n], in_=src[:, 0:n])
                        return t, tk

                    yield
                    bu, buk = mm_pairs(Lm, Lmk, None, None, trans=True)
                    yield
                    Um, Umk = evac_copy(bu, buk, "act")
                    Pt, Ptk = BFP.get()
                    OP("pool", "tensor_tensor", ["identb", Umk], [Ptk], out=v3(Pt), in0=identb[:].unsqueeze(1).to_broadcast([128, npair, 128]), in1=v3(Um), op=ALU.subtract)
                    Lp, Lpk, Up, Upk = Lm, Lmk, Um, Umk
                    for lvl in range(1, 6):
                        yield
                        b1, b1k = mm_pairs(Up, Upk, Lp, Lpk)
                        if lvl < 5:
                            b2, b2k = mm_pairs(Lp, Lpk, Up, Upk)
                        yield
                        Ln_, Lnk = evac_copy(b1, b1k, "act")
                        if lvl < 5:
                            Un_, Unk = evac_copy(b2, b2k, "dve")
                        yield
                        b3, b3k = mm_pairs(Ln_, Lnk, Pt, Ptk)
                        yield
                        Pn, Pnk = BFP.get()
                        OP("dve", "tensor_tensor", [b3k, Ptk], [Pnk], out=Pn[:, 0:n], in0=b3[:, 0:n], in1=Pt[:, 0:n], op=ALU.add)
                        Pt, Ptk = Pn, Pnk
                        Lp, Lpk = Ln_, Lnk
                        if lvl < 5:
                            Up, Upk = Un_, Unk
                    Tt, Ttk = Pt, Ptk
                    yield
                    bkt, bktk = mm_pairs(kn, knk, None, None, trans=True)
                    bvt, bvtk = mm_pairs(vT, vTk, None, None, trans=True)
                    yield
                    kbe, kbek = BFP.get()
                    for pi in range(npair):
                        cs_ = slice(pi * 128, (pi + 1) * 128)
                        OP("act", "activation", [bktk, "sc%dBEG" % d], [kbek], out=kbe[:, cs_], in_=bkt[:, cs_], func=AF.Identity, scale=BEG[:, p0 + pi, hd:hd + 1])
                        OP("act", "activation", [bktk, "sc%dEGR" % d], [(pk, "kd")], out=P["kd"][:, pi, :], in_=bkt[:, cs_], func=AF.Identity, scale=EGR[:, p0 + pi, hd:hd + 1])
                    vb, vbk = BFP.get()
                    for pi in range(npair):
                        cs_ = slice(pi * 128, (pi + 1) * 128)
                        OP("act", "activation", [bvtk, "sc%dB" % d], [vbk], out=vb[:, cs_], in_=bvt[:, cs_], func=AF.Identity, scale=Bt[:, p0 + pi, hd:hd + 1])
                    yield
                    bu2, bu2k = mm_pairs(Tt, Ttk, vb, vbk)
                    bw, bwk = mm_pairs(kbe, kbek, Tt, Ttk)
                    yield
                    OP("dve", "tensor_copy", [bu2k], [(pk, "u")], out=P["u"][:, 0:npair, :], in_=v3(bu2))
                    OP("act", "activation", [bwk], [(pk, "wT")], out=P["wT"][:, 0:n], in_=bw[:, 0:n], func=AF.Identity)

                def rec(gi, tpar):
                    o, n = groups[gi]
                    nch = n // 64
                    p0 = o // 128
                    is_lat = o >= S_CTX
                    for ci in range(nch):
                        pi, hf = ci // 2, ci % 2
                        Ps = slice(hf * 64, hf * 64 + 64)
                        for d in range(2):
                            P = pers[(d, tpar)]
                            pk = P["k"]
                            cur = hstate[d]
                            nxt = 1 - cur
                            hb_c, hb_n = hbf[d][cur], hbf[d][nxt]
                            bwh, bwhk = prot.get()
                            OP("pe", "matmul", [(pk, "wT"), ("hbf", par, d, cur)], [bwhk], out=bwh[Ps, 0:128], lhsT=P["wT"][:, ci * 64:(ci + 1) * 64], rhs=hb_c[:],
                               start=True, stop=True)
                            vn, vnk = VNP[d].get()
                            OP("dve", "tensor_tensor", [(pk, "u"), bwhk], [vnk], out=vn[Ps, :], in0=P["u"][Ps, pi, :], in1=bwh[Ps, 0:128], op=ALU.subtract)
                            if is_lat:
                                ob = banks[6 + d]
                                OP("pe", "matmul", [("hbf", par, d, cur), (pk, "qe")], [("obank", d)], out=ob[:, ci * 64:(ci + 1) * 64], lhsT=hb_c[:],
                                   rhs=P["qe"][:, ci * 64:(ci + 1) * 64], start=True, stop=False)
                                OP("pe", "matmul", [vnk, (pk, "qkt")], [("obank", d)], out=ob[:, ci * 64:(ci + 1) * 64], lhsT=vn[Ps, :],
                                   rhs=P["qkt"][Ps, pi, hf * 64:hf * 64 + 64], start=False, stop=True)
                            bh, bhk = prot.get()
                            OP("pe", "matmul", [(pk, "kd"), vnk], [bhk], out=bh[:, 0:128], lhsT=P["kd"][Ps, pi, :], rhs=vn[Ps, :], start=True, stop=True)
                            egl = EGLt[d][:, p0 + pi, hf, hd:hd + 1]
                            OP("dve", "scalar_tensor_tensor", [("h32", par, d), bhk, "sc%dEGL" % d], [("hbf", par, d, nxt)], out=hb_n[:], in0=h32[d][:], scalar=egl,
                               in1=bh[:, 0:128], op0=ALU.mult, op1=ALU.add)
                            OP("dve", "scalar_tensor_tensor", [("h32", par, d), bhk, "sc%dEGL" % d], [("h32", par, d)], out=h32[d][:], in0=h32[d][:], scalar=egl,
                               in1=bh[:, 0:128], op0=ALU.mult, op1=ALU.add)
                            hstate[d] = nxt
                            yield
                    if is_lat:
                        cl0 = (o - S_CTX) // 64
                        for d in range(2):
                            Ob, Obk = Obuf[0][d]
                            src = banks[6 + d][:, 0:512].rearrange("p (c i) -> p c i", i=64)
                            if d == 0:
                                OP("act", "activation", [("obank", d)], [Obk], out=Ob[:, cl0:cl0 + 8, :], in_=src[:, :, 0:32], func=AF.Identity)
                            else:
                                hi = 63 - cl0
                                OP("act", "activation", [("obank", d)], [Obk], out=Ob[:, hi - 7:hi + 1, :][:, ::-1, :], in_=src[:, :, 32:64][:, :, ::-1], func=AF.Identity)

                def finalize():
                  F32P, BFP = F32Pd[0], BFPd[0]
                  O0, O0k = Obuf[0][0]
                  O1, O1k = Obuf[0][1]
                  Of = O0[:].rearrange("p c r -> p (c r)")
                  OP("pool", "tensor_tensor", [O0k, O1k], [O0k], out=Of, in0=Of, in1=O1[:].rearrange("p c r -> p (c r)"), op=ALU.add)
                  for q4 in range(4):
                      cs_ = slice(q4 * 512, (q4 + 1) * 512)
                      SQ, SQk = BFP.get()
                      OP("pool", "tensor_tensor", [O0k], [SQk], out=SQ[:], in0=Of[:, cs_], in1=Of[:, cs_], op=ALU.mult)
                      bk, bkk = prot.get()
                      OP("pe", "matmul", ["onesb", SQk], [bkk], out=bk[:], lhsT=onesb[:], rhs=SQ[:], start=True, stop=True)
                      RS, RSk = F32P.get()
                      OP("act", "activation", [bkk], [RSk], out=RS[:], in_=bk[:], func=AF.Ln, scale=1.0 / 128, bias=EPS)
                      OP("act", "activation", [RSk], [RSk], out=RS[:], in_=RS[:], func=AF.Exp, scale=-0.5)
                      OP("dve", "scalar_tensor_tensor", [O0k, RSk, "gnorm"], [O1k], out=O1[:].rearrange("p c r -> p (c r)")[:, cs_], in0=Of[:, cs_],
                         scalar=gnorm[:, 0:1], in1=RS[:], op0=ALU.mult, op1=ALU.mult)
                  OP("pool", "tensor_tensor", [O1k, szk], [xok], out=xo[:].rearrange("p (r c) -> p r c", c=64), in0=O1[:].rearrange("p c r -> p r c"),
                     in1=sz[:].rearrange("p (r c) -> p r c", c=64), op=ALU.mult)
                  DMA(XDN[hd * 128:(hd + 1) * 128, :], xo[:], reads=[xok])

                pending = prev
                for gi in range(len(groups)):
                    tpar = tcount["t"] % 2
                    tcount["t"] += 1
                    gens = [prep(0, gi, tpar), prep(1, gi, tpar)]
                    if pending is not None:
                        gens.append(pending["rec"])
                    run_rr(gens)
                    if pending is not None and pending.get("fin") is not None:
                        pending["fin"]()
                    pending = dict(rec=rec(gi, tpar), fin=(finalize if gi == len(groups) - 1 else None))
                return pending

            pend = None
            for hd in dn_heads:
                pend = dn_head(hd, pend)
            run_rr([pend["rec"]])
            pend["fin"]()
            S.barrier()
            arena["off"] = mark0

        top = {"off": ARENA_WORDS}

        def sb_top(name, shape, dt=F32):
            shape = list(shape)
            elems = int(np.prod(shape[1:]))
            words = elems if dt == F32 else (elems + 1) // 2
            words += words % 2
            top["off"] -= words
            off = top["off"]
            ap = arena_t[0:shape[0], off:off + words]
            if dt != F32:
                ap = ap.bitcast(dt)[:, 0:elems]
            else:
                ap = ap[:, 0:elems]
            if len(shape) == 3:
                ap = ap.rearrange("p (a b) -> p a b", a=shape[1])
            return ap

        NT = OWN // 128
        if "tail" in phases:
            hfT = sb_top("hfT", [128, 8, OWN], BF16)
            CW = sb_top("CW", [128, NT, NE])
            G2P = sb_top("G2P", [128, D])
            prot = TP.__new__(TP)
            prot.t = [(banks[i], "bank%d" % i) for i in range(8)]
            prot.i = 0
            G1P = sb("G1P", [128, D])
            GP2 = sb("GP2", [128, D])
            SH2t = sb("SH2t", [128, D])
            rb = sb("rb", [128, NE])
            rw = sb("rw", [128, 8, NE])
            mark_t = arena["off"]
            MOD = sb("MOD", [128, 4, D])
            rows = sb("rows", [128, 3, D])
            csrep = sb("csrep", [128, 8, 128])
            adab_r = sb("adab_r", [128, 4 * D])
            DMA(adab_r[:], ada_b[:, 2048:6144].partition_broadcast(128), writes=["adab_r"])
            DMA(rows[:, 0, :], gpost.partition_broadcast(128), writes=["rows"])
            DMA(rows[:, 1, :], gfpre.partition_broadcast(128), writes=["rows"])
            DMA(rows[:, 2, :], gfpost.partition_broadcast(128), writes=["rows"])
            DMA(rb[:], router_b.partition_broadcast(128), writes=["rb"])
            DMA(rw[:], router_w.rearrange("(kc p) e -> p kc e", p=128), writes=["rw"])
            for kc in range(8):
                OP("dve", "tensor_copy", [], [("csrep", kc)], out=csrep[:, kc, :], in_=cs[:, kc, 0:1].to_broadcast([128, 128]))
            awp = TP("awp", 2, [128, 8, 512], F32)
            for pc_ in range(8):
                aw, awk = awp.get()
                for kc in range(8):
                    DMA(aw[:, kc, :], ada_w[kc * 128:(kc + 1) * 128, 2048 + pc_ * 512:2048 + (pc_ + 1) * 512], writes=[(awk, kc)])
                bk, bkk = prot.get()
                for kc in range(8):
                    OP("pe", "matmul", [(awk, kc), ("csrep", kc)], [bkk], out=bk[:], lhsT=csrep[:, kc, :], rhs=aw[:, kc, :], start=(kc == 0), stop=(kc == 7))
                OP("dve", "tensor_tensor", [bkk, "adab_r"], ["MOD"], out=MOD[:].rearrange("p j d -> p (j d)")[:, pc_ * 512:(pc_ + 1) * 512], in0=bk[:],
                   in1=adab_r[:, pc_ * 512:(pc_ + 1) * 512], op=ALU.add)
            OP("dve", "tensor_tensor", ["MOD", "rows"], ["G1P"], out=G1P[:], in0=MOD[:, 0, :], in1=rows[:, 0, :], op=ALU.mult)
            OP("dve", "scalar_tensor_tensor", ["MOD", "rows"], ["GP2"], out=GP2[:], in0=MOD[:, 2, :], scalar=1.0, in1=rows[:, 1, :], op0=ALU.add, op1=ALU.mult)
            OP("dve", "tensor_tensor", ["MOD", "rows"], ["G2P"], out=G2P[:], in0=MOD[:, 3, :], in1=rows[:, 2, :], op=ALU.mult)
            OP("pool", "tensor_copy", ["MOD"], ["SH2t"], out=SH2t[:], in_=MOD[:, 1, :])
            SH2 = SH2t[:]
            S.barrier()
            arena["off"] = mark_t
            n_blk = 4 if tail_stop >= 2 else 0
            Wrg = sb("Wrg", [128, 8, D], BF16)
            Wdn = sb("Wdn", [128, 8, D], BF16)
            Wou = sb("Wou", [128, 8, D], BF16)
            for (wt, src, wk) in ((Wrg, rg_w_o, "Wrg"), (Wdn, dn_w_o, "Wdn"), (Wou, w_out, "Wou")):
                for kc in range(8):
                    S.op("pool", lambda e, wt=wt, src=src, kc=kc: e.dma_start(out=wt[:, kc, :], in_=src[kc * 128:(kc + 1) * 128, :]), writes=[(wk, kc)], dma=True)
            xrb = sb("t_xrg", [128, 8, 512], BF16)
            xdb = sb("t_xdn", [128, 8, 512], BF16)
            s1b = sb("t_sg1", [128, 8, 512], BF16)
            s2b = sb("t_sg2", [128, 8, 512], BF16)
            mrg = sb("t_mrg", [128, 8, 512], BF16)
            F4 = TP("t_f4", 6, [128, D], F32)
            F2 = TP("t_f2", 4, [128, 512], F32)
            hf32 = TP("t_hf32", 2, [128, 8, 128], F32)
            sst = TP("t_ss", 4, [128, 8], F32)
            junk = sb("t_junk", [128, D], BF16)
            print("TAIL arena bottom", arena["off"], "top", top["off"])
            assert arena["off"] <= top["off"], "tail arena overlap"
            for blk in range(n_blk):
                t0 = blk * 512
                for kc in range(8):
                    DMA(xrb[:, kc, :], XRG[kc * 128:(kc + 1) * 128, t0:t0 + 512], writes=[("xrb", kc)])
                    DMA(xdb[:, kc, :], XDN[kc * 128:(kc + 1) * 128, t0:t0 + 512], writes=[("xdb", kc)])
                    DMA(s1b[:, kc, :], GATES[2048 + kc * 128:2048 + (kc + 1) * 128, t0:t0 + 512], writes=[("s1b", kc)])
                    DMA(s2b[:, kc, :], GATES[3072 + kc * 128:3072 + (kc + 1) * 128, t0:t0 + 512], writes=[("s2b", kc)])
                for m in range(8):
                    b1, b1k = prot.get()
                    b2, b2k = prot.get()
                    for kc in range(8):
                        OP("pe", "matmul", [("Wrg", kc), ("xrb", kc)], [b1k], out=b1[:], lhsT=Wrg[:, kc, m * 128:(m + 1) * 128], rhs=xrb[:, kc, :], start=(kc == 0), stop=(kc == 7))
                    for kc in range(8):
                        OP("pe", "matmul", [("Wdn", kc), ("xdb", kc)], [b2k], out=b2[:], lhsT=Wdn[:, kc, m * 128:(m + 1) * 128], rhs=xdb[:, kc, :], start=(kc == 0), stop=(kc == 7))
                    ta, tak = F2.get()
                    tb_, tbk = F2.get()
                    OP("dve", "tensor_tensor", [b1k, ("s1b", m)], [tak], out=ta[:], in0=b1[:], in1=s1b[:, m, :], op=ALU.mult)
                    OP("dve", "tensor_tensor", [b2k, ("s2b", m)], [tbk], out=tb_[:], in0=b2[:], in1=s2b[:, m, :], op=ALU.mult)
                    OP("pool", "tensor_tensor", [tak, tbk], [("mrg", m)], out=mrg[:, m, :], in0=ta[:], in1=tb_[:], op=ALU.add)
                for tt in range(4 if tail_stop >= 3 else 0):
                    gt_ = blk * 4 + tt
                    r0 = gt_ * 128
                    xt, xtk = F4.get()
                    DMA(xt[:], x[r0:r0 + 128, :], writes=[xtk])
                    ss, ssk = sst.get()
                    yb = []
                    for n2 in range(2):
                        bk, bkk = prot.get()
                        for kc in range(8):
                            OP("pe", "matmul", [("Wou", kc), ("mrg", kc)], [bkk], out=bk[:], lhsT=mrg[:, kc, tt * 128:(tt + 1) * 128], rhs=Wou[:, kc, n2 * 512:(n2 + 1) * 512],
                               start=(kc == 0), stop=(kc == 7))
                        OP("act", "activation", [bkk], ["t_junk", (ssk, n2)], out=junk[:, 0:512], in_=bk[:], func=AF.Square, accum_out=ss[:, n2:n2 + 1])
                        yb.append((bk, bkk))
                    if tail_stop < 3.2:
                        continue
                    OP("dve", "tensor_tensor", [(ssk, 0), (ssk, 1)], [(ssk, 2)], out=ss[:, 2:3], in0=ss[:, 0:1], in1=ss[:, 1:2], op=ALU.add)
                    OP("dve", "tensor_scalar", [(ssk, 2)], [(ssk, 2)], out=ss[:, 2:3], in0=ss[:, 2:3], scalar1=1.0 / D, scalar2=EPS, op0=ALU.mult, op1=ALU.add)
                    OP("act", "activation", [(ssk, 2)], [(ssk, 2)], out=ss[:, 2:3], in_=ss[:, 2:3], func=AF.Sqrt)
                    OP("dve", "reciprocal", [(ssk, 2)], [(ssk, 2)], out=ss[:, 2:3], in_=ss[:, 2:3])
                    xn, xnk = F4.get()
                    for n2 in range(2):
                        bk, bkk = yb[n2]
                        cs_ = slice(n2 * 512, (n2 + 1) * 512)
                        OP("dve", "scalar_tensor_tensor", [bkk, (ssk, 2), "G1P"], [(xnk, n2)], out=xn[:, cs_], in0=bk[:], scalar=ss[:, 2:3], in1=G1P[:, cs_], op0=ALU.mult, op1=ALU.mult)
                    OP("pool", "tensor_tensor", [(xnk, 0), (xnk, 1), xtk], [xnk, (xnk, 0), (xnk, 1)], out=xn[:], in0=xn[:], in1=xt[:], op=ALU.add)
                    if tail_stop < 3.4:
                        continue
                    DMA(XNEW[r0:r0 + 128, :], xn[:], reads=[xnk])
                    OP("act", "activation", [xnk], ["t_junk", (ssk, 3)], out=junk[:], in_=xn[:], func=AF.Square, accum_out=ss[:, 3:4])
                    OP("dve", "tensor_scalar", [(ssk, 3)], [(ssk, 3)], out=ss[:, 3:4], in0=ss[:, 3:4], scalar1=1.0 / D, scalar2=EPS, op0=ALU.mult, op1=ALU.add)
                    OP("act", "activation", [(ssk, 3)], [(ssk, 3)], out=ss[:, 3:4], in_=ss[:, 3:4], func=AF.Sqrt)
                    OP("dve", "reciprocal", [(ssk, 3)], [(ssk, 3)], out=ss[:, 3:4], in_=ss[:, 3:4])
                    if tail_stop < 3.6:
                        continue
                    hf, hfk = F4.get()
                    OP("dve", "scalar_tensor_tensor", [xnk, (ssk, 3), "GP2"], [hfk], out=hf[:], in0=xn[:], scalar=ss[:, 3:4], in1=GP2[:], op0=ALU.mult, op1=ALU.mult)
                    OP("pool", "tensor_tensor", [hfk, "SH2t"], [hfk], out=hf[:], in0=hf[:], in1=SH2, op=ALU.add)
                    if tail_stop < 3.8:
                        continue
                    h32t, h32k = hf32.get()
                    for half in range(2):
                        bk, bkk = prot.get()
                        for c4 in range(4):
                            c = half * 4 + c4
                            OP("pe", "transpose", [hfk, "ident"], [bkk], out=bk[:, c4 * 128:(c4 + 1) * 128], in_=hf[:, c * 128:(c + 1) * 128], identity=ident[:])
                        OP("dve", "tensor_copy", [bkk], [(h32k, half)], out=h32t[:, half * 4:half * 4 + 4, :], in_=bk[:].rearrange("p (c t) -> p c t", c=4))
                        OP("pool", "tensor_copy", [(h32k, half)], [("hfT", gt_, half)], out=hfT[:, half * 4:half * 4 + 4, r0:r0 + 128], in_=h32t[:, half * 4:half * 4 + 4, :])
                    if tail_stop < 4:
                        continue
                    bl, blk_ = prot.get()
                    for kc in range(8):
                        OP("pe", "matmul", [(h32k, kc // 4), "rw"], [blk_], out=bl[:, 0:NE], lhsT=h32t[:, kc, :], rhs=rw[:, kc, :], start=(kc == 0), stop=(kc == 7))
                    lg, lgk = F2.get()
                    OP("dve", "tensor_tensor", [blk_, "rb"], [lgk], out=lg[:, 0:NE], in0=bl[:, 0:NE], in1=rb[:], op=ALU.add)
                    OP("dve", "max", [lgk], [(lgk, "m8")], out=lg[:, 64:72], in_=lg[:, 0:NE])
                    OP("dve", "tensor_scalar", [lgk, (lgk, "m8")], [(lgk, "mask")], out=lg[:, 128:128 + NE], in0=lg[:, 0:NE], scalar1=lg[:, 67:68], scalar2=None, op0=ALU.is_ge)
                    OP("dve", "tensor_scalar", [(lgk, "m8")], [(lgk, "nm")], out=lg[:, 72:73], in0=lg[:, 64:65], scalar1=-1.0, scalar2=None, op0=ALU.mult)
                    OP("act", "activation", [lgk, (lgk, "nm")], [(lgk, "e")], out=lg[:, 192:192 + NE], in_=lg[:, 0:NE], func=AF.Exp, bias=lg[:, 72:73])
                    OP("dve", "tensor_tensor", [(lgk, "e"), (lgk, "mask")], [(lgk, "em")], out=lg[:, 256:256 + NE], in0=lg[:, 192:192 + NE], in1=lg[:, 128:128 + NE], op=ALU.mult)
                    OP("dve", "tensor_reduce", [(lgk, "em")], [(lgk, "sum")], out=lg[:, 73:74], in_=lg[:, 256:256 + NE], axis=mybir.AxisListType.X, op=ALU.add)
                    OP("dve", "reciprocal", [(lgk, "sum")], [(lgk, "sum")], out=lg[:, 73:74], in_=lg[:, 73:74])
                    OP("dve", "tensor_scalar", [(lgk, "em"), (lgk, "sum")], [("CW", gt_)], out=CW[:, gt_, :], in0=lg[:, 256:256 + NE], scalar1=lg[:, 73:74], scalar2=None, op0=ALU.mult)
            if debug:
                dbg("CW", CW[:], [("CW", i) for i in range(NT)])
                dbg("hfT", hfT[:, :, :], [("hfT", i, h) for i in range(NT) for h in range(2)])
            S.barrier()
            arena["off"] = mark0

        if "moe" in phases:
            prot = TP.__new__(TP)
            prot.t = [(banks[i], "bank%d" % i) for i in range(8)]
            prot.i = 0
            acc = sb("acc", [128, NT, D])
            b1T = sb("b1T", [128, NE, 16])
            mark_m = arena["off"]
            eb2 = sb("eb2", [NE, D])
            cwT = sb("cwT", [NE, NT, 128])
            DMA(b1T[:], e_b1T, writes=["b1T"])
            DMA(eb2[:], e_b2, writes=["eb2"])
            for tt in range(NT):
                bk, bkk = prot.get()
                OP("pe", "transpose", [], [bkk], out=bk[0:NE, 0:128], in_=CW[:, tt, :], identity=ident[:])
                OP("dve", "tensor_copy", [bkk], [("cwT", tt)], out=cwT[:, tt, :], in_=bk[0:NE, 0:128])
                for n2 in range(2):
                    bk2, bk2k = prot.get()
                    OP("pe", "matmul", [("cwT", tt), "eb2"], [bk2k], out=bk2[:], lhsT=cwT[:, tt, :], rhs=eb2[:, n2 * 512:(n2 + 1) * 512], start=True, stop=True)
                    OP("act", "activation", [bk2k], [("acc", tt, n2)], out=acc[:, tt, n2 * 512:(n2 + 1) * 512], in_=bk2[:], func=AF.Identity)
            S.barrier()
            arena["off"] = mark_m
            w1b = sb("w1b", [128, 8, 2 * D], BF16)
            w2b = sb("w2b", [128, 8, D], BF16)
            stg = TP("m_stg", 4, [128, D], F32)
            stg2 = TP("m_stg2", 2, [128, D], F32)
            actT = sb("actT", [128, 8, OWN // 2], BF16)
            EF = TP("m_ef", 6, [128, 512], F32)
            print("MOE arena bottom", arena["off"], "top", top["off"])
            assert arena["off"] <= top["off"], "moe arena overlap"
            n_exp = moe_experts
            w1v = w1b[:].rearrange("p kc (g m c) -> p kc g m c", g=2, m=8)
            w1_stage = {}
            w1_next = {}

            def w1_dma(e_):
                u = w1_next.get(e_, 0)
                if u >= 16:
                    return
                w1_next[e_] = u + 1
                m, g = u // 2, u % 2
                sg, sgk = stg.get()
                src = e_w1[e_].rearrange("(kc p) (g m c) -> p kc g m c", p=128, g=2, m=8)[:, :, g, m, :]
                DMA(sg[:].rearrange("p (kc c) -> p kc c", kc=8), src, writes=[sgk])
                w1_stage[(e_, u)] = (sg, sgk)

            def w1_cast(e_, u):
                m, g = u // 2, u % 2
                sg, sgk = w1_stage.pop((e_, u))
                OP("act", "activation", [sgk], [("w1b", m, g)], out=w1v[:, :, g, m, :], in_=sg[:].rearrange("p (kc c) -> p kc c", kc=8),
                   func=AF.Identity)

            def w2_load(e_):
                for kc in range(8):
                    sg, sgk = stg2.get()
                    DMA(sg[:], e_w2[e_, kc * 128:(kc + 1) * 128, :], writes=[sgk])
                    OP("pool", "tensor_copy", [sgk], [("w2b", kc)], out=w2b[:, kc, :], in_=sg[:])

            for u in range(16):
                w1_dma(0)
                w1_cast(0, u)
            for e_ in range(n_exp):
                w2_load(e_)
                nxt = e_ + 1 if e_ + 1 < n_exp else None
                for half in range(2):
                    h0 = half * (OWN // 2)
                    for m in range(8):
                        for nt_ in range(2):
                            tk0 = h0 + nt_ * 512
                            bg, bgk = prot.get()
                            bl, blk_ = prot.get()
                            for kc in range(8):
                                OP("pe", "matmul", [("w1b", m, 0)], [bgk], out=bg[:], lhsT=w1b[:, kc, m * 128:(m + 1) * 128], rhs=hfT[:, kc, tk0:tk0 + 512], start=(kc == 0), stop=(kc == 7))
                            for kc in range(8):
                                OP("pe", "matmul", [("w1b", m, 1)], [blk_], out=bl[:], lhsT=w1b[:, kc, D + m * 128:D + (m + 1) * 128], rhs=hfT[:, kc, tk0:tk0 + 512], start=(kc == 0), stop=(kc == 7))
                            tg, tgk = EF.get()
                            ts, tsk = EF.get()
                            tl, tlk = EF.get()
                            OP("dve", "tensor_scalar", [bgk, "b1T"], [tgk], out=tg[:], in0=bg[:], scalar1=b1T[:, e_, m:m + 1], scalar2=7.0, op0=ALU.add, op1=ALU.min)
                            OP("act", "activation", [tgk], [tsk], out=ts[:], in_=tg[:], func=AF.Sigmoid, scale=1.702)
                            OP("dve", "tensor_scalar", [blk_, "b1T"], [tlk], out=tl[:], in0=bl[:], scalar1=b1T[:, e_, 8 + m:9 + m], scalar2=7.0, op0=ALU.add, op1=ALU.min)
                            OP("dve", "tensor_scalar", [tlk], [tlk], out=tl[:], in0=tl[:], scalar1=-7.0, scalar2=1.0, op0=ALU.max, op1=ALU.add)
                            OP("pool", "tensor_tensor", [tgk, tsk], [tgk], out=tg[:], in0=tg[:], in1=ts[:], op=ALU.mult)
                            OP("pool", "tensor_tensor", [tgk, tlk], [("actT", m, nt_)], out=actT[:, m, nt_ * 512:(nt_ + 1) * 512], in0=tg[:], in1=tl[:], op=ALU.mult)
                        if nxt is not None:
                            if half == 0 and m == 7:
                                for _ in range(3):
                                    w1_dma(nxt)
                            if half == 1:
                                w1_cast(nxt, 2 * m)
                                w1_dma(nxt)
                                w1_cast(nxt, 2 * m + 1)
                                w1_dma(nxt)
                    for tt in range(8):
                        gt_ = half * 8 + tt
                        for n2 in range(2):
                            bk, bkk = prot.get()
                            for m in range(8):
                                OP("pe", "matmul", [("actT", m, tt // 4), ("w2b", m)], [bkk], out=bk[:], lhsT=actT[:, m, tt * 128:(tt + 1) * 128], rhs=w2b[:, m, n2 * 512:(n2 + 1) * 512],
                                   start=(m == 0), stop=(m == 7))
                            OP("dve", "scalar_tensor_tensor", [bkk, ("acc", gt_, n2)], [("acc", gt_, n2)], out=acc[:, gt_, n2 * 512:(n2 + 1) * 512], in0=bk[:],
                               scalar=CW[:, gt_, e_:e_ + 1], in1=acc[:, gt_, n2 * 512:(n2 + 1) * 512], op0=ALU.mult, op1=ALU.add)
            S.barrier()
            arena["off"] = mark_m
            FX = TP("m_fx", 3, [128, D], F32)
            FO = TP("m_fo", 3, [128, D], F32)
            sst2 = TP("m_ss", 3, [128, 2], F32)
            junk2 = sb("m_junk", [128, D], BF16)
            outs = []
            for tt in range(NT):
                r0 = tt * 128
                xn, xnk = FX.get()
                DMA(xn[:], XNEW[r0:r0 + 128, :], writes=[xnk])
                ss, ssk = sst2.get()
                OP("act", "activation", [("acc", tt, 0), ("acc", tt, 1)], ["m_junk", ssk], out=junk2[:], in_=acc[:, tt, :], func=AF.Square, accum_out=ss[:, 0:1])
                OP("dve", "tensor_scalar", [ssk], [ssk], out=ss[:, 0:1], in0=ss[:, 0:1], scalar1=1.0 / D, scalar2=EPS, op0=ALU.mult, op1=ALU.add)
                OP("act", "activation", [ssk], [ssk], out=ss[:, 0:1], in_=ss[:, 0:1], func=AF.Sqrt)
                OP("dve", "reciprocal", [ssk], [ssk], out=ss[:, 0:1], in_=ss[:, 0:1])
                fo, fok = FO.get()
                OP("dve", "scalar_tensor_tensor", [("acc", tt, 0), ("acc", tt, 1), ssk], [fok], out=fo[:], in0=acc[:, tt, :], scalar=ss[:, 0:1], in1=G2P[:], op0=ALU.mult, op1=ALU.mult)
                OP("pool", "tensor_tensor", [fok, xnk], [fok], out=fo[:], in0=fo[:], in1=xn[:], op=ALU.add)
                outs.append(S.op("sp", lambda e, fo=fo, r0=r0: e.dma_start(out=out[r0:r0 + 128, :], in_=fo[:]), reads=[fok], dma=True))
            S.barrier()

        S.emit(st)
    return nc


def prepare_core_inputs(inputs, b, half):
    f = np.ascontiguousarray
    flip = (half == 1)
    xs = inputs["x"][b]
    cs = inputs["ctx"][b]
    if flip:
        xs = xs[::-1]
        cs = cs[::-1]
    w_in = inputs["w_in"][0]
    if flip:
        w_in = w_in.copy()
        a0 = 6144
        w_in[:, a0:a0 + 8], w_in[:, a0 + 8:a0 + 16] = inputs["w_in"][0][:, a0 + 8:a0 + 16], inputs["w_in"][0][:, a0:a0 + 8]
        b0 = 6160
        w_in[:, b0:b0 + 8], w_in[:, b0 + 8:b0 + 16] = inputs["w_in"][0][:, b0 + 8:b0 + 16], inputs["w_in"][0][:, b0:b0 + 8]
    cvec = np.stack([inputs["c"][b], inputs["c_ctx"]], axis=-1)
    m = {
        "x": f(xs), "ctx": f(cs),
        "cc": f(cvec.reshape(8, 128, 2).transpose(1, 0, 2)),
        "ada_w": f(inputs["ada_w"][0]),
        "ada_bT": f(inputs["ada_b"][0].reshape(48, 128).T),
        "ada_b": f(inputs["ada_b"][0].reshape(1, -1)),
        "gpreT": f(inputs["mix_pre_g"][0].reshape(8, 128).T),
        "w_in": f(w_in),
    }
    dsel = (lambda a: a[::-1]) if flip else (lambda a: a)
    P = lambda k: dsel(inputs[k][0])
    rgp = np.concatenate([P("rg_conv_w").transpose(0, 2, 1),
                          P("rg_conv_b")[:, :, None], P("rg_ba")[:, :, None], P("rg_bi")[:, :, None], P("rg_lam")[:, :, None]], axis=2)
    m["rgp"] = f(rgp.reshape(2, 8, 128, 8).transpose(2, 1, 0, 3))
    m["rg_wa"] = f(P("rg_wa"))
    m["rg_wi"] = f(P("rg_wi"))
    cw = P("dn_conv_w")
    m["dncw"] = f(cw.reshape(2, 4, 24, 128).transpose(3, 2, 0, 1))
    m["dnc"] = f(np.concatenate([P("dn_a_log").reshape(-1), P("dn_dt_bias").reshape(-1)]).reshape(1, 32))
    m["dn_norm_g"] = f(inputs["dn_norm_g"][0].reshape(128, 1))
    m["gpost"] = f(inputs["mix_post_g"][0].reshape(1, -1))
    m["gfpre"] = f(inputs["ffn_pre_g"][0].reshape(1, -1))
    m["gfpost"] = f(inputs["ffn_post_g"][0].reshape(1, -1))
    for k in ("rg_w_o", "dn_w_o", "w_out", "router_w", "e_w1", "e_w2", "e_b2"):
        m[k] = f(inputs[k][0])
    m["router_b"] = f(inputs["router_b"][0].reshape(1, -1))
    m["e_b1T"] = f(inputs["e_b1"][0].reshape(NE, 16, 128).transpose(2, 0, 1))
    return {k: np.asarray(v, dtype=np.float32) for k, v in m.items()}


_PROG = {}


def kernel(**inputs):
    inputs = {k: np.asarray(v) for k, v in inputs.items()}
    if "nc" not in _PROG:
        _PROG["nc"] = build_program()
    nc = _PROG["nc"]
    in_maps = []
    for core in range(8):
        b, half = core // 2, core % 2
        in_maps.append(prepare_core_inputs(inputs, b, half))
    res = run_bass_kernel_spmd(nc, in_maps, core_ids=list(range(8)))
    B = inputs["x"].shape[0]
    outp = np.zeros((B, S_LAT, D), np.float32)
    for core in range(8):
        b, half = core // 2, core % 2
        o = np.asarray(res.results[core]["out"], dtype=np.float32)
        if half == 0:
            outp[b, 0:OWN] = o
        else:
            outp[b, S_LAT - OWN:] = o[::-1]
    return outp
```
